# Optimizing a Trainium2 kernel written in Bass

```python
import math
import jax
import jax.numpy as jnp
from jax import lax
import numpy as np

D_MODEL = 1024
BATCH = 4
SEQ = 8192
DEPTH = 2

N_BRANCH = 4
HEAD_DIM = 64
MIX_W = D_MODEL // N_BRANCH
N_HEADS_MIX = MIX_W // HEAD_DIM
N_META = 16
CHUNK = 64
PAD = CHUNK - N_META
NEG = -1e30
RWKV_LORA_W = 64
RWKV_LORA_A = 64
RWKV_LORA_G = 128
RWKV_GN_EPS = HEAD_DIM * 1e-5
GLA_DK = HEAD_DIM // 2
GLA_LORA = 16
GLA_TAU = 16.0
DSA_KV_RANK = 128
IDX_HEADS = 8
IDX_DIM = 32
TOPK_MAX = 256
Q_BLOCK = 128
N_BUCKETS = 32
MAX_DISTANCE = 128
CONV_W = 4
N_GROUPS = 4
EXPERTS_PER_GROUP = 4
N_EXPERTS = N_GROUPS * EXPERTS_PER_GROUP
TOP_K_INNER = 2
D_EXPERT = D_MODEL // 4
DN_ALPHA = (2 * DEPTH) ** 0.25
DN_BETA = (8 * DEPTH) ** -0.25
LN_EPS = 1e-5

RWKV_SPLITS = (MIX_W, MIX_W, MIX_W, RWKV_LORA_W, RWKV_LORA_A, RWKV_LORA_G)
GLA_SPLITS = (N_HEADS_MIX * GLA_DK, N_HEADS_MIX * GLA_DK, MIX_W, GLA_LORA, MIX_W)
DSA_SPLITS = (MIX_W, DSA_KV_RANK, IDX_HEADS * IDX_DIM, IDX_DIM, IDX_HEADS)
MLSTM_SPLITS = (MIX_W, MIX_W, MIX_W, N_HEADS_MIX, N_HEADS_MIX, MIX_W)
IN_SPLITS = (sum(RWKV_SPLITS), sum(GLA_SPLITS), sum(DSA_SPLITS), sum(MLSTM_SPLITS), N_BRANCH * D_MODEL)
IN_COLS = sum(IN_SPLITS)

kernel_name = 'hybrid_gated_rwkv7_gla_dsa_mlstm_hmoe'


def _split(a, sizes):
    return jnp.split(a, [int(s) for s in np.cumsum(sizes)[:-1]], axis=-1)


def _layer_norm(x, g, b):
    xf = x.astype(jnp.float32)
    mu = jnp.mean(xf, -1, keepdims=True)
    var = jnp.mean(jnp.square(xf - mu), -1, keepdims=True)
    return ((xf - mu) * lax.rsqrt(var + LN_EPS)).astype(x.dtype) * g + b


def _std_norm(y, eps):
    mu = jnp.mean(y, -1, keepdims=True)
    var = jnp.mean(jnp.square(y - mu), -1, keepdims=True)
    return (y - mu) * lax.rsqrt(var + eps)


def _rms(y, eps=1e-6):
    return y * lax.rsqrt(jnp.mean(jnp.square(y), -1, keepdims=True) + eps)


def _token_shift(a):
    return jnp.pad(a, ((0, 0), (1, 0), (0, 0)))[:, :-1]


def _causal_dwconv(a, w, b):
    out = lax.conv_general_dilated(a, w.astype(a.dtype)[:, None, :], (1,), [(CONV_W - 1, 0)],
                                   dimension_numbers=('NWC', 'WIO', 'NWC'),
                                   feature_group_count=a.shape[-1])
    return out + b


def _to_chunks(t, n_heads, fill=0.0):
    B, T, C = t.shape
    t = jnp.pad(t, ((0, 0), (PAD, 0), (0, 0)), constant_values=fill)
    return t.reshape(B, (T + PAD) // CHUNK, CHUNK, n_heads, C // n_heads).transpose(0, 3, 1, 2, 4)


def _from_chunks(t):
    B, H, NC, L, d = t.shape
    return t.transpose(0, 2, 3, 1, 4).reshape(B, NC * L, H, d)[:, PAD:]


def _t5_bucket(dist):
    max_exact = N_BUCKETS // 2
    n = jnp.maximum(dist, 0)
    large = max_exact + (jnp.log(jnp.maximum(n, 1).astype(jnp.float32) / max_exact)
                         / math.log(MAX_DISTANCE / max_exact) * (N_BUCKETS - max_exact)).astype(jnp.int32)
    return jnp.where(n < max_exact, n, jnp.minimum(large, N_BUCKETS - 1))


def _rwkv7_mixer(p, mu, w_up, w0, a_up, a0, g_up, k_k, k_a, r_k, gn_g, gn_b):
    B, T, _ = p.shape
    H, N = N_HEADS_MIX, HEAD_DIM
    p = p + (_token_shift(p) - p) * mu
    r, k, v, xw, xa, xg = _split(p, RWKV_SPLITS)
    w_log = -jax.nn.softplus(-(w0 + jnp.tanh(xw) @ w_up)) - 0.5
    decay = jnp.exp(-jnp.exp(w_log))
    a = jax.nn.sigmoid(a0 + xa @ a_up)
    g = jax.nn.sigmoid(xg) @ g_up
    kk = (k * k_k).reshape(B, T, H, N)
    kk = kk / jnp.maximum(jnp.sqrt(jnp.sum(kk * kk, -1, keepdims=True)), 1e-12)
    k = k * (1.0 + (a - 1.0) * k_a)
    r, decay, k, v, a = (t.reshape(B, T, H, N) for t in (r, decay, k, v, a))

    def step(s, inp):
        r_t, w_t, k_t, v_t, kk_t, a_t = inp
        s_kk = jnp.einsum('bhvk,bhk->bhv', s, kk_t)
        s = (s * w_t[:, :, None, :] - s_kk[..., None] * (kk_t * a_t)[:, :, None, :]
             + v_t[..., None] * k_t[:, :, None, :])
        return s, jnp.einsum('bhvk,bhk->bhv', s, r_t)

    s0 = jnp.zeros((B, H, N, N), jnp.float32)
    _, y = lax.scan(step, s0, tuple(jnp.moveaxis(t, 1, 0) for t in (r, decay, k, v, kk, a)))
    y = jnp.moveaxis(y, 0, 1)
    y = _std_norm(y, RWKV_GN_EPS).reshape(B, T, H * N) * gn_g + gn_b
    bonus = jnp.sum(r * k * r_k, -1, keepdims=True) * v
    return (y + bonus.reshape(B, T, H * N)) * g


def _gla_mixer(p, a_up, a_b, norm_g):
    B, T, _ = p.shape
    H = N_HEADS_MIX
    q, k, v, xa, og = _split(p, GLA_SPLITS)
    la = jax.nn.log_sigmoid(xa @ a_up + a_b) / GLA_TAU
    q = _to_chunks(q, H) * GLA_DK ** -0.5
    k = _to_chunks(k, H)
    v = _to_chunks(v, H)
    la = _to_chunks(la, H)
    b = jnp.cumsum(la, axis=3)
    b_last = b[:, :, :, -1:]
    q_g = q * jnp.exp(b)
    att = jnp.einsum('bhcld,bhcsd->bhcls', q_g, k * jnp.exp(-b))
    att = jnp.where(jnp.tril(jnp.ones((CHUNK, CHUNK), bool)), att, 0.0)
    o = jnp.einsum('bhcls,bhcsv->bhclv', att, v)
    u = jnp.einsum('bhcsd,bhcsv->bhcdv', k * jnp.exp(b_last - b), v)
    dec = jnp.exp(b_last[:, :, :, 0])

    def step(s, inp):
        dec_c, u_c = inp
        return dec_c[..., None] * s + u_c, s

    s0 = jnp.zeros((B, H, GLA_DK, HEAD_DIM), jnp.float32)
    _, s_in = lax.scan(step, s0, (jnp.moveaxis(dec, 2, 0), jnp.moveaxis(u, 2, 0)))
    o = o + jnp.einsum('bhcld,bhcdv->bhclv', q_g, jnp.moveaxis(s_in, 0, 2))
    o = _rms(_from_chunks(o)) * norm_g
    return o.reshape(B, T, H * HEAD_DIM) * jax.nn.silu(og)


def _dsa_mixer(p, kv_norm_g, w_uk, w_uv, rel_bias, topk):
    B, T, _ = p.shape
    H, N = N_HEADS_MIX, HEAD_DIM
    q, ckv, qi, ki, wi = _split(p, DSA_SPLITS)
    c = _rms(ckv) * kv_norm_g
    k = c @ w_uk
    v = c @ w_uv
    n_blk = -(-T // Q_BLOCK)
    tq_len = n_blk * Q_BLOCK

    def blocks(t):
        t = jnp.pad(t, ((0, 0), (0, tq_len - T)) + ((0, 0),) * (t.ndim - 2))
        return jnp.moveaxis(t.reshape((B, n_blk, Q_BLOCK) + t.shape[2:]), 1, 0)

    q_b = blocks(q.reshape(B, T, H, N) * N ** -0.5)
    qi_b = blocks(qi.reshape(B, T, IDX_HEADS, IDX_DIM))
    wi_b = blocks(wi * (IDX_HEADS * IDX_DIM) ** -0.5)
    pos_b = jnp.arange(tq_len, dtype=jnp.int32).reshape(n_blk, Q_BLOCK)
    key_pos = jnp.arange(T, dtype=jnp.int32)
    gather = jax.vmap(lambda src, idx: src[idx])

    def attend(args):
        qb, qib, wib, tq = args
        rel = jax.nn.relu(jnp.einsum('bqhd,bsd->bqhs', qib, ki))
        score = jnp.einsum('bqhs,bqh->bqs', rel, wib)
        score = jnp.where(key_pos < N_META, jnp.inf, score)
        score = jnp.where(key_pos[None, :] <= tq[:, None], score, -jnp.inf)
        _, idx = lax.top_k(score, topk)
        valid = idx <= tq[None, :, None]
        k_sel = gather(k, idx)
        v_sel = gather(v, idx)
        logits = jnp.einsum('bqhd,bqkd->bhqk', qb, k_sel)
        bias = rel_bias[_t5_bucket(tq[None, :, None] - idx)]
        logits = logits + jnp.moveaxis(bias, -1, 1)
        logits = jnp.where(valid[:, None], logits, -jnp.inf)
        prob = jax.nn.softmax(logits, axis=-1)
        return jnp.einsum('bhqk,bqkd->bqhd', prob, v_sel)

    out = lax.map(attend, (q_b, qi_b, wi_b, pos_b))
    return jnp.moveaxis(out, 0, 1).reshape(B, tq_len, H * N)[:, :T]


def _mlstm_mixer(p, conv_w, conv_b, i_b, f_b, norm_g):
    B, T, _ = p.shape
    H, N = N_HEADS_MIX, HEAD_DIM
    q, k, v, ig, fg, og = _split(p, MLSTM_SPLITS)
    qk = jax.nn.silu(_causal_dwconv(jnp.concatenate([q, k], -1), conv_w, conv_b))
    q = _to_chunks(qk[..., :MIX_W], H)
    k = _to_chunks(qk[..., MIX_W:], H) * N ** -0.5
    v = _to_chunks(v, H)
    li = _to_chunks(ig + i_b, H, NEG)[..., 0]
    lf = _to_chunks(jax.nn.log_sigmoid(fg + f_b), H)[..., 0]
    b = jnp.cumsum(lf, -1)
    b_last = b[..., -1]
    g_loc = b_last[..., None] - b + li
    m_loc = jnp.max(g_loc, -1)
    w_loc = jnp.exp(g_loc - m_loc[..., None])
    c_loc = jnp.einsum('bhcs,bhcsd,bhcsv->bhcdv', w_loc, k, v)
    n_loc = jnp.einsum('bhcs,bhcsd->bhcd', w_loc, k)

    def step(carry, inp):
        c_st, n_st, m_st = carry
        bl, ml, cl, nl = inp
        m_new = jnp.maximum(bl + m_st, ml)
        s_old = jnp.exp(bl + m_st - m_new)
        s_new = jnp.exp(ml - m_new)
        return ((s_old[..., None, None] * c_st + s_new[..., None, None] * cl,
                 s_old[..., None] * n_st + s_new[..., None] * nl, m_new), (c_st, n_st, m_st))

    init = (jnp.zeros((B, H, N, N), jnp.float32), jnp.zeros((B, H, N), jnp.float32),
            jnp.zeros((B, H), jnp.float32))
    _, (c_in, n_in, m_in) = lax.scan(step, init, tuple(jnp.moveaxis(t, 2, 0) for t in (b_last, m_loc, c_loc, n_loc)))
    c_in, n_in, m_in = (jnp.moveaxis(t, 0, 2) for t in (c_in, n_in, m_in))
    causal = jnp.tril(jnp.ones((CHUNK, CHUNK), bool))
    d_log = jnp.where(causal, b[..., :, None] - b[..., None, :] + li[..., None, :], -jnp.inf)
    inter = b + m_in[..., None]
    m_t = jnp.maximum(inter, jnp.max(d_log, -1))
    s_w = jnp.exp(d_log - m_t[..., None]) * jnp.einsum('bhcld,bhcsd->bhcls', q, k)
    w_inter = jnp.exp(inter - m_t)
    num = (jnp.einsum('bhcls,bhcsv->bhclv', s_w, v)
           + w_inter[..., None] * jnp.einsum('bhcld,bhcdv->bhclv', q, c_in))
    den = jnp.sum(s_w, -1) + w_inter * jnp.einsum('bhcld,bhcd->bhcl', q, n_in)
    h = num / jnp.maximum(jnp.abs(den), jnp.exp(-m_t))[..., None]
    h = _from_chunks(h) * jax.nn.sigmoid(og).reshape(B, T, H, N)
    return _std_norm(h, 1e-5).reshape(B, T, H * N) * norm_g


def _hier_moe(x, w_grp, b_grp, w_rt, b_rt, w_gate, w_up, w_down):
    B, T, D = x.shape
    xf = x.reshape(B * T, D)
    g_logit = (xf @ w_grp).astype(jnp.float32) + b_grp
    g_sel = jnp.argmax(g_logit, -1)
    p_grp = jnp.take_along_axis(jax.nn.softmax(g_logit, -1), g_sel[:, None], 1)
    e_logit = ((xf @ w_rt).astype(jnp.float32) + b_rt).reshape(-1, N_GROUPS, EXPERTS_PER_GROUP)
    e_logit = jnp.take_along_axis(e_logit, g_sel[:, None, None], 1)[:, 0]
    top_val, top_idx = lax.top_k(e_logit, TOP_K_INNER)
    w = jax.nn.softmax(top_val, -1) * p_grp
    e_idx = g_sel[:, None] * EXPERTS_PER_GROUP + top_idx
    gate = jnp.einsum('nke,nk->ne', jax.nn.one_hot(e_idx, N_EXPERTS, dtype=jnp.float32), w).astype(x.dtype)
    y = jnp.zeros_like(xf)
    for e in range(N_EXPERTS):
        hid = jax.nn.silu(xf @ w_gate[e]) * (xf @ w_up[e])
        y = y + gate[:, e:e + 1] * (hid @ w_down[e])
    return y.reshape(B, T, D)


def setup_inputs(seed: int = 0) -> dict:
    key = jax.random.key(seed)
    keys = iter([jax.random.fold_in(key, i) for i in range(64)])

    def nrm(shape, scale):
        return jax.random.normal(next(keys), shape, jnp.float32) * scale

    def unif(shape, lo, hi):
        return jax.random.uniform(next(keys), shape, jnp.float32, minval=lo, maxval=hi)

    def gain(shape):
        return 1.0 + nrm(shape, 0.02)

    L, D, H, N, W = DEPTH, D_MODEL, N_HEADS_MIX, HEAD_DIM, MIX_W
    return {
        'x': nrm((BATCH, SEQ, D), 1.0),
        'meta': nrm((N_META, D), 1.0),
        'ln_in_g': gain((D,)),
        'ln_in_b': nrm((D,), 0.02),
        'rel_bias': nrm((N_BUCKETS, H), 0.5),
        'w_in': nrm((L, D, IN_COLS), D ** -0.5),
        'rwkv_mu': unif((L, sum(RWKV_SPLITS)), 0.0, 1.0),
        'rwkv_w_up': nrm((L, RWKV_LORA_W, W), RWKV_LORA_W ** -0.5),
        'rwkv_w0': unif((L, W), -6.0, 1.0),
        'rwkv_a_up': nrm((L, RWKV_LORA_A, W), RWKV_LORA_A ** -0.5),
        'rwkv_a0': nrm((L, W), 0.1),
        'rwkv_g_up': nrm((L, RWKV_LORA_G, W), RWKV_LORA_G ** -0.5),
        'rwkv_k_k': 0.85 + nrm((L, W), 0.02),
        'rwkv_k_a': gain((L, W)),
        'rwkv_r_k': nrm((L, H, N), 0.1),
        'rwkv_gn_g': gain((L, W)),
        'rwkv_gn_b': nrm((L, W), 0.02),
        'gla_a_up': nrm((L, GLA_LORA, H * GLA_DK), GLA_LORA ** -0.5),
        'gla_a_b': nrm((L, H * GLA_DK), 0.1),
        'gla_norm_g': gain((L, N)),
        'dsa_kv_norm_g': gain((L, DSA_KV_RANK)),
        'dsa_w_uk': nrm((L, DSA_KV_RANK, N), DSA_KV_RANK ** -0.5),
        'dsa_w_uv': nrm((L, DSA_KV_RANK, N), DSA_KV_RANK ** -0.5),
        'mlstm_conv_w': nrm((L, CONV_W, 2 * W), CONV_W ** -0.5),
        'mlstm_conv_b': nrm((L, 2 * W), 0.02),
        'mlstm_i_b': nrm((L, H), 0.5),
        'mlstm_f_b': unif((L, H), 3.0, 6.0),
        'mlstm_norm_g': gain((L, W)),
        'w_branch': nrm((L, N_BRANCH, W, D), DN_BETA * W ** -0.5),
        'w_out': nrm((L, D, D), DN_BETA * D ** -0.5),
        'ln1_g': gain((L, D)),
        'ln1_b': nrm((L, D), 0.02),
        'moe_w_grp': nrm((L, D, N_GROUPS), D ** -0.5),
        'moe_b_grp': nrm((L, N_GROUPS), 0.01),
        'moe_w_rt': nrm((L, D, N_EXPERTS), D ** -0.5),
        'moe_b_rt': nrm((L, N_EXPERTS), 0.01),
        'moe_w_gate': nrm((L, N_EXPERTS, D, D_EXPERT), D ** -0.5),
        'moe_w_up': nrm((L, N_EXPERTS, D, D_EXPERT), D ** -0.5),
        'moe_w_down': nrm((L, N_EXPERTS, D_EXPERT, D), DN_BETA * D_EXPERT ** -0.5),
        'ln2_g': gain((L, D)),
        'ln2_b': nrm((L, D), 0.02),
    }


def reference(x, meta, ln_in_g, ln_in_b, rel_bias, w_in,
              rwkv_mu, rwkv_w_up, rwkv_w0, rwkv_a_up, rwkv_a0, rwkv_g_up, rwkv_k_k, rwkv_k_a, rwkv_r_k,
              rwkv_gn_g, rwkv_gn_b,
              gla_a_up, gla_a_b, gla_norm_g,
              dsa_kv_norm_g, dsa_w_uk, dsa_w_uv,
              mlstm_conv_w, mlstm_conv_b, mlstm_i_b, mlstm_f_b, mlstm_norm_g,
              w_branch, w_out, ln1_g, ln1_b,
              moe_w_grp, moe_b_grp, moe_w_rt, moe_b_rt, moe_w_gate, moe_w_up, moe_w_down, ln2_g, ln2_b):
    B, S, D = x.shape
    topk = min(TOPK_MAX, S // 4)
    h = jnp.concatenate([jnp.broadcast_to(meta.astype(x.dtype), (B, N_META, D)), x], axis=1)
    h = _layer_norm(h, ln_in_g, ln_in_b)
    for l in range(DEPTH):
        p = (h @ w_in[l]).astype(jnp.float32)
        p_a, p_b, p_c, p_d, p_g = _split(p, IN_SPLITS)
        y_a = _rwkv7_mixer(p_a, rwkv_mu[l], rwkv_w_up[l], rwkv_w0[l], rwkv_a_up[l], rwkv_a0[l], rwkv_g_up[l],
                           rwkv_k_k[l], rwkv_k_a[l], rwkv_r_k[l], rwkv_gn_g[l], rwkv_gn_b[l])
        y_b = _gla_mixer(p_b, gla_a_up[l], gla_a_b[l], gla_norm_g[l])
        y_c = _dsa_mixer(p_c, dsa_kv_norm_g[l], dsa_w_uk[l], dsa_w_uv[l], rel_bias, topk)
        y_d = _mlstm_mixer(p_d, mlstm_conv_w[l], mlstm_conv_b[l], mlstm_i_b[l], mlstm_f_b[l], mlstm_norm_g[l])
        gates = jnp.split(jax.nn.sigmoid(p_g), N_BRANCH, axis=-1)
        merged = jnp.zeros_like(h)
        for i, y in enumerate((y_a, y_b, y_c, y_d)):
            merged = merged + gates[i].astype(h.dtype) * (y.astype(h.dtype) @ w_branch[l, i])
        h = _layer_norm(DN_ALPHA * h + merged @ w_out[l], ln1_g[l], ln1_b[l])
        moe_out = _hier_moe(h, moe_w_grp[l], moe_b_grp[l], moe_w_rt[l], moe_b_rt[l],
                            moe_w_gate[l], moe_w_up[l], moe_w_down[l])
        h = _layer_norm(DN_ALPHA * h + moe_out, ln2_g[l], ln2_b[l])
    return h[:, N_META:]
```

```python
import numpy as np
import concourse.bass as bass
import concourse.mybir as mybir
from concourse.bass_utils import run_bass_kernel_spmd
from contextlib import ExitStack

F32 = mybir.dt.float32
BF16 = mybir.dt.bfloat16
ALU = mybir.AluOpType
AF = mybir.ActivationFunctionType
AX = mybir.AxisListType

D = 1024
DEPTH = 2
N_META = 16
DN_ALPHA = (2 * DEPTH) ** 0.25
LN_EPS = 1e-5
NCORES = 8

ENGS = ('pe', 'act', 'dve', 'pool', 'sp')
N_DMA_SEMS = 16
CC_INC = 1


class Prog:
    def __init__(self, nc):
        self.nc = nc
        self.E = {'pe': nc.tensor, 'act': nc.scalar, 'dve': nc.vector, 'pool': nc.gpsimd, 'sp': nc.sync}
        self.sem = {e: nc.alloc_semaphore("s_" + e) for e in ENGS}
        self.cnt = {e: 0 for e in ENGS}
        self.dsem = [nc.alloc_semaphore("d_%d" % i) for i in range(N_DMA_SEMS)]
        self.dcnt = [0] * N_DMA_SEMS
        self.dnext = 0
        self.known = {e: {} for e in ENGS}
        self.last_w = {}
        self.readers = {}
        self.semobj = {}
        for e in ENGS:
            self.semobj['s_' + e] = self.sem[e]
        for i in range(N_DMA_SEMS):
            self.semobj['d_%d' % i] = self.dsem[i]
        self.n_ins = 0
        self.n_wait = 0
        self._uid = 0
        self.stacks = []

    def sb(self, shape, dt=F32, name=None):
        self._uid += 1
        nm = (name or "t") + "_%d" % self._uid
        if self.stacks:
            return self.stacks[-1].enter_context(self.nc.sbuf_tensor(nm, list(shape), dt)).ap()
        return self.nc.alloc_sbuf_tensor(nm, list(shape), dt).ap()

    def push(self):
        self.stacks.append(ExitStack())

    def pop(self):
        self.barrier()
        self.stacks.pop().close()

    def barrier(self):
        for e in ENGS:
            for f in ENGS:
                if f != e and self.cnt[f] > self.known[e].get('s_' + f, 0):
                    self.E[e].wait_ge(self.sem[f], self.cnt[f])
                    self.known[e]['s_' + f] = self.cnt[f]
            for i in range(N_DMA_SEMS):
                sn = 'd_%d' % i
                if self.dcnt[i] > self.known[e].get(sn, 0):
                    self.E[e].wait_ge(self.dsem[i], self.dcnt[i])
                    self.known[e][sn] = self.dcnt[i]

    def ps(self, shape, dt=F32, name=None):
        self._uid += 1
        return self.nc.alloc_psum_tensor(name or ("p%d" % self._uid), list(shape), dt).ap()

    @staticmethod
    def key_of(x):
        if isinstance(x, (str, tuple)):
            return x
        return x.tensor.name

    def _deps(self, eng, reads, writes):
        need = {}

        def add(sn, v, prod_eng):
            if prod_eng == eng and eng == 'pe':
                return
            if need.get(sn, 0) < v:
                need[sn] = v
        for k in reads:
            w = self.last_w.get(k)
            if w is not None:
                add(*w)
        for k in writes:
            w = self.last_w.get(k)
            if w is not None:
                add(*w)
            for (sn, v, pe) in self.readers.get(k, {}).values():
                if pe == eng:
                    continue
                add(sn, v, pe)
        for sn, v in need.items():
            if self.known[eng].get(sn, 0) < v:
                self.E[eng].wait_ge(self.semobj[sn], v)
                self.known[eng][sn] = v
                self.n_wait += 1

    def _record(self, reads, writes, tok):
        for k in reads:
            self.readers.setdefault(k, {})[tok[0]] = tok
        for k in writes:
            self.last_w[k] = tok
            self.readers[k] = {}

    def op(self, eng, fn, r=(), w=()):
        reads = [self.key_of(x) for x in r]
        writes = [self.key_of(x) for x in w]
        self._deps(eng, reads, writes)
        ins = fn(self.E[eng])
        self.cnt[eng] += 1
        ins.then_inc(self.sem[eng], 1)
        tok = ('s_' + eng, self.cnt[eng], eng)
        self._record(reads, writes, tok)
        self.n_ins += 1
        return ins

    def dma(self, eng, out, in_, r=None, w=None, **kw):
        reads = [self.key_of(x) for x in (r if r is not None else [in_])]
        writes = [self.key_of(x) for x in (w if w is not None else [out])]
        self._deps(eng, reads, writes)
        i = self.dnext
        self.dnext = (self.dnext + 1) % N_DMA_SEMS
        sn = 'd_%d' % i
        if self.known[eng].get(sn, 0) < self.dcnt[i]:
            self.E[eng].wait_ge(self.dsem[i], self.dcnt[i])
            self.known[eng][sn] = self.dcnt[i]
        ins = self.E[eng].dma_start(out=out, in_=in_, **kw)
        self.dcnt[i] += 16
        ins.then_inc(self.dsem[i], 16)
        tok = (sn, self.dcnt[i], 'dma')
        self._record(reads, writes, tok)
        self.n_ins += 1
        return ins

    def coll(self, kind, in_, out, groups):
        eng = 'pool'
        reads = [self.key_of(in_)]
        writes = [self.key_of(out)]
        self._deps(eng, reads, writes)
        i = self.dnext
        self.dnext = (self.dnext + 1) % N_DMA_SEMS
        sn = 'd_%d' % i
        if self.known[eng].get(sn, 0) < self.dcnt[i]:
            self.E[eng].wait_ge(self.dsem[i], self.dcnt[i])
            self.known[eng][sn] = self.dcnt[i]
        ins = self.nc.gpsimd.collective_compute(kind, ALU.bypass, replica_groups=groups, ins=[in_.opt()], outs=[out.opt()])
        self.dcnt[i] += CC_INC
        ins.then_inc(self.dsem[i], CC_INC)
        tok = (sn, self.dcnt[i], 'dma')
        self._record(reads, writes, tok)
        self.n_ins += 1
        return ins

    def finish(self, eng='sp'):
        for i in range(N_DMA_SEMS):
            if self.dcnt[i] > 0:
                self.E[eng].wait_ge(self.dsem[i], self.dcnt[i])
        for e in ENGS:
            if self.cnt[e] > 0 and e != eng:
                self.E[eng].wait_ge(self.sem[e], self.cnt[e])

    def mm(self, out, lhsT, rhs, start=True, stop=True, **kw):
        return self.op('pe', lambda e: e.matmul(out, lhsT, rhs, start=start, stop=stop, **kw),
                       r=[lhsT, rhs], w=[out])

    def tr(self, out, in_, ident):
        return self.op('pe', lambda e: e.transpose(out, in_, ident), r=[in_, ident], w=[out])

    def act(self, out, in_, func, bias=None, scale=1.0, accum_out=None, eng='act'):
        r = [in_]
        w = [out]
        kw = {}
        if bias is not None:
            kw['bias'] = bias
            if not isinstance(bias, (int, float)):
                r.append(bias)
        if not isinstance(scale, (int, float)):
            r.append(scale)
        if accum_out is not None:
            kw['accum_out'] = accum_out
            w.append(accum_out)
        return self.op('act', lambda e: e.activation(out, in_, func, scale=scale, **kw), r=r, w=w)

    def tt(self, eng, out, a, b, op):
        return self.op(eng, lambda e: e.tensor_tensor(out, a, b, op), r=[a, b], w=[out])

    def ts(self, eng, out, a, s1, s2, op0, op1=None, accum_out=None):
        r = [a] + [s for s in (s1, s2) if s is not None and not isinstance(s, (int, float))]
        w = [out] + ([accum_out] if accum_out is not None else [])
        kw = {}
        if accum_out is not None:
            kw['accum_out'] = accum_out
        if op1 is None:
            return self.op(eng, lambda e: e.tensor_scalar(out, a, s1, None, op0, **kw), r=r, w=w)
        return self.op(eng, lambda e: e.tensor_scalar(out, a, s1, s2, op0, op1, **kw), r=r, w=w)

    def stt(self, out, a, s, b, op0, op1, accum_out=None):
        r = [a, b] + ([s] if not isinstance(s, (int, float)) else [])
        w = [out] + ([accum_out] if accum_out is not None else [])
        kw = {}
        if accum_out is not None:
            kw['accum_out'] = accum_out
        return self.op('dve', lambda e: e.scalar_tensor_tensor(out, a, s, b, op0, op1, **kw), r=r, w=w)

    def copy(self, eng, out, in_):
        if eng == 'act':
            return self.op('act', lambda e: e.copy(out, in_), r=[in_], w=[out])
        return self.op(eng, lambda e: e.tensor_copy(out, in_), r=[in_], w=[out])

    def memset(self, eng, out, v):
        return self.op(eng, lambda e: e.memset(out, v), w=[out])


class Ctx:
    def __init__(self, nc, p, PS, prefix="", over=None):
        self.nc, self.p, self.PS, self.prefix, self.over = nc, p, PS, prefix, dict(over or {})

    def inp(self, n, s):
        if n in self.over:
            return self.over[n]
        return self.nc.dram_tensor(self.prefix + n, list(s), F32, kind="ExternalInput").ap()

    def out(self, n, s):
        if n in self.over:
            return self.over[n]
        return self.nc.dram_tensor(self.prefix + n, list(s), F32, kind="ExternalOutput").ap()

    def scratch(self, n, s, dt=F32):
        return self.nc.dram_tensor(self.prefix + n, list(s), dt, kind="Internal").ap()


def new_prog():
    nc = bass.Bass("TRN2", target_bir_lowering=False)
    p = Prog(nc)
    PS = [p.ps([128, 512], name="bank%d" % i) for i in range(8)]
    return nc, p, PS


def standalone(emit, *a, **k):
    nc, p, PS = new_prog()
    emit(Ctx(nc, p, PS), *a, **k)
    p.finish()
    return nc


def emit_transposed(p, PS, src, dstT_d, cols, ident, tmpT):
    for c in range(8):
        pt = PS[4 + c // 4]
        p.tr(pt[:, (c % 4) * 128:(c % 4 + 1) * 128], src[:, c * 128:(c + 1) * 128], ident)
        if c % 4 == 3:
            p.copy('act', tmpT[:, c - 3:c + 1, :], pt.rearrange("p (a n) -> p a n", a=4))
    p.dma('sp', dstT_d.rearrange("(c f) n -> f c n", f=128)[:, :, cols], tmpT)


def emit_ln(p, xt, out, G, Bt, tmp, eps):
    st, mv, rs = tmp
    for c in range(2):
        p.op('dve', lambda e: e.bn_stats(st[:, c * 6:(c + 1) * 6], xt[:, c * 512:(c + 1) * 512]), r=[xt], w=[st])
    p.op('dve', lambda e: e.bn_aggr(mv, st.rearrange("p (c s) -> p c s", s=6)), r=[st], w=[mv])
    p.act(rs, mv[:, 1:2], AF.Sqrt, bias=eps)
    p.op('dve', lambda e: e.reciprocal(rs, rs), r=[rs], w=[rs])
    p.ts('dve', out, xt, mv[:, 0:1], rs[:, 0:1], ALU.subtract, ALU.mult)
    p.tt('pool', out, out, G, ALU.mult)
    p.tt('pool', out, out, Bt, ALU.add)


def load_cast(p, dst, src, stage, chunk_cols, engs=('pool', 'act')):
    a, n = dst.shape[1], dst.shape[2]
    chunk_cols = min(chunk_cols, stage[0].shape[1])
    k = 0
    for i in range(a):
        for c0 in range(0, n, chunk_cols):
            c1 = min(n, c0 + chunk_cols)
            stg = stage[k % len(stage)]
            p.dma('sp', stg[:, 0:c1 - c0], src[:, i, c0:c1])
            p.copy(engs[k % len(engs)], dst[:, i, c0:c1], stg[:, 0:c1 - c0])
            k += 1


def emit_ln_in(cx, NTL):
    nc, p, PS = cx.nc, cx.p, cx.PS
    x = cx.inp("x", [NTL * 128, D]); g = cx.inp("g", [D]); b = cx.inp("b", [D])
    y = cx.out("y", [NTL * 128, D])
    hT_out = cx.over.get("hT_out")
    p.push()
    G = p.sb([128, D]); Bt = p.sb([128, D]); eps = p.sb([128, 1])
    p.memset('dve', eps, LN_EPS)
    p.dma('sp', G, g.partition_broadcast(128))
    p.dma('sp', Bt, b.partition_broadcast(128))
    xts = [p.sb([128, D]) for _ in range(2)]
    ots = [p.sb([128, D]) for _ in range(2)]
    tmp = (p.sb([128, 12]), p.sb([128, 2]), p.sb([128, 1]))
    if hT_out is not None:
        ident = p.sb([128, 128]); p.dma('sp', ident, cx.inp("ident", [128, 128]))
        tmpT = p.sb([128, 8, 128])
    for t in range(NTL):
        xt = xts[t % 2]; ot = ots[t % 2]
        p.dma('sp', xt, x[t * 128:(t + 1) * 128, :])
        emit_ln(p, xt, ot, G, Bt, tmp, eps)
        p.dma('sp', y[t * 128:(t + 1) * 128, :], ot)
        if hT_out is not None:
            emit_transposed(p, PS, ot, hT_out, slice(t * 128, (t + 1) * 128), ident, tmpT)
    p.pop()


def build_ln_in(NTL):
    return standalone(emit_ln_in, NTL)


def emit_post(cx, NTL, GT=8):
    nc, p, PS = cx.nc, cx.p, cx.PS
    NTOK = NTL * 128
    di = cx.inp
    h_d = di("h", [NTOK, D]); hT_d = di("hT", [D, NTOK])
    fused = "G_abd" in cx.over
    if fused:
        G_abd, G_c, rk_d = cx.over["G_abd"], cx.over["G_c"], cx.over["rk"]
        PWy, HALF, NSLOT = cx.over["PWy"], cx.over["HALF"], cx.over["NSLOT"]
    else:
        yT_d = di("yT", [D, NTOK])
    hT_out = cx.over.get("hT_out")
    wg_d = di("wg", [D, 4 * D]); wb_d = di("wb", [D, D]); wo_d = di("wo", [D, D])
    ln1g = di("ln1g", [D]); ln1b = di("ln1b", [D]); ln2g = di("ln2g", [D]); ln2b = di("ln2b", [D])
    wr_d = di("wr", [D, 20]); br_d = di("br", [20])
    mg_d = di("mg", [16, D, 256]); mu_d = di("mu", [16, D, 256]); md_d = di("md", [16, 256, D])
    id_d = di("ident", [128, 128])
    out_d = cx.out("out", [NTOK, D])
    h1_d = cx.scratch("h1s", [NTOK, D], F32)
    xT_d = cx.scratch("xTs", [128, 8, NTOK], BF16)
    p.push()
    ident = p.sb([128, 128]); p.dma('sp', ident, id_d)
    eps = p.sb([128, 1]); p.memset('dve', eps, LN_EPS)
    G1 = p.sb([128, D]); B1 = p.sb([128, D]); G2 = p.sb([128, D]); B2 = p.sb([128, D])
    for tdst, src in ((G1, ln1g), (B1, ln1b), (G2, ln2g), (B2, ln2b)):
        p.dma('sp', tdst, src.partition_broadcast(128))
    Wr = p.sb([128, 8, 20]); p.dma('sp', Wr, wr_d.rearrange("(c f) n -> f c n", f=128))
    Br = p.sb([128, 20]); p.dma('sp', Br, br_d.partition_broadcast(128))
    gate_all = p.sb([128, NTL, 16])
    lntmp = (p.sb([128, 12]), p.sb([128, 2]), p.sb([128, 1]))
    stage = [p.sb([128, 2048], name="stg%d" % i) for i in range(2)]

    p.push()
    Wg = p.sb([128, 8, 4 * D], BF16, name="Wg")
    Wb = p.sb([128, 8, D], BF16, name="Wb")
    Wo = p.sb([128, 8, D], BF16, name="Wo")
    load_cast(p, Wg, wg_d.rearrange("(c f) n -> f c n", f=128), stage, 2048)
    load_cast(p, Wb, wb_d.rearrange("(c f) n -> f c n", f=128), stage, 2048)
    load_cast(p, Wo, wo_d.rearrange("(c f) n -> f c n", f=128), stage, 2048)
    hTf = p.sb([128, 8, 128]); hTb = p.sb([128, 8, 128], BF16)
    yTf = p.sb([128, 8, 128]); yTb = p.sb([128, 8, 128], BF16)
    if fused:
        rk = p.sb([128, 2], name="rk"); p.dma('sp', rk, rk_d)
        ycand = [p.sb([128, D], name="ycand%d" % i) for i in range(2)]
        ytile = p.sb([128, D], name="ytile")
    ht = p.sb([128, D]); gsb = [p.sb([128, 512]) for _ in range(2)]
    merged = p.sb([128, D]); tmpm = p.sb([128, 512])
    mTb = p.sb([128, 8, 128], BF16)
    pre = p.sb([128, D]); h1 = p.sb([128, D])
    x32 = p.sb([128, 8, 128]); xb = p.sb([128, 8, 128], BF16)
    lg = p.sb([128, 20]); sm = p.sb([128, 16], name="smallr")
    gm4 = p.sb([128, 4]); ge4 = p.sb([128, 4]); em = p.sb([128, 16]); elm = p.sb([128, 16])
    top8 = p.sb([128, 8]); g0 = p.sb([128, 16]); g1 = p.sb([128, 16])
    for t in range(NTL):
        ts_ = slice(t * 128, (t + 1) * 128)
        p.dma('sp', hTf, hT_d.rearrange("(c f) n -> f c n", f=128)[:, :, ts_])
        p.dma('sp', ht, h_d[ts_, :])
        p.copy('pool', hTb, hTf)
        if not fused:
            p.dma('sp', yTf, yT_d.rearrange("(c f) n -> f c n", f=128)[:, :, ts_])
            p.copy('pool', yTb, yTf)
        else:
            for rc in range(2):
                r0 = 48 + HALF * rc + t * 128
                yc = ycand[rc].rearrange("p (m q c) -> p m q c", m=4, q=2)
                for q in range(2):
                    src = G_abd.rows(q, r0, 128).rearrange("p (m c) -> p m c", m=3)
                    p.dma('sp', yc[:, 0:2, q, :], src[:, 0:2, :])
                    p.dma('sp', yc[:, 3, q, :], src[:, 2, :])
                slot = (HALF // 256) * rc + t // 2
                if slot < NSLOT:
                    p.dma('sp', ycand[rc][:, 512:768], G_c.rows(t % 2, slot * 128, 128))
                else:
                    p.memset('pool', ycand[rc][:, 512:768], 0.0)
            p.ts('dve', ytile, ycand[0], rk[:, 0:1], None, ALU.mult)
            p.stt(ytile, ycand[1], rk[:, 1:2], ytile, ALU.mult, ALU.add)
            for c in range(8):
                pt = PS[4 + c // 4]
                p.tr(pt[:, (c % 4) * 128:(c % 4 + 1) * 128], ytile[:, c * 128:(c + 1) * 128], ident)
                if c % 4 == 3:
                    p.copy('act', yTb[:, c - 3:c + 1, :], pt.rearrange("p (a n) -> p a n", a=4))
        for i in range(4):
            for hf in range(2):
                pg = PS[hf]; py = PS[2 + hf]
                for c in range(8):
                    p.mm(pg, hTb[:, c, :], Wg[:, c, i * D + hf * 512: i * D + hf * 512 + 512], start=(c == 0), stop=(c == 7))
                p.act(gsb[hf], pg, AF.Sigmoid)
                for c in range(2):
                    p.mm(py, yTb[:, 2 * i + c, :], Wb[:, 2 * i + c, hf * 512:(hf + 1) * 512], start=(c == 0), stop=(c == 1))
                if i == 0:
                    p.tt('dve', merged[:, hf * 512:(hf + 1) * 512], gsb[hf], py, ALU.mult)
                else:
                    p.tt('dve', tmpm, gsb[hf], py, ALU.mult)
                    p.tt('pool', merged[:, hf * 512:(hf + 1) * 512], merged[:, hf * 512:(hf + 1) * 512], tmpm, ALU.add)
        for c in range(8):
            pt = PS[4 + c // 4]
            p.tr(pt[:, (c % 4) * 128:(c % 4 + 1) * 128], merged[:, c * 128:(c + 1) * 128], ident)
            if c % 4 == 3:
                p.copy('act', mTb[:, c - 3:c + 1, :], pt.rearrange("p (a n) -> p a n", a=4))
        for hf in range(2):
            po = PS[6 + hf]
            for c in range(8):
                p.mm(po, mTb[:, c, :], Wo[:, c, hf * 512:(hf + 1) * 512], start=(c == 0), stop=(c == 7))
            p.stt(pre[:, hf * 512:(hf + 1) * 512], ht[:, hf * 512:(hf + 1) * 512], DN_ALPHA, po, ALU.mult, ALU.add)
        emit_ln(p, pre, h1, G1, B1, lntmp, eps)
        p.dma('sp', h1_d[ts_, :], h1)
        for c in range(8):
            pt = PS[4 + c // 4]
            p.tr(pt[:, (c % 4) * 128:(c % 4 + 1) * 128], h1[:, c * 128:(c + 1) * 128], ident)
            if c % 4 == 3:
                p.copy('act', x32[:, c - 3:c + 1, :], pt.rearrange("p (a n) -> p a n", a=4))
        p.copy('pool', xb, x32)
        p.dma('sp', xT_d[:, :, ts_], xb)
        pr = PS[0]
        for c in range(8):
            p.mm(pr[:, 0:20], x32[:, c, :], Wr[:, c, :], start=(c == 0), stop=(c == 7))
        p.tt('dve', lg, pr[:, 0:20], Br, ALU.add)
        p.op('dve', lambda e: e.reduce_max(sm[:, 0:1], lg[:, 0:4], AX.X), r=[lg], w=[sm])
        p.ts('dve', sm[:, 1:2], sm[:, 0:1], -1.0, None, ALU.mult)
        p.act(ge4, lg[:, 0:4], AF.Exp, bias=sm[:, 1:2], accum_out=sm[:, 2:3])
        p.op('dve', lambda e: e.reciprocal(sm[:, 3:4], sm[:, 2:3]), r=[sm], w=[sm])
        p.ts('dve', gm4, lg[:, 0:4], sm[:, 0:1], None, ALU.is_ge)
        p.copy('dve', em.rearrange("p (g e) -> p g e", e=4), gm4.unsqueeze(2).to_broadcast([128, 4, 4]))
        p.ts('dve', em, em, 1e30, -1e30, ALU.mult, ALU.add)
        p.tt('dve', elm, lg[:, 4:20], em, ALU.add)
        p.op('dve', lambda e: e.max(top8, elm), r=[elm], w=[top8])
        p.tt('dve', sm[:, 4:5], top8[:, 1:2], top8[:, 0:1], ALU.subtract)
        p.act(sm[:, 5:6], sm[:, 4:5], AF.Sigmoid)
        p.ts('dve', sm[:, 6:7], sm[:, 5:6], -1.0, 1.0, ALU.mult, ALU.add)
        p.tt('dve', sm[:, 7:8], sm[:, 6:7], sm[:, 3:4], ALU.mult)
        p.tt('dve', sm[:, 8:9], sm[:, 5:6], sm[:, 3:4], ALU.mult)
        p.ts('dve', g0, elm, top8[:, 0:1], sm[:, 7:8], ALU.is_equal, ALU.mult)
        p.ts('dve', g1, elm, top8[:, 1:2], sm[:, 8:9], ALU.is_equal, ALU.mult)
        p.tt('dve', gate_all[:, t, :], g0, g1, ALU.add)

    p.pop()
    p.push()
    We_g = [p.sb([128, 8, 256], BF16, name="Weg%d" % i) for i in range(2)]
    We_u = [p.sb([128, 8, 256], BF16, name="Weu%d" % i) for i in range(2)]
    We_d = [p.sb([128, 2, D], BF16, name="Wed%d" % i) for i in range(2)]
    xg = p.sb([128, 8, GT * 128], BF16, name="xg")
    yacc = p.sb([128, GT, D], name="yacc")
    sg = [p.sb([128, 512], name="sg%d" % i) for i in range(2)]
    hid = [p.sb([128, 512], BF16, name="hid%d" % i) for i in range(2)]
    h1t = p.sb([128, D], name="h1t"); pre2 = p.sb([128, D], name="pre2"); o2 = p.sb([128, D], name="o2")
    tmpT2 = p.sb([128, 8, 128], name="tmpT2")
    k = 0
    for g0_ in range(0, NTL, GT):
        ntg = min(GT, NTL - g0_)
        p.dma('sp', xg[:, :, 0:ntg * 128], xT_d[:, :, g0_ * 128:(g0_ + ntg) * 128])
        for e_ in range(16):
            wgt, wut, wdt = We_g[k % 2], We_u[k % 2], We_d[k % 2]
            k += 1
            load_cast(p, wgt, mg_d[e_].rearrange("(c f) n -> f c n", f=128), stage, 2048)
            load_cast(p, wut, mu_d[e_].rearrange("(c f) n -> f c n", f=128), stage, 2048)
            load_cast(p, wdt, md_d[e_].rearrange("(c f) n -> f c n", f=128), stage, 2048)
            for tb in range(0, ntg, 4):
                nb = min(4, ntg - tb)
                ncol = nb * 128
                cs = slice(tb * 128, tb * 128 + ncol)
                for j in range(2):
                    pa = PS[j * 2]; pb = PS[j * 2 + 1]
                    for c in range(8):
                        p.mm(pa[:, 0:ncol], wgt[:, c, j * 128:(j + 1) * 128], xg[:, c, cs], start=(c == 0), stop=(c == 7))
                    for c in range(8):
                        p.mm(pb[:, 0:ncol], wut[:, c, j * 128:(j + 1) * 128], xg[:, c, cs], start=(c == 0), stop=(c == 7))
                    p.act(sg[j][:, 0:ncol], pa[:, 0:ncol], AF.Silu)
                    p.tt('dve', hid[j][:, 0:ncol], sg[j][:, 0:ncol], pb[:, 0:ncol], ALU.mult)
                for tt_ in range(nb):
                    tile_i = tb + tt_
                    for hf in range(2):
                        pd = PS[4 + (tt_ * 2 + hf) % 4]
                        for j in range(2):
                            p.mm(pd, hid[j][:, tt_ * 128:(tt_ + 1) * 128], wdt[:, j, hf * 512:(hf + 1) * 512], start=(j == 0), stop=(j == 1))
                        ya = yacc[:, tile_i, hf * 512:(hf + 1) * 512]
                        gcol = gate_all[:, g0_ + tile_i, e_:e_ + 1]
                        if e_ == 0:
                            p.ts('dve', ya, pd, gcol, None, ALU.mult)
                        else:
                            p.stt(ya, pd, gcol, ya, ALU.mult, ALU.add)
        for tt_ in range(ntg):
            ts_ = slice((g0_ + tt_) * 128, (g0_ + tt_ + 1) * 128)
            p.dma('sp', h1t, h1_d[ts_, :])
            p.stt(pre2, h1t, DN_ALPHA, yacc[:, tt_, :], ALU.mult, ALU.add)
            emit_ln(p, pre2, o2, G2, B2, lntmp, eps)
            p.dma('sp', out_d[ts_, :], o2)
            if hT_out is not None:
                emit_transposed(p, PS, o2, hT_out, ts_, ident, tmpT2)
    p.pop()
    p.pop()


def build_post(NTL, GT=8):
    return standalone(emit_post, NTL, GT)


def load_w_bf16(p, src_d, ncols, stage, name):
    W = p.sb([128, 8, ncols], BF16, name=name)
    load_cast(p, W, src_d.rearrange("(c f) n -> f c n", f=128), stage, 2048)
    return W


def load_hblock(p, hT_d, pos0, npos, hTf, hTb):
    p.dma('sp', hTf[:, :, 0:npos], hT_d.rearrange("(c f) n -> f c n", f=128)[:, :, pos0:pos0 + npos])
    p.copy('pool', hTb[:, :, 0:npos], hTf[:, :, 0:npos])


def proj_fm(p, ps_out, W, c0, ncols, hTb, n0, npos):
    for c in range(8):
        p.mm(ps_out, W[:, c, c0:c0 + ncols], hTb[:, c, n0:n0 + npos], start=(c == 0), stop=(c == 7))


def proj_tm(p, ps_out, W, c0, ncols, hTb, n0, npos):
    for c in range(8):
        p.mm(ps_out, hTb[:, c, n0:n0 + npos], W[:, c, c0:c0 + ncols], start=(c == 0), stop=(c == 7))


def emit_gla(cx, NCH, stop_at=99):
    nc, p, PS = cx.nc, cx.p, cx.PS
    P_ = NCH * 64
    BLK = 512
    di = cx.inp
    hT_d = di("hT", [D, P_])
    wqk_d = di("wqk", [D, 128]); wvo_d = di("wvo", [D, 256]); wa_d = di("wa", [D, 16])
    aup_d = di("aup", [16, 64]); ab_d = di("ab", [64, 1]); ng_d = di("ng", [128])
    cm_d = di("cmask", [P_]); mu_d = di("maskU2", [64, 128]); id_d = di("ident", [128, 128])
    y_d = cx.out("y", [P_, 128])
    p.push()
    stage = [p.sb([128, 2048], name="stg%d" % i) for i in range(2)]
    Wqk = load_w_bf16(p, wqk_d, 128, stage, "Wqk")
    Wvo = load_w_bf16(p, wvo_d, 256, stage, "Wvo")
    Wa = load_w_bf16(p, wa_d, 16, stage, "Wa")
    aup = p.sb([16, 64]); p.dma('sp', aup, aup_d)
    ab = [p.sb([32, 1]) for _ in range(2)]
    for h in range(2):
        p.dma('sp', ab[h], ab_d[h * 32:(h + 1) * 32, :])
    ng = p.sb([64, 128]); p.dma('sp', ng, ng_d.partition_broadcast(64))
    maskU = p.sb([64, 128]); p.dma('sp', maskU, mu_d)
    ident = p.sb([128, 128]); p.dma('sp', ident, id_d)
    eps6 = p.sb([64, 1]); p.memset('dve', eps6, 1e-6)
    S = [p.sb([32, 64], name="S%d" % h) for h in range(2)]
    for h in range(2):
        p.memset('dve', S[h], 0.0)
    hTf = p.sb([128, 8, BLK]); hTb = p.sb([128, 8, BLK], BF16)
    cm = p.sb([32, BLK]); xa = p.sb([16, BLK])
    mk = lambda nm: [p.sb([32, BLK], name=nm + str(h)) for h in range(2)]
    qT, kT, la, bT, eb, enb, ekb, qg, kg, ku = (mk(n) for n in ("qT", "kT", "la", "bT", "eb", "enb", "ekb", "qg", "kg", "ku"))
    dec = [p.sb([32, BLK // 64], name="dec%d" % h) for h in range(2)]
    vo = p.sb([64, 256]); sgo = p.sb([64, 128]); kut = p.sb([64, 64]); attm = p.sb([64, 128])
    o_sb = p.sb([64, 128]); sq = p.sb([64, 128]); ms = p.sb([64, 2]); yt = p.sb([64, 128])
    for b0 in range(0, P_, BLK):
        nb = min(BLK, P_ - b0)
        nch = nb // 64
        load_hblock(p, hT_d, b0, nb, hTf, hTb)
        p.dma('sp', cm[:, 0:nb], cm_d[b0:b0 + nb].partition_broadcast(32))
        proj_fm(p, PS[2][0:16, 0:nb], Wa, 0, 16, hTb, 0, nb)
        p.copy('act', xa[:, 0:nb], PS[2][0:16, 0:nb])
        for h in range(2):
            sl = (slice(None), slice(0, nb))
            proj_fm(p, PS[0][0:32, 0:nb], Wqk, h * 32, 32, hTb, 0, nb)
            p.op('act', lambda e: e.mul(qT[h][sl], PS[0][0:32, 0:nb], 32 ** -0.5), r=[PS[0]], w=[qT[h]])
            proj_fm(p, PS[1][0:32, 0:nb], Wqk, 64 + h * 32, 32, hTb, 0, nb)
            p.copy('act', kT[h][sl], PS[1][0:32, 0:nb])
            p.mm(PS[3][0:32, 0:nb], aup[:, h * 32:(h + 1) * 32], xa[:, 0:nb])
            p.act(la[h][sl], PS[3][0:32, 0:nb], AF.Sigmoid, bias=ab[h])
            p.act(la[h][sl], la[h][sl], AF.Ln)
            p.ts('pool', la[h][sl], la[h][sl], 1.0 / 16.0, None, ALU.mult)
            p.op('dve', lambda e: e.tensor_tensor_scan(bT[h][sl], cm[:, 0:nb], la[h][sl], 0.0, ALU.mult, ALU.add),
                 r=[cm, la[h]], w=[bT[h]])
            p.act(eb[h][sl], bT[h][sl], AF.Exp)
            p.act(enb[h][sl], bT[h][sl], AF.Exp, scale=-1.0)
            b3 = bT[h][sl].rearrange("p (c l) -> p c l", l=64)
            p.tt('dve', ekb[h][sl].rearrange("p (c l) -> p c l", l=64), b3[:, :, 63:64].to_broadcast([32, nch, 64]), b3, ALU.subtract)
            p.act(ekb[h][sl], ekb[h][sl], AF.Exp)
            p.act(dec[h][:, 0:nch], b3[:, :, 63], AF.Exp)
            p.tt('dve', qg[h][sl], qT[h][sl], eb[h][sl], ALU.mult)
            p.tt('pool', kg[h][sl], kT[h][sl], enb[h][sl], ALU.mult)
            p.tt('pool', ku[h][sl], kT[h][sl], ekb[h][sl], ALU.mult)
        for ci in range(nch):
            cs = slice(ci * 64, ci * 64 + 64)
            proj_tm(p, PS[4][0:64, 0:256], Wvo, 0, 256, hTb, ci * 64, 64)
            p.copy('act', vo[:, 0:128], PS[4][0:64, 0:128])
            p.act(sgo, PS[4][0:64, 128:256], AF.Silu)
            for h in range(2):
                p.tr(PS[5][0:64, h * 32:h * 32 + 32], ku[h][:, cs], ident[0:32, 0:32])
            p.copy('act', kut, PS[5][0:64, 0:64])
            for h in range(2):
                p.mm(PS[6][0:64, h * 64:h * 64 + 64], kg[h][:, cs], qg[h][:, cs])
            p.tt('dve', attm, PS[6][0:64, 0:128], maskU, ALU.mult)
            for h in range(2):
                p.mm(PS[7][0:64, h * 64:h * 64 + 64], attm[:, h * 64:h * 64 + 64], vo[:, h * 64:h * 64 + 64], start=True, stop=False)
                p.mm(PS[7][0:64, h * 64:h * 64 + 64], qg[h][:, cs], S[h], start=False, stop=True)
            for h in range(2):
                p.mm(PS[5][0:32, 128 + h * 64:128 + h * 64 + 64], kut[:, h * 32:h * 32 + 32], vo[:, h * 64:h * 64 + 64])
                p.stt(S[h], S[h], dec[h][:, ci:ci + 1], PS[5][0:32, 128 + h * 64:128 + h * 64 + 64], ALU.mult, ALU.add)
            p.copy('act', o_sb, PS[7][0:64, 0:128])
            p.tt('dve', sq, o_sb, o_sb, ALU.mult)
            p.op('dve', lambda e: e.reduce_sum(ms, sq.rearrange("p (h d) -> p h d", d=64), AX.X), r=[sq], w=[ms])
            p.act(ms, ms, AF.Sqrt, bias=eps6, scale=1.0 / 64.0)
            p.op('dve', lambda e: e.reciprocal(ms, ms), r=[ms], w=[ms])
            p.tt('dve', yt.rearrange("p (h d) -> p h d", d=64), o_sb.rearrange("p (h d) -> p h d", d=64),
                 ms.unsqueeze(2).to_broadcast([64, 2, 64]), ALU.mult)
            p.tt('pool', yt, yt, ng, ALU.mult)
            p.tt('pool', yt, yt, sgo, ALU.mult)
            p.dma('sp', y_d[b0 + ci * 64:b0 + ci * 64 + 64, :], yt)
    p.pop()


def build_gla(NCH, stop_at=99):
    return standalone(emit_gla, NCH, stop_at)


def emit_mlstm(cx, NCH):
    nc, p, PS = cx.nc, cx.p, cx.PS
    P_ = NCH * 64
    BLK = 512
    di = cx.inp
    hT_d = di("hT", [D, P_])
    wq_d = di("wq", [D, 128]); wk_d = di("wk", [D, 128]); wvo_d = di("wvo", [D, 256])
    wi_d = di("wi", [D, 128]); wf_d = di("wf", [D, 128])
    cwq_d = di("cwq", [128, 4]); cwk_d = di("cwk", [128, 4]); cbq_d = di("cbq", [128, 1]); cbk_d = di("cbk", [128, 1])
    ib_d = di("ib", [128, 1]); fb_d = di("fb", [128, 1]); ng_d = di("ng", [128])
    cm_d = di("cmask", [P_]); pm_d = di("pm01", [P_]); pn_d = di("pmneg", [P_])
    ml_d = di("maskL", [64, 64]); id_d = di("ident", [128, 128])
    y_d = cx.out("y", [P_, 128])
    p.push()
    stage = [p.sb([128, 2048], name="stg%d" % i) for i in range(2)]
    Wq = load_w_bf16(p, wq_d, 128, stage, "Wq"); Wk = load_w_bf16(p, wk_d, 128, stage, "Wk")
    Wvo = load_w_bf16(p, wvo_d, 256, stage, "Wvo")
    Wi = load_w_bf16(p, wi_d, 128, stage, "Wi"); Wf = load_w_bf16(p, wf_d, 128, stage, "Wf")
    def ld(src, shape, nm):
        t = p.sb(shape, name=nm); p.dma('sp', t, src); return t
    cw = {}
    for h in range(2):
        hs = slice(h * 64, h * 64 + 64)
        cw['q', h] = (ld(cwq_d[hs, :], [64, 4], "cwq%d" % h), ld(cbq_d[hs, :], [64, 1], "cbq%d" % h))
        cw['k', h] = (ld(cwk_d[hs, :], [64, 4], "cwk%d" % h), ld(cbk_d[hs, :], [64, 1], "cbk%d" % h))
    ib = [ld(ib_d[h * 64:h * 64 + 64, :], [64, 1], "ib%d" % h) for h in range(2)]
    fb = [ld(fb_d[h * 64:h * 64 + 64, :], [64, 1], "fb%d" % h) for h in range(2)]
    ng = p.sb([64, 128]); p.dma('sp', ng, ng_d.partition_broadcast(64))
    maskL = ld(ml_d, [64, 64], "maskL"); ident = ld(id_d, [128, 128], "ident")
    eps5 = p.sb([64, 1]); p.memset('dve', eps5, 1e-5)
    Cst = [p.sb([64, 65], name="C%d" % h) for h in range(2)]
    mst = [p.sb([64, 1], name="m%d" % h) for h in range(2)]
    for h in range(2):
        p.memset('dve', Cst[h], 0.0); p.memset('dve', mst[h], 0.0)
    hTf = p.sb([128, 8, BLK]); hTb = p.sb([128, 8, BLK], BF16)
    cm = p.sb([64, BLK]); pm = p.sb([64, BLK]); pn = p.sb([64, BLK])
    mk = lambda nm, w=BLK: [p.sb([64, w], name=nm + str(h)) for h in range(2)]
    qpre = mk("qpre", BLK + 3); kpre = mk("kpre", BLK + 3)
    for h in range(2):
        p.memset('dve', qpre[h], 0.0); p.memset('dve', kpre[h], 0.0)
    acc = mk("acc"); qT = mk("qT"); kT = mk("kT"); liR = mk("liR"); lfR = mk("lfR"); bR = mk("bR"); gR = mk("gR")
    vo1 = p.sb([64, 2, 65]); sgo = p.sb([64, 128]); hh = p.sb([64, 128]); yt = p.sb([64, 128])
    for h in range(2):
        p.memset('dve', vo1[:, h, 64:65], 1.0)
    bcol = p.sb([64, 1]); dl = p.sb([64, 64]); sm = p.sb([64, 12], name="msm"); sw = p.sb([64, 64]); swT = p.sb([64, 64])
    t2 = p.sb([64, 65]); nd = p.sb([64, 65]); wl = p.sb([64, 64]); kw = p.sb([64, 64]); kwt = p.sb([64, 64]); cl = p.sb([64, 65])
    bst = p.sb([64, 6]); bmv = p.sb([64, 2])
    for b0 in range(0, P_, BLK):
        nb = min(BLK, P_ - b0)
        nch = nb // 64
        sl = (slice(None), slice(0, nb))
        load_hblock(p, hT_d, b0, nb, hTf, hTb)
        for tdst, src in ((cm, cm_d), (pm, pm_d), (pn, pn_d)):
            p.dma('sp', tdst[:, 0:nb], src[b0:b0 + nb].partition_broadcast(64))
        for h in range(2):
            for (W, pre, outT, key, scl) in ((Wq, qpre[h], qT[h], 'q', 1.0), (Wk, kpre[h], kT[h], 'k', 0.125)):
                proj_fm(p, PS[0][0:64, 0:nb], W, h * 64, 64, hTb, 0, nb)
                p.copy('act', pre[:, 3:3 + nb], PS[0][0:64, 0:nb])
                cwt, cbt = cw[key, h]
                p.ts('dve', acc[h][sl], pre[:, 3:3 + nb], cwt[:, 3:4], None, ALU.mult)
                for j in range(3):
                    p.stt(acc[h][sl], pre[:, j:j + nb], cwt[:, j:j + 1], acc[h][sl], ALU.mult, ALU.add)
                p.act(outT[sl], acc[h][sl], AF.Silu, bias=cbt)
                if scl != 1.0:
                    p.ts('pool', outT[sl], outT[sl], scl, None, ALU.mult)
                p.copy('pool', pre[:, 0:3], pre[:, nb:nb + 3])
            proj_fm(p, PS[1][0:64, 0:nb], Wi, h * 64, 64, hTb, 0, nb)
            p.stt(liR[h][sl], PS[1][0:64, 0:nb], ib[h], pn[:, 0:nb], ALU.add, ALU.add)
            proj_fm(p, PS[2][0:64, 0:nb], Wf, h * 64, 64, hTb, 0, nb)
            p.act(lfR[h][sl], PS[2][0:64, 0:nb], AF.Sigmoid, bias=fb[h])
            p.act(lfR[h][sl], lfR[h][sl], AF.Ln)
            p.tt('pool', lfR[h][sl], lfR[h][sl], pm[:, 0:nb], ALU.mult)
            p.op('dve', lambda e: e.tensor_tensor_scan(bR[h][sl], cm[:, 0:nb], lfR[h][sl], 0.0, ALU.mult, ALU.add),
                 r=[cm, lfR[h]], w=[bR[h]])
            p.tt('dve', gR[h][sl], liR[h][sl], bR[h][sl], ALU.subtract)
        for ci in range(nch):
            cs = slice(ci * 64, ci * 64 + 64)
            proj_tm(p, PS[3][0:64, 0:256], Wvo, 0, 256, hTb, ci * 64, 64)
            p.copy('act', vo1[:, :, 0:64], PS[3][0:64, 0:128].rearrange("p (h d) -> p h d", d=64))
            p.act(sgo, PS[3][0:64, 128:256], AF.Sigmoid)
            for h in range(2):
                C = Cst[h]; m = mst[h]
                p.tr(PS[4][0:64, 0:64], bR[h][:, cs], ident[0:64, 0:64])
                p.copy('act', bcol, PS[4][0:64, 0:1])
                p.stt(dl, gR[h][:, cs], bcol, maskL, ALU.add, ALU.add)
                p.op('dve', lambda e: e.reduce_max(sm[:, 0:1], dl, AX.X), r=[dl], w=[sm])
                p.tt('dve', sm[:, 1:2], bcol, m, ALU.add)
                p.tt('dve', sm[:, 2:3], sm[:, 0:1], sm[:, 1:2], ALU.max)
                p.ts('dve', sm[:, 3:4], sm[:, 2:3], -1.0, None, ALU.mult)
                p.act(sw, dl, AF.Exp, bias=sm[:, 3:4])
                p.mm(PS[5][0:64, 0:64], qT[h][:, cs], kT[h][:, cs])
                p.tt('dve', sw, sw, PS[5][0:64, 0:64], ALU.mult)
                p.tr(PS[4][0:64, 64:128], sw, ident[0:64, 0:64])
                p.copy('act', swT, PS[4][0:64, 64:128])
                p.mm(PS[6][0:64, 0:65], swT, vo1[:, h, :])
                p.mm(PS[6][0:64, 128:193], qT[h][:, cs], C)
                p.tt('dve', sm[:, 4:5], sm[:, 1:2], sm[:, 2:3], ALU.subtract)
                p.act(sm[:, 5:6], sm[:, 4:5], AF.Exp)
                p.act(t2, PS[6][0:64, 128:193], AF.Copy, scale=sm[:, 5:6])
                p.tt('dve', nd, PS[6][0:64, 0:65], t2, ALU.add)
                p.ts('dve', sm[:, 6:7], nd[:, 64:65], -1.0, None, ALU.mult)
                p.tt('dve', sm[:, 6:7], sm[:, 6:7], nd[:, 64:65], ALU.max)
                p.act(sm[:, 7:8], sm[:, 2:3], AF.Exp, scale=-1.0)
                p.tt('dve', sm[:, 8:9], sm[:, 6:7], sm[:, 7:8], ALU.max)
                p.op('dve', lambda e: e.reciprocal(sm[:, 9:10], sm[:, 8:9]), r=[sm], w=[sm])
                p.ts('dve', hh[:, h * 64:h * 64 + 64], nd[:, 0:64], sm[:, 9:10], None, ALU.mult)
                blast = bR[h][:, ci * 64 + 63:ci * 64 + 64]
                p.op('dve', lambda e: e.reduce_max(sm[:, 10:11], gR[h][:, cs], AX.X), r=[gR[h]], w=[sm])
                p.ts('dve', sm[:, 11:12], sm[:, 10:11], -1.0, None, ALU.mult)
                p.tt('dve', sm[:, 10:11], sm[:, 10:11], blast, ALU.add)
                p.act(wl, gR[h][:, cs], AF.Exp, bias=sm[:, 11:12], scale=1.0)
                p.tt('dve', kw, kT[h][:, cs], wl, ALU.mult)
                p.tr(PS[7][0:64, 0:64], kw, ident[0:64, 0:64])
                p.copy('act', kwt, PS[7][0:64, 0:64])
                p.mm(PS[7][0:64, 128:193], kwt, vo1[:, h, :])
                p.tt('dve', sm[:, 0:1], blast, m, ALU.add)
                p.tt('dve', sm[:, 1:2], sm[:, 0:1], sm[:, 10:11], ALU.max)
                p.tt('dve', sm[:, 2:3], sm[:, 0:1], sm[:, 1:2], ALU.subtract)
                p.act(sm[:, 2:3], sm[:, 2:3], AF.Exp)
                p.tt('dve', sm[:, 3:4], sm[:, 10:11], sm[:, 1:2], ALU.subtract)
                p.act(sm[:, 3:4], sm[:, 3:4], AF.Exp)
                p.act(cl, PS[7][0:64, 128:193], AF.Copy, scale=sm[:, 3:4])
                p.stt(C, C, sm[:, 2:3], cl, ALU.mult, ALU.add)
                p.copy('dve', m, sm[:, 1:2])
            p.tt('dve', hh, hh, sgo, ALU.mult)
            for h in range(2):
                hs = slice(h * 64, h * 64 + 64)
                p.op('dve', lambda e: e.bn_stats(bst, hh[:, hs]), r=[hh], w=[bst])
                p.op('dve', lambda e: e.bn_aggr(bmv, bst), r=[bst], w=[bmv])
                p.act(bmv[:, 1:2], bmv[:, 1:2], AF.Sqrt, bias=eps5)
                p.op('dve', lambda e: e.reciprocal(bmv[:, 1:2], bmv[:, 1:2]), r=[bmv], w=[bmv])
                p.ts('dve', yt[:, hs], hh[:, hs], bmv[:, 0:1], bmv[:, 1:2], ALU.subtract, ALU.mult)
            p.tt('pool', yt, yt, ng, ALU.mult)
            p.dma('sp', y_d[b0 + ci * 64:b0 + ci * 64 + 64, :], yt)
    p.pop()


def build_mlstm(NCH):
    return standalone(emit_mlstm, NCH)


def emit_rwkv(cx, NCH, NSTEP):
    nc, p, PS = cx.nc, cx.p, cx.PS
    P_ = NCH * 64
    BLK = 512
    SUB = 32
    di = cx.inp
    hT_d = di("hT", [D, P_])
    w_d = di("w", [D, 640]); mu_d = di("mu", [128, 6])
    wup_d = di("wup", [64, 128]); aup_d = di("aup", [64, 128]); gup_d = di("gup", [128, 128])
    cols_d = di("cols", [128, 8])
    gng_d = di("gng", [128]); gnb_d = di("gnb", [128]); bo_d = di("blockones", [128, 128]); id_d = di("ident", [128, 128])
    y_d = cx.out("y", [P_, 128])
    scr = lambda n: cx.scratch(n, [P_ + 1, 128])
    k2s, nkas, vs, bons, gs, yraw = (scr(n) for n in ("k2s", "nkas", "vs", "bons", "gs", "yraw"))
    p.push()
    decT = p.sb([128, P_], name="decT"); kkT = p.sb([128, P_], name="kkT"); rTh = p.sb([128, P_ + 1], name="rTh")
    gng = p.sb([128, 128]); p.dma('sp', gng, gng_d.partition_broadcast(128))
    gnb = p.sb([128, 128]); p.dma('sp', gnb, gnb_d.partition_broadcast(128))
    epsg = p.sb([128, 1]); p.memset('dve', epsg, 64e-5)
    p.push()
    stage = [p.sb([128, 2048], name="stg%d" % i) for i in range(2)]
    W = load_w_bf16(p, w_d, 640, stage, "W")
    def ld(src, shape, nm):
        t = p.sb(shape, name=nm); p.dma('sp', t, src); return t
    mu = ld(mu_d, [128, 6], "mu"); wup = ld(wup_d, [64, 128], "wup"); aup = ld(aup_d, [64, 128], "aup")
    gup = ld(gup_d, [128, 128], "gup"); cols = ld(cols_d, [128, 8], "cols")
    bones = ld(bo_d, [128, 128], "bones"); ident = ld(id_d, [128, 128], "ident")
    omka = p.sb([128, 1]); p.ts('dve', omka, cols[:, 3:4], -1.0, 1.0, ALU.mult, ALU.add)
    p.memset('dve', rTh[:, 0:1], 0.0)
    hTf = p.sb([128, 8, BLK]); hTb = p.sb([128, 8, BLK], BF16)
    nrows = [128, 128, 128, 64, 64, 128]
    pre = [p.sb([nrows[i], BLK + 1], name="pre%d" % i) for i in range(6)]
    lp = [p.sb([nrows[i], BLK], name="lp%d" % i) for i in range(6)]
    for i in range(6):
        p.memset('dve', pre[i][:, 0:1], 0.0)
    dtmp = p.sb([128, BLK]); a_t = p.sb([128, BLK]); t1 = p.sb([128, BLK]); k2 = p.sb([128, BLK]); nka = p.sb([128, BLK])
    g_t = p.sb([128, BLK]); bon = p.sb([128, BLK]); tok = p.sb([128, 128], name="tokst")
    for b0 in range(0, P_, BLK):
        nb = min(BLK, P_ - b0)
        sl = (slice(None), slice(0, nb))
        load_hblock(p, hT_d, b0, nb, hTf, hTb)
        c0 = 0
        for i in range(6):
            nr = nrows[i]
            proj_fm(p, PS[i % 2][0:nr, 0:nb], W, c0, nr, hTb, 0, nb)
            c0 += nr
            p.copy('act', pre[i][:, 1:1 + nb], PS[i % 2][0:nr, 0:nb])
            p.tt('dve', dtmp[0:nr, 0:nb], pre[i][:, 0:nb], pre[i][:, 1:1 + nb], ALU.subtract)
            p.stt(lp[i][sl], dtmp[0:nr, 0:nb], mu[0:nr, i:i + 1], pre[i][:, 1:1 + nb], ALU.mult, ALU.add)
            p.copy('pool', pre[i][:, 0:1], pre[i][:, nb:nb + 1])
        r_, k_, v_, xw, xa, xg = lp
        bs = slice(b0, b0 + nb)
        p.copy('pool', rTh[:, 1 + b0:1 + b0 + nb], r_[sl])
        p.act(xw[sl], xw[sl], AF.Tanh)
        p.mm(PS[2][:, 0:nb], wup, xw[sl])
        p.act(t1[sl], PS[2][:, 0:nb], AF.Sigmoid, bias=cols[:, 0:1])
        p.act(decT[:, bs], t1[sl], AF.Exp, scale=-float(np.exp(-0.5)))
        p.mm(PS[3][:, 0:nb], aup, xa[sl])
        p.act(a_t[sl], PS[3][:, 0:nb], AF.Sigmoid, bias=cols[:, 1:2])
        p.act(xg[sl], xg[sl], AF.Sigmoid)
        p.mm(PS[2][:, 0:nb], gup, xg[sl])
        p.copy('act', g_t[sl], PS[2][:, 0:nb])
        p.ts('dve', t1[sl], k_[sl], cols[:, 2:3], None, ALU.mult)
        p.tt('pool', dtmp[sl], t1[sl], t1[sl], ALU.mult)
        p.mm(PS[3][:, 0:nb], bones, dtmp[sl])
        p.act(dtmp[sl], PS[3][:, 0:nb], AF.Sqrt)
        p.ts('dve', dtmp[sl], dtmp[sl], 1e-12, None, ALU.max)
        p.op('dve', lambda e: e.reciprocal(dtmp[sl], dtmp[sl]), r=[dtmp], w=[dtmp])
        p.tt('dve', kkT[:, bs], t1[sl], dtmp[sl], ALU.mult)
        p.ts('dve', t1[sl], a_t[sl], cols[:, 3:4], omka, ALU.mult, ALU.add)
        p.tt('dve', k2[sl], k_[sl], t1[sl], ALU.mult)
        p.stt(nka[sl], kkT[:, bs], -1.0, a_t[sl], ALU.mult, ALU.mult)
        p.stt(t1[sl], r_[sl], cols[:, 4:5], k2[sl], ALU.mult, ALU.mult)
        p.mm(PS[2][:, 0:nb], bones, t1[sl])
        p.tt('dve', bon[sl], PS[2][:, 0:nb], v_[sl], ALU.mult)
        for (src, dst) in ((k2, k2s), (nka, nkas), (v_, vs), (bon, bons), (g_t, gs)):
            for j in range(nb // 128):
                p.tr(PS[4 + j % 2][:, 0:128], src[:, j * 128:(j + 1) * 128], ident)
                p.copy('act', tok, PS[4 + j % 2][:, 0:128])
                p.dma('sp', dst[b0 + j * 128:b0 + (j + 1) * 128, :], tok)
    p.pop()
    ST = p.sb([128, 64], name="ST"); p.memset('dve', ST, 0.0)
    KVl = p.sb([2, SUB, 128], name="KVl"); KAl = p.sb([4, SUB, 128], name="KAl"); Vr = p.sb([2, SUB, 64], name="Vr")
    Ycp = p.sb([4, SUB, 64], name="Ycp"); L1 = p.sb([128, SUB, 4], name="L1")
    p.memset('dve', KVl, 0.0); p.memset('dve', KAl, 0.0); p.memset('dve', L1, 0.0)
    for s0 in range(0, NSTEP, SUB):
        ns = min(SUB, NSTEP - s0)
        for h in range(2):
            hc = slice(h * 64, h * 64 + 64)
            p.dma('sp', KVl[h:h + 1, 0:ns, hc], k2s[s0:s0 + ns, hc].unsqueeze(0))
            p.dma('sp', KAl[2 * h:2 * h + 1, 0:ns, hc], nkas[s0:s0 + ns, hc].unsqueeze(0))
            p.dma('sp', Vr[h:h + 1, 0:ns, :], vs[s0:s0 + ns, hc].unsqueeze(0))
            p.copy('pool', L1[hc, 0:ns, 2 * h], kkT[hc, s0:s0 + ns])
            p.copy('pool', L1[hc, 0:ns, 2 * h + 1], rTh[hc, s0:s0 + ns])
        for s in range(ns):
            t = s0 + s
            pa = PS[t % 2]; pb = PS[2 + t % 2]
            p.mm(pa[0:4, 0:64], L1[:, s, :], ST)
            p.copy('act', Ycp[:, s, :], pa[0:4, 0:64])
            p.mm(pb[:, 0:64], KVl[:, s, :], Vr[:, s, :], start=True, stop=False)
            p.mm(pb[:, 0:64], KAl[:, s, :], Ycp[:, s, :], start=False, stop=True)
            p.stt(ST, ST, decT[:, t:t + 1], pb[:, 0:64], ALU.mult, ALU.add)
        for h in range(2):
            p.dma('sp', yraw[s0:s0 + ns, h * 64:h * 64 + 64].unsqueeze(0), Ycp[2 * h + 1:2 * h + 2, 0:ns, :])
    yt = p.sb([128, 128]); bt = p.sb([128, 128]); gt = p.sb([128, 128]); ot = p.sb([128, 128])
    bst = p.sb([128, 6]); bmv = p.sb([128, 2])
    for t0 in range(0, P_, 128):
        if t0 + 1 >= NSTEP:
            break
        nt = min(128, NSTEP - 1 - t0)
        p.dma('sp', yt[0:nt, :], yraw[t0 + 1:t0 + 1 + nt, :])
        p.dma('sp', bt[0:nt, :], bons[t0:t0 + nt, :])
        p.dma('sp', gt[0:nt, :], gs[t0:t0 + nt, :])
        for h in range(2):
            hs = slice(h * 64, h * 64 + 64)
            p.op('dve', lambda e: e.bn_stats(bst[0:nt, :], yt[0:nt, hs]), r=[yt], w=[bst])
            p.op('dve', lambda e: e.bn_aggr(bmv[0:nt, :], bst[0:nt, :]), r=[bst], w=[bmv])
            p.act(bmv[0:nt, 1:2], bmv[0:nt, 1:2], AF.Sqrt, bias=epsg[0:nt, :])
            p.op('dve', lambda e: e.reciprocal(bmv[0:nt, 1:2], bmv[0:nt, 1:2]), r=[bmv], w=[bmv])
            p.ts('dve', ot[0:nt, hs], yt[0:nt, hs], bmv[0:nt, 0:1], bmv[0:nt, 1:2], ALU.subtract, ALU.mult)
        p.tt('pool', ot[0:nt, :], ot[0:nt, :], gng[0:nt, :], ALU.mult)
        p.tt('pool', ot[0:nt, :], ot[0:nt, :], gnb[0:nt, :], ALU.add)
        p.tt('dve', ot[0:nt, :], ot[0:nt, :], bt[0:nt, :], ALU.add)
        p.tt('dve', ot[0:nt, :], ot[0:nt, :], gt[0:nt, :], ALU.mult)
        p.dma('sp', y_d[t0:t0 + nt, :], ot[0:nt, :])
    p.pop()


def build_rwkv(NCH, NSTEP):
    return standalone(emit_rwkv, NCH, NSTEP)


def emit_dsa(cx, NKT, NSLOT, NROUND):
    nc, p, PS = cx.nc, cx.p, cx.PS
    TK = NKT * 128
    NBq = NSLOT
    di = cx.inp
    hT_d = di("hT", [D, TK])
    fused = "rk" in cx.over
    if not fused:
        hTq_d = di("hTq", [D, NSLOT * 128])
    wq_d = di("wq", [D, 256]); wckv_d = di("wckv", [D, 128]); widx_d = di("widx", [D, 296])
    kvg_d = di("kvg", [128]); wuk_d = di("wuk", [128, 64]); wuv_d = di("wuv", [128, 64])
    b3_d = di("B3raw", [3, 4, 128, 128]); mA_d = di("maskA", [128, 128]); mB_d = di("maskB", [128, 128]); c31_d = di("c31", [128, 4])
    id_d = di("ident", [128, 128])
    y_d = cx.out("y", [NBq * 128, 256])
    p.push()
    stage = [p.sb([128, 512], name="stg%d" % i) for i in range(2)]
    Wq = load_w_bf16(p, wq_d, 256, stage, "Wq"); Wc = load_w_bf16(p, wckv_d, 128, stage, "Wc")
    Widx = p.sb([128, 8, 296], name="Widx"); p.dma('sp', Widx, widx_d.rearrange("(c f) n -> f c n", f=128))
    def ld(src, shape, nm):
        t = p.sb(shape, name=nm); p.dma('sp', t, src); return t
    kvg = p.sb([128, 128]); p.dma('sp', kvg, kvg_d.partition_broadcast(128))
    wuk = ld(wuk_d, [128, 64], "wuk"); wuv = ld(wuv_d, [128, 64], "wuv")
    c31 = ld(c31_d, [128, 4], "c31"); maskA = ld(mA_d, [128, 128], "maskA"); maskB = ld(mB_d, [128, 128], "maskB"); ident = ld(id_d, [128, 128], "ident")
    Badj = [p.sb([128, 4, 128], name="Badj%d" % k) for k in range(3)]
    for k in range(3):
        p.dma('sp', Badj[k], b3_d[k].rearrange("h i s -> i h s"))
        for h in range(4):
            p.ts('dve', Badj[k][:, h, :], Badj[k][:, h, :], c31[:, h:h + 1], None, ALU.subtract)
    eps6 = p.sb([128, 1]); p.memset('dve', eps6, 1e-6)
    kiT = p.sb([32, TK], name="kiT"); kT = p.sb([64, TK], name="kT"); v_all = p.sb([128, NKT, 64], name="v_all")
    score = p.sb([128, TK], name="score"); wk = p.sb([128, TK], name="wk")
    hTf = p.sb([128, 8, 128]); hTb = p.sb([128, 8, 128], BF16)
    ct = p.sb([128, 128]); sq = p.sb([128, 128]); cT = p.sb([128, 128]); sm = p.sb([128, 8], name="dsm")
    for kt in range(NKT):
        ks = slice(kt * 128, kt * 128 + 128)
        load_hblock(p, hT_d, kt * 128, 128, hTf, hTb)
        for c in range(8):
            p.mm(PS[0][0:32, 0:128], Widx[:, c, 256:288], hTf[:, c, :], start=(c == 0), stop=(c == 7))
        p.copy('act', kiT[:, ks], PS[0][0:32, 0:128])
        proj_tm(p, PS[1][:, 0:128], Wc, 0, 128, hTb, 0, 128)
        p.copy('act', ct, PS[1][:, 0:128])
        p.tt('dve', sq, ct, ct, ALU.mult)
        p.op('dve', lambda e: e.reduce_sum(sm[:, 0:1], sq, AX.X), r=[sq], w=[sm])
        p.act(sm[:, 0:1], sm[:, 0:1], AF.Sqrt, bias=eps6, scale=1.0 / 128.0)
        p.op('dve', lambda e: e.reciprocal(sm[:, 0:1], sm[:, 0:1]), r=[sm], w=[sm])
        p.stt(ct, ct, sm[:, 0:1], kvg, ALU.mult, ALU.mult)
        p.tr(PS[2][:, 0:128], ct, ident)
        p.copy('act', cT, PS[2][:, 0:128])
        p.mm(PS[3][0:64, 0:128], wuk, cT)
        p.copy('act', kT[:, ks], PS[3][0:64, 0:128])
        p.mm(PS[3][:, 128:192], cT, wuv)
        p.copy('act', v_all[:, kt, :], PS[3][:, 128:192])
    qT = p.sb([64, 4, 128], name="qT"); qiT = p.sb([32, 8, 128], name="qiT"); wi = p.sb([128, 8], name="wi")
    if fused:
        hTf2 = p.sb([128, 8, 128], name="hTf2"); rkq = p.sb([128, 2], name="rkq"); p.dma('sp', rkq, cx.over["rk"])
    rel = [p.sb([128, 512], name="rel%d" % i) for i in range(2)]
    m8 = p.sb([128, 8], name="m8"); PT = [p.sb([128, 128], name="PT%d" % i) for i in range(2)]
    yt = p.sb([128, 256], name="yt")
    for bi in range(NSLOT):
        S = min((2 * bi + 2) * 128, TK)
        jA = 2 * bi
        if not fused:
            load_hblock(p, hTq_d, bi * 128, 128, hTf, hTb)
        else:
            hsrc = hT_d.rearrange("(c f) n -> f c n", f=128)
            p.dma('sp', hTf, hsrc[:, :, jA * 128:jA * 128 + 128])
            if (jA + 2) * 128 <= TK:
                p.dma('sp', hTf2, hsrc[:, :, (jA + 1) * 128:(jA + 2) * 128])
            else:
                p.memset('pool', hTf2, 0.0)
            p.ts('dve', hTf, hTf, rkq[:, 0:1], None, ALU.mult)
            p.stt(hTf, hTf2, rkq[:, 1:2], hTf, ALU.mult, ALU.add)
            p.copy('pool', hTb, hTf)
        for h in range(4):
            proj_fm(p, PS[0][0:64, 0:128], Wq, h * 64, 64, hTb, 0, 128)
            p.op('act', lambda e: e.mul(qT[:, h, :], PS[0][0:64, 0:128], 0.125), r=[PS[0]], w=[qT])
        for hi in range(8):
            for c in range(8):
                p.mm(PS[1][0:32, 0:128], Widx[:, c, hi * 32:(hi + 1) * 32], hTf[:, c, :], start=(c == 0), stop=(c == 7))
            p.copy('act', qiT[:, hi, :], PS[1][0:32, 0:128])
        for c in range(8):
            p.mm(PS[2][:, 0:8], hTf[:, c, :], Widx[:, c, 288:296], start=(c == 0), stop=(c == 7))
        p.op('act', lambda e: e.mul(wi, PS[2][:, 0:8], 1.0 / 16.0), r=[PS[2]], w=[wi])
        for k0 in range(0, S, 512):
            kn = min(512, S - k0)
            for hi in range(8):
                pb = PS[3 + hi % 2]
                p.mm(pb[:, 0:kn], qiT[:, hi, :], kiT[:, k0:k0 + kn])
                r_ = rel[hi % 2]
                p.act(r_[:, 0:kn], pb[:, 0:kn], AF.Relu)
                if hi == 0:
                    p.ts('dve', score[:, k0:k0 + kn], r_[:, 0:kn], wi[:, 0:1], None, ALU.mult)
                else:
                    p.stt(score[:, k0:k0 + kn], r_[:, 0:kn], wi[:, hi:hi + 1], score[:, k0:k0 + kn], ALU.mult, ALU.add)
        p.memset('dve', score[:, 0:N_META], 1e30)
        p.tt('dve', score[:, jA * 128:jA * 128 + 128], score[:, jA * 128:jA * 128 + 128], maskA, ALU.min)
        if (jA + 2) * 128 <= S:
            p.tt('dve', score[:, (jA + 1) * 128:(jA + 2) * 128], score[:, (jA + 1) * 128:(jA + 2) * 128], maskB, ALU.min)
        if S > NROUND * 8:
            p.copy('pool', wk[:, 0:S], score[:, 0:S])
            for r in range(NROUND):
                p.op('dve', lambda e: e.max(m8, wk[:, 0:S]), r=[wk], w=[m8])
                if r < NROUND - 1:
                    p.op('dve', lambda e: e.match_replace(wk[:, 0:S], m8, wk[:, 0:S], -3e38), r=[m8, wk], w=[wk])
            p.ts('dve', sm[:, 1:2], m8[:, 7:8], -1e29, None, ALU.max)
        else:
            p.memset('dve', sm[:, 1:2], -1e29)
        p.ts('dve', wk[:, 0:S], score[:, 0:S], sm[:, 1:2], 1.0, ALU.is_ge, ALU.subtract)
        lg = score
        for h in range(4):
            for k0 in range(0, S, 512):
                kn = min(512, S - k0)
                pb = PS[5 + (k0 // 512) % 2]
                p.mm(pb[:, 0:kn], qT[:, h, :], kT[:, k0:k0 + kn])
                p.stt(lg[:, k0:k0 + kn], wk[:, k0:k0 + kn], 1e30, pb[:, 0:kn], ALU.mult, ALU.add)
            for k in range(3):
                jb = jA - 1 + k
                if jb >= 0 and (jb + 1) * 128 <= S:
                    p.tt('dve', lg[:, jb * 128:(jb + 1) * 128], lg[:, jb * 128:(jb + 1) * 128], Badj[k][:, h, :], ALU.add)
            p.op('dve', lambda e: e.reduce_max(sm[:, 2:3], lg[:, 0:S], AX.X), r=[lg], w=[sm])
            p.ts('dve', sm[:, 3:4], sm[:, 2:3], -1.0, None, ALU.mult)
            p.act(lg[:, 0:S], lg[:, 0:S], AF.Exp, bias=sm[:, 3:4], accum_out=sm[:, 4:5])
            nkb = S // 128
            for kb in range(nkb):
                pt = PS[1 + kb % 2]
                p.tr(pt[:, 0:128], lg[:, kb * 128:(kb + 1) * 128], ident)
                p.copy('act' if kb % 2 == 0 else 'dve', PT[kb % 2], pt[:, 0:128])
                p.mm(PS[7][:, 0:64], PT[kb % 2], v_all[:, kb, :], start=(kb == 0), stop=(kb == nkb - 1))
            p.op('dve', lambda e: e.reciprocal(sm[:, 5:6], sm[:, 4:5]), r=[sm], w=[sm])
            p.ts('dve', yt[:, h * 64:(h + 1) * 64], PS[7][:, 0:64], sm[:, 5:6], None, ALU.mult)
        p.dma('sp', y_d[bi * 128:(bi + 1) * 128, :], yt)
    p.pop()


def build_dsa(NKT, NSLOT, NROUND):
    return standalone(emit_dsa, NKT, NSLOT, NROUND)


OFFS = [0, 1024, 1808, 2488, 3520, 7616]


def _c(a):
    return np.ascontiguousarray(a, dtype=np.float32)


def _t5_bucket(n):
    n = np.maximum(n, 0)
    me = 16
    large = me + (np.log(np.maximum(n, 1).astype(np.float32) / me) / np.log(128 / me) * (32 - me)).astype(np.int32)
    return np.where(n < me, n, np.minimum(large, 31))


def _run(nc, in_maps):
    res = run_bass_kernel_spmd(nc, in_maps, core_ids=list(range(NCORES)))
    return res.results


def _gla_inputs(inp, l, hp, hT, NCH):
    w_in = inp['w_in'][l]; c0 = OFFS[1]
    heads = [2 * hp, 2 * hp + 1]
    qcols = np.concatenate([np.arange(c0 + hh * 32, c0 + hh * 32 + 32) for hh in heads])
    kcols = qcols + 128
    vcols = np.concatenate([np.arange(c0 + 256 + hh * 64, c0 + 256 + hh * 64 + 64) for hh in heads])
    acols = np.arange(c0 + 512, c0 + 528)
    ocols = vcols + 256 + 16
    return dict(hT=hT, wqk=_c(w_in[:, np.concatenate([qcols, kcols])]), wvo=_c(w_in[:, np.concatenate([vcols, ocols])]),
                wa=_c(w_in[:, acols]), aup=_c(inp['gla_a_up'][l][:, hp * 64:(hp + 1) * 64]),
                ab=_c(inp['gla_a_b'][l][hp * 64:(hp + 1) * 64, None]), ng=_c(np.tile(inp['gla_norm_g'][l], 2)),
                cmask=(np.arange(NCH * 64) % 64 != 0).astype(np.float32),
                maskU2=_c(np.tile(np.triu(np.ones((64, 64), np.float32)), (1, 2))), ident=np.eye(128, dtype=np.float32))


def _mlstm_inputs(inp, l, hp, hT, NCH):
    w_in = inp['w_in'][l]; c0 = OFFS[3]
    heads = [2 * hp, 2 * hp + 1]
    hc = np.concatenate([np.arange(hh * 64, hh * 64 + 64) for hh in heads])
    P = NCH * 64; pos = np.arange(P); real = (pos >= 48)
    return dict(hT=hT, wq=_c(w_in[:, c0 + hc]), wk=_c(w_in[:, c0 + 256 + hc]),
                wvo=_c(w_in[:, np.concatenate([c0 + 512 + hc, c0 + 776 + hc])]),
                wi=_c(np.repeat(w_in[:, [c0 + 768 + hh for hh in heads]], 64, axis=1)),
                wf=_c(np.repeat(w_in[:, [c0 + 772 + hh for hh in heads]], 64, axis=1)),
                cwq=_c(inp['mlstm_conv_w'][l][:, hc].T), cwk=_c(inp['mlstm_conv_w'][l][:, 256 + hc].T),
                cbq=_c(inp['mlstm_conv_b'][l][hc, None]), cbk=_c(inp['mlstm_conv_b'][l][256 + hc, None]),
                ib=_c(np.repeat(inp['mlstm_i_b'][l][heads], 64)[:, None]), fb=_c(np.repeat(inp['mlstm_f_b'][l][heads], 64)[:, None]),
                ng=_c(inp['mlstm_norm_g'][l][hc]), cmask=(pos % 64 != 0).astype(np.float32),
                pm01=real.astype(np.float32), pmneg=np.where(real, 0.0, -1e30).astype(np.float32),
                maskL=np.where(np.tril(np.ones((64, 64))) > 0, 0.0, -1e30).astype(np.float32), ident=np.eye(128, dtype=np.float32))


def _rwkv_inputs(inp, l, hp, hT):
    w_in = inp['w_in'][l]
    hc = np.arange(hp * 128, hp * 128 + 128)
    colsel = np.concatenate([hc, 256 + hc, 512 + hc, np.arange(768, 832), np.arange(832, 896), np.arange(896, 1024)])
    mu = inp['rwkv_mu'][l]
    mu6 = np.zeros((128, 6), np.float32)
    mu6[:, 0] = mu[hc]; mu6[:, 1] = mu[256 + hc]; mu6[:, 2] = mu[512 + hc]
    mu6[:64, 3] = mu[768:832]; mu6[:64, 4] = mu[832:896]; mu6[:, 5] = mu[896:1024]
    cols = np.zeros((128, 8), np.float32)
    cols[:, 0] = inp['rwkv_w0'][l][hc]; cols[:, 1] = inp['rwkv_a0'][l][hc]; cols[:, 2] = inp['rwkv_k_k'][l][hc]
    cols[:, 3] = inp['rwkv_k_a'][l][hc]; cols[:, 4] = inp['rwkv_r_k'][l].reshape(-1)[hc]
    bo = np.zeros((128, 128), np.float32); bo[:64, :64] = 1; bo[64:, 64:] = 1
    return dict(hT=hT, w=_c(w_in[:, colsel]), mu=mu6, wup=_c(inp['rwkv_w_up'][l][:, hc]), aup=_c(inp['rwkv_a_up'][l][:, hc]),
                gup=_c(inp['rwkv_g_up'][l][:, hc]), cols=cols, gng=_c(inp['rwkv_gn_g'][l][hc]), gnb=_c(inp['rwkv_gn_b'][l][hc]),
                blockones=bo, ident=np.eye(128, dtype=np.float32))


def _dsa_inputs(inp, l, half, hT_tok, NSLOT):
    w_in = inp['w_in'][l]; c0 = OFFS[2]; rb = inp['rel_bias']
    i = np.arange(128)[:, None]; s = np.arange(128)[None, :]
    bd = _t5_bucket(i - s); bp = _t5_bucket(128 + i - s)
    Draw = np.stack([np.where(s <= i, rb[bd, hh], rb[31, hh]) for hh in range(4)]).astype(np.float32)
    Praw = np.stack([rb[bp, hh] for hh in range(4)]).astype(np.float32)
    c31t = np.stack([np.full((128, 128), rb[31, hh]) for hh in range(4)]).astype(np.float32)
    B3 = np.stack([Praw, Draw, c31t]) if half == 0 else np.stack([c31t, Praw, Draw])
    mm = np.where(s <= i, 3e38, -1e30).astype(np.float32)
    maskA = mm if half == 0 else np.full((128, 128), 3e38, np.float32)
    maskB = np.full((128, 128), -1e30, np.float32) if half == 0 else mm
    hTq = np.zeros((1024, NSLOT * 128), np.float32)
    for ii in range(NSLOT):
        j = 2 * ii + half
        if j * 128 < hT_tok.shape[1]:
            hTq[:, ii * 128:(ii + 1) * 128] = hT_tok[:, j * 128:(j + 1) * 128]
    return dict(hT=hT_tok, hTq=hTq, B3raw=_c(B3), maskA=maskA, maskB=maskB,
                wq=_c(w_in[:, c0:c0 + 256]), wckv=_c(w_in[:, c0 + 256:c0 + 384]), widx=_c(w_in[:, c0 + 384:c0 + 680]),
                kvg=_c(inp['dsa_kv_norm_g'][l]), wuk=_c(inp['dsa_w_uk'][l]), wuv=_c(inp['dsa_w_uv'][l]),
                c31=_c(np.tile(rb[31][None, :], (128, 1))), ident=np.eye(128, dtype=np.float32))


def build_fused(NTL, NCH, NSTEP, NKT, NSLOT, NROUND, HALF):
    nc, p, PS = new_prog()
    NTOK = NTL * 128; P_ = NCH * 64; TK = NKT * 128
    PW = max(48 + HALF + NTOK, 48 + TK, P_)
    PWy = PW
    groups = [[2 * g, 2 * g + 1] for g in range(NCORES // 2)]
    ext = lambda n, s_: nc.dram_tensor(n, list(s_), F32, kind="ExternalInput").ap()
    itn = lambda n, s_: nc.dram_tensor(n, list(s_), F32, kind="Internal").ap()
    ident_d = ext("ident", [128, 128]); rk_d = ext("rk", [128, 2]); x_d = ext("x", [NTOK, D])
    out_d = nc.dram_tensor("out", [NTOK, D], F32, kind="ExternalOutput").ap()
    h_own = itn("h_own", [NTOK, D]); hT_own = itn("hT_own", [D, NTOK])
    hT_pos = itn("hT_pos", [D, PW]); Y_abd = itn("Y_abd", [PWy, 384]); Y_c = itn("Y_c", [NSLOT * 128, 256])

    class Gathered:
        def __init__(self, name, src, bounds):
            self.src, self.bounds = src, bounds
            self.bufs = [itn("%s_%d" % (name, k), [2 * (b1 - b0), src.shape[1]]) for k, (b0, b1) in enumerate(bounds)]

        def gather(self):
            for (b0, b1), g in zip(self.bounds, self.bufs):
                p.coll("AllGather", self.src[b0:b1, :], g, groups)

        def rows(self, q, r0, n):
            for (b0, b1), g in zip(self.bounds, self.bufs):
                if b0 <= r0 and r0 + n <= b1:
                    return g[q * (b1 - b0) + r0 - b0:q * (b1 - b0) + r0 - b0 + n, :]
            raise ValueError("row range straddles gather chunks")

    def bounds_of(total, step, first=None):
        bs = []
        b0 = 0
        nxt = first if first is not None else step
        while b0 < total:
            b1 = min(total, nxt)
            bs.append((b0, b1)); b0 = b1; nxt = b1 + step
        return bs
    G_h = Gathered("G_h", hT_own, bounds_of(D, 64))
    G_abd = Gathered("G_abd", Y_abd, bounds_of(PWy, 1024, 48 + 1024))
    G_c = Gathered("G_c", Y_c, bounds_of(NSLOT * 128, 1024))
    p.push()
    z = p.sb([128, 512], name="zeros"); p.memset('dve', z, 0.0)
    for c in range(8):
        p.dma('sp', hT_pos[c * 128:(c + 1) * 128, 0:48], z[:, 0:48])
    for r0 in range(0, PWy, 128):
        n = min(128, PWy - r0)
        p.dma('sp', Y_abd[r0:r0 + n, :], z[0:n, 0:384])
    p.pop()
    emit_ln_in(Ctx(nc, p, PS, "ln_", dict(x=x_d, y=h_own, hT_out=hT_own, ident=ident_d)), NTL)
    for l in range(DEPTH):
        pre = "l%d_" % l
        G_h.gather()
        for q in range(2):
            for (b0, b1) in G_h.bounds:
                p.dma('sp', hT_pos[b0:b1, 48 + HALF * q:48 + HALF * q + NTOK], G_h.rows(q, b0, b1 - b0))
        emit_rwkv(Ctx(nc, p, PS, pre + "rwkv_", dict(hT=hT_pos[:, 0:P_], y=Y_abd[0:P_, 0:128], ident=ident_d)), NCH, NSTEP)
        emit_gla(Ctx(nc, p, PS, pre + "gla_", dict(hT=hT_pos[:, 0:P_], y=Y_abd[0:P_, 128:256], ident=ident_d)), NCH)
        emit_mlstm(Ctx(nc, p, PS, pre + "mlstm_", dict(hT=hT_pos[:, 0:P_], y=Y_abd[0:P_, 256:384], ident=ident_d)), NCH)
        emit_dsa(Ctx(nc, p, PS, pre + "dsa_", dict(hT=hT_pos[:, 48:48 + TK], y=Y_c, ident=ident_d, rk=rk_d)), NKT, NSLOT, NROUND)
        G_abd.gather()
        G_c.gather()
        last = (l == DEPTH - 1)
        over = dict(h=h_own, hT=hT_own, G_abd=G_abd, G_c=G_c, rk=rk_d, PWy=PWy, HALF=HALF, NSLOT=NSLOT, ident=ident_d,
                    out=(out_d if last else h_own))
        if not last:
            over["hT_out"] = hT_own
        emit_post(Ctx(nc, p, PS, pre + "post_", over), NTL, 8)
    p.finish()
    return nc


def kernel(**inputs):
    inp = {k: np.asarray(v) for k, v in inputs.items()}
    x = inp['x'].astype(np.float32)
    B, S, _ = x.shape
    T = S + N_META
    HALF = S // 2
    NTL = -(-(T - HALF) // 128)
    NTOK = NTL * 128
    NCH = -(-(T + 48 + 1) // 64); NCH += NCH % 2
    NSTEP = 48 + T + 1
    NKT = -(-T // 128)
    NSLOT = (NKT + 1) // 2
    NROUND = min(256, S // 4) // 8
    assert B * 2 == NCORES and (HALF // 128) % 2 == 0
    nc = build_fused(NTL, NCH, NSTEP, NKT, NSLOT, NROUND, HALF)
    hcat = np.concatenate([np.broadcast_to(inp['meta'][None].astype(np.float32), (B, N_META, D)), x], 1)
    maps = []
    for c in range(NCORES):
        b, r = c // 2, c % 2
        xo = np.zeros((NTOK, D), np.float32)
        seg = hcat[b, r * HALF:min(T, r * HALF + NTOK)] if r == 1 else hcat[b, 0:HALF]
        xo[:len(seg)] = seg
        m = dict(x=xo, ident=np.eye(128, dtype=np.float32), rk=np.tile(np.array([[1.0 - r, float(r)]], np.float32), (128, 1)))
        m["ln_g"] = _c(inp['ln_in_g']); m["ln_b"] = _c(inp['ln_in_b'])
        dummy = np.zeros((D, 1), np.float32)
        for l in range(DEPTH):
            pre = "l%d_" % l
            for nm, d in (("rwkv_", _rwkv_inputs(inp, l, r, dummy)), ("gla_", _gla_inputs(inp, l, r, dummy, NCH)),
                          ("mlstm_", _mlstm_inputs(inp, l, r, dummy, NCH)), ("dsa_", _dsa_inputs(inp, l, r, dummy, 0))):
                for k, v in d.items():
                    if k not in ("hT", "hTq", "ident"):
                        m[pre + nm + k] = v
            wr = _c(np.concatenate([inp['moe_w_grp'][l], inp['moe_w_rt'][l]], 1))
            br = _c(np.concatenate([inp['moe_b_grp'][l], inp['moe_b_rt'][l]], 0))
            post = dict(wg=_c(inp['w_in'][l][:, OFFS[4]:OFFS[5]]), wb=_c(inp['w_branch'][l].reshape(D, D)), wo=_c(inp['w_out'][l]),
                        ln1g=_c(inp['ln1_g'][l]), ln1b=_c(inp['ln1_b'][l]), ln2g=_c(inp['ln2_g'][l]), ln2b=_c(inp['ln2_b'][l]),
                        wr=wr, br=br, mg=_c(inp['moe_w_gate'][l]), mu=_c(inp['moe_w_up'][l]), md=_c(inp['moe_w_down'][l]))
            for k, v in post.items():
                m[pre + "post_" + k] = v
        maps.append(m)
    res = _run(nc, maps)
    out = np.zeros((B, T, D), np.float32)
    for c in range(NCORES):
        b, r = c // 2, c % 2
        n = HALF if r == 0 else T - HALF
        out[b, r * HALF:r * HALF + n] = res[c]["out"][:n]
    return np.ascontiguousarray(out[:, N_META:])


def kernel_unfused(**inputs):
    inp = {k: np.asarray(v) for k, v in inputs.items()}
    x = inp['x'].astype(np.float32)
    B, S, _ = x.shape
    T = S + N_META
    NTOKC = (B * T) // NCORES
    NTL = -(-NTOKC // 128)
    NCH = -(-(T + 48) // 64); NCH += NCH % 2
    if NCH * 64 < 48 + T + 1:
        NCH += 2
    P = NCH * 64
    NSTEP = 48 + T + 1
    NKT = -(-T // 128)
    NSLOT = (NKT + 1) // 2
    NROUND = min(256, S // 4) // 8
    ident = np.eye(128, dtype=np.float32)

    def tok_split(a):
        out = []
        for c in range(NCORES):
            sh = np.zeros((NTL * 128, a.shape[1]), np.float32)
            sh[:NTOKC] = a[c * NTOKC:(c + 1) * NTOKC]
            out.append(sh)
        return out

    def tok_merge(res, key):
        return np.concatenate([r[key][:NTOKC] for r in res], 0)

    hcat = np.concatenate([np.broadcast_to(inp['meta'][None], (B, N_META, D)), x], 1).reshape(B * T, D)
    res = _run(build_ln_in(NTL), [dict(x=s_, g=_c(inp['ln_in_g']), b=_c(inp['ln_in_b'])) for s_ in tok_split(hcat)])
    h = tok_merge(res, "y")

    for l in range(DEPTH):
        hb = h.reshape(B, T, D)
        hTpos = []
        hTtok = []
        for b in range(B):
            a = np.zeros((D, P), np.float32); a[:, 48:48 + T] = hb[b].T; hTpos.append(a)
            a2 = np.zeros((D, NKT * 128), np.float32); a2[:, :T] = hb[b].T; hTtok.append(a2)
        y = np.zeros((B, T, D), np.float32)
        res = _run(build_rwkv(NCH, NSTEP), [_rwkv_inputs(inp, l, c % 2, hTpos[c // 2]) for c in range(NCORES)])
        for c in range(NCORES):
            y[c // 2, :, (c % 2) * 128:(c % 2) * 128 + 128] = res[c]["y"][48:48 + T]
        res = _run(build_gla(NCH), [_gla_inputs(inp, l, c % 2, hTpos[c // 2], NCH) for c in range(NCORES)])
        for c in range(NCORES):
            y[c // 2, :, 256 + (c % 2) * 128:256 + (c % 2) * 128 + 128] = res[c]["y"][48:48 + T]
        res = _run(build_mlstm(NCH), [_mlstm_inputs(inp, l, c % 2, hTpos[c // 2], NCH) for c in range(NCORES)])
        for c in range(NCORES):
            y[c // 2, :, 768 + (c % 2) * 128:768 + (c % 2) * 128 + 128] = res[c]["y"][48:48 + T]
        res = _run(build_dsa(NKT, NSLOT, NROUND), [_dsa_inputs(inp, l, c % 2, hTtok[c // 2], NSLOT) for c in range(NCORES)])
        for c in range(NCORES):
            half = c % 2
            for ii in range(NSLOT):
                j = 2 * ii + half
                if j * 128 >= T:
                    continue
                n = min(128, T - j * 128)
                y[c // 2, j * 128:j * 128 + n, 512:768] = res[c]["y"][ii * 128:ii * 128 + n]
        yf = y.reshape(B * T, D)
        hs = tok_split(h); ys = tok_split(yf)
        wr = _c(np.concatenate([inp['moe_w_grp'][l], inp['moe_w_rt'][l]], 1))
        br = _c(np.concatenate([inp['moe_b_grp'][l], inp['moe_b_rt'][l]], 0))
        common = dict(wg=_c(inp['w_in'][l][:, OFFS[4]:OFFS[5]]), wb=_c(inp['w_branch'][l].reshape(D, D)), wo=_c(inp['w_out'][l]),
                      ln1g=_c(inp['ln1_g'][l]), ln1b=_c(inp['ln1_b'][l]), ln2g=_c(inp['ln2_g'][l]), ln2b=_c(inp['ln2_b'][l]),
                      wr=wr, br=br, mg=_c(inp['moe_w_gate'][l]), mu=_c(inp['moe_w_up'][l]), md=_c(inp['moe_w_down'][l]), ident=ident)
        maps = [dict(h=hs[c], hT=_c(hs[c].T), yT=_c(ys[c].T), **common) for c in range(NCORES)]
        res = _run(build_post(NTL, 8), maps)
        h = tok_merge(res, "out")
    return np.ascontiguousarray(h.reshape(B, T, D)[:, N_META:]).astype(np.float32)
```

```python
import numpy as np
import concourse.bass as bass
import concourse.mybir as mybir
from concourse.bass_utils import run_bass_kernel_spmd
from contextlib import ExitStack

F32 = mybir.dt.float32
BF16 = mybir.dt.bfloat16
ALU = mybir.AluOpType
AF = mybir.ActivationFunctionType
AX = mybir.AxisListType

D = 1024
DEPTH = 2
N_META = 16
DN_ALPHA = (2 * DEPTH) ** 0.25
LN_EPS = 1e-5
NCORES = 8

ENGS = ('pe', 'act', 'dve', 'pool', 'sp')
N_DMA_SEMS = 16
CC_INC = 1


class Prog:
    def __init__(self, nc):
        self.nc = nc
        self.E = {'pe': nc.tensor, 'act': nc.scalar, 'dve': nc.vector, 'pool': nc.gpsimd, 'sp': nc.sync}
        self.sem = {e: nc.alloc_semaphore("s_" + e) for e in ENGS}
        self.cnt = {e: 0 for e in ENGS}
        self.dsem = [nc.alloc_semaphore("d_%d" % i) for i in range(N_DMA_SEMS)]
        self.dcnt = [0] * N_DMA_SEMS
        self.dnext = 0
        self.known = {e: {} for e in ENGS}
        self.last_w = {}
        self.readers = {}
        self.semobj = {}
        for e in ENGS:
            self.semobj['s_' + e] = self.sem[e]
        for i in range(N_DMA_SEMS):
            self.semobj['d_%d' % i] = self.dsem[i]
        self.n_ins = 0
        self.n_wait = 0
        self._uid = 0
        self.stacks = []

    def sb(self, shape, dt=F32, name=None):
        self._uid += 1
        nm = (name or "t") + "_%d" % self._uid
        if self.stacks:
            return self.stacks[-1].enter_context(self.nc.sbuf_tensor(nm, list(shape), dt)).ap()
        return self.nc.alloc_sbuf_tensor(nm, list(shape), dt).ap()

    def push(self):
        self.stacks.append(ExitStack())

    def pop(self):
        self.barrier()
        self.stacks.pop().close()

    def barrier(self):
        for e in ENGS:
            for f in ENGS:
                if f != e and self.cnt[f] > self.known[e].get('s_' + f, 0):
                    self.E[e].wait_ge(self.sem[f], self.cnt[f])
                    self.known[e]['s_' + f] = self.cnt[f]
            for i in range(N_DMA_SEMS):
                sn = 'd_%d' % i
                if self.dcnt[i] > self.known[e].get(sn, 0):
                    self.E[e].wait_ge(self.dsem[i], self.dcnt[i])
                    self.known[e][sn] = self.dcnt[i]

    def ps(self, shape, dt=F32, name=None):
        self._uid += 1
        return self.nc.alloc_psum_tensor(name or ("p%d" % self._uid), list(shape), dt).ap()

    @staticmethod
    def key_of(x):
        if isinstance(x, (str, tuple)):
            return x
        return x.tensor.name

    def _deps(self, eng, reads, writes):
        need = {}

        def add(sn, v, prod_eng):
            if prod_eng == eng and eng == 'pe':
                return
            if need.get(sn, 0) < v:
                need[sn] = v
        for k in reads:
            w = self.last_w.get(k)
            if w is not None:
                add(*w)
        for k in writes:
            w = self.last_w.get(k)
            if w is not None:
                add(*w)
            for (sn, v, pe) in self.readers.get(k, {}).values():
                if pe == eng:
                    continue
                add(sn, v, pe)
        for sn, v in need.items():
            if self.known[eng].get(sn, 0) < v:
                self.E[eng].wait_ge(self.semobj[sn], v)
                self.known[eng][sn] = v
                self.n_wait += 1

    def _record(self, reads, writes, tok):
        for k in reads:
            self.readers.setdefault(k, {})[tok[0]] = tok
        for k in writes:
            self.last_w[k] = tok
            self.readers[k] = {}

    def op(self, eng, fn, r=(), w=()):
        reads = [self.key_of(x) for x in r]
        writes = [self.key_of(x) for x in w]
        self._deps(eng, reads, writes)
        ins = fn(self.E[eng])
        self.cnt[eng] += 1
        ins.then_inc(self.sem[eng], 1)
        tok = ('s_' + eng, self.cnt[eng], eng)
        self._record(reads, writes, tok)
        self.n_ins += 1
        return ins

    def dma(self, eng, out, in_, r=None, w=None, **kw):
        reads = [self.key_of(x) for x in (r if r is not None else [in_])]
        writes = [self.key_of(x) for x in (w if w is not None else [out])]
        self._deps(eng, reads, writes)
        i = self.dnext
        self.dnext = (self.dnext + 1) % N_DMA_SEMS
        sn = 'd_%d' % i
        if self.known[eng].get(sn, 0) < self.dcnt[i]:
            self.E[eng].wait_ge(self.dsem[i], self.dcnt[i])
            self.known[eng][sn] = self.dcnt[i]
        ins = self.E[eng].dma_start(out=out, in_=in_, **kw)
        self.dcnt[i] += 16
        ins.then_inc(self.dsem[i], 16)
        tok = (sn, self.dcnt[i], 'dma')
        self._record(reads, writes, tok)
        self.n_ins += 1
        return ins

    def coll(self, kind, in_, out, groups):
        eng = 'pool'
        reads = [self.key_of(in_)]
        writes = [self.key_of(out)]
        self._deps(eng, reads, writes)
        i = self.dnext
        self.dnext = (self.dnext + 1) % N_DMA_SEMS
        sn = 'd_%d' % i
        if self.known[eng].get(sn, 0) < self.dcnt[i]:
            self.E[eng].wait_ge(self.dsem[i], self.dcnt[i])
            self.known[eng][sn] = self.dcnt[i]
        ins = self.nc.gpsimd.collective_compute(kind, ALU.bypass, replica_groups=groups, ins=[in_.opt()], outs=[out.opt()])
        self.dcnt[i] += CC_INC
        ins.then_inc(self.dsem[i], CC_INC)
        tok = (sn, self.dcnt[i], 'dma')
        self._record(reads, writes, tok)
        self.n_ins += 1
        return ins

    def finish(self, eng='sp'):
        for i in range(N_DMA_SEMS):
            if self.dcnt[i] > 0:
                self.E[eng].wait_ge(self.dsem[i], self.dcnt[i])
        for e in ENGS:
            if self.cnt[e] > 0 and e != eng:
                self.E[eng].wait_ge(self.sem[e], self.cnt[e])

    def mm(self, out, lhsT, rhs, start=True, stop=True, **kw):
        return self.op('pe', lambda e: e.matmul(out, lhsT, rhs, start=start, stop=stop, **kw),
                       r=[lhsT, rhs], w=[out])

    def tr(self, out, in_, ident):
        return self.op('pe', lambda e: e.transpose(out, in_, ident), r=[in_, ident], w=[out])

    def act(self, out, in_, func, bias=None, scale=1.0, accum_out=None, eng='act'):
        r = [in_]
        w = [out]
        kw = {}
        if bias is not None:
            kw['bias'] = bias
            if not isinstance(bias, (int, float)):
                r.append(bias)
        if not isinstance(scale, (int, float)):
            r.append(scale)
        if accum_out is not None:
            kw['accum_out'] = accum_out
            w.append(accum_out)
        return self.op('act', lambda e: e.activation(out, in_, func, scale=scale, **kw), r=r, w=w)

    def tt(self, eng, out, a, b, op):
        return self.op(eng, lambda e: e.tensor_tensor(out, a, b, op), r=[a, b], w=[out])

    def ts(self, eng, out, a, s1, s2, op0, op1=None, accum_out=None):
        r = [a] + [s for s in (s1, s2) if s is not None and not isinstance(s, (int, float))]
        w = [out] + ([accum_out] if accum_out is not None else [])
        kw = {}
        if accum_out is not None:
            kw['accum_out'] = accum_out
        if op1 is None:
            return self.op(eng, lambda e: e.tensor_scalar(out, a, s1, None, op0, **kw), r=r, w=w)
        return self.op(eng, lambda e: e.tensor_scalar(out, a, s1, s2, op0, op1, **kw), r=r, w=w)

    def stt(self, out, a, s, b, op0, op1, accum_out=None):
        r = [a, b] + ([s] if not isinstance(s, (int, float)) else [])
        w = [out] + ([accum_out] if accum_out is not None else [])
        kw = {}
        if accum_out is not None:
            kw['accum_out'] = accum_out
        return self.op('dve', lambda e: e.scalar_tensor_tensor(out, a, s, b, op0, op1, **kw), r=r, w=w)

    def copy(self, eng, out, in_):
        if eng == 'act':
            return self.op('act', lambda e: e.copy(out, in_), r=[in_], w=[out])
        return self.op(eng, lambda e: e.tensor_copy(out, in_), r=[in_], w=[out])

    def memset(self, eng, out, v):
        return self.op(eng, lambda e: e.memset(out, v), w=[out])


class Ctx:
    def __init__(self, nc, p, PS, prefix="", over=None):
        self.nc, self.p, self.PS, self.prefix, self.over = nc, p, PS, prefix, dict(over or {})

    def inp(self, n, s):
        if n in self.over:
            return self.over[n]
        return self.nc.dram_tensor(self.prefix + n, list(s), F32, kind="ExternalInput").ap()

    def out(self, n, s):
        if n in self.over:
            return self.over[n]
        return self.nc.dram_tensor(self.prefix + n, list(s), F32, kind="ExternalOutput").ap()

    def scratch(self, n, s, dt=F32):
        return self.nc.dram_tensor(self.prefix + n, list(s), dt, kind="Internal").ap()


def new_prog():
    nc = bass.Bass("TRN2", target_bir_lowering=False)
    p = Prog(nc)
    PS = [p.ps([128, 512], name="bank%d" % i) for i in range(8)]
    return nc, p, PS


def standalone(emit, *a, **k):
    nc, p, PS = new_prog()
    emit(Ctx(nc, p, PS), *a, **k)
    p.finish()
    return nc


def emit_transposed(p, PS, src, dstT_d, cols, ident, tmpT):
    for c in range(8):
        pt = PS[4 + c // 4]
        p.tr(pt[:, (c % 4) * 128:(c % 4 + 1) * 128], src[:, c * 128:(c + 1) * 128], ident)
        if c % 4 == 3:
            p.copy('act', tmpT[:, c - 3:c + 1, :], pt.rearrange("p (a n) -> p a n", a=4))
    p.dma('sp', dstT_d.rearrange("(c f) n -> f c n", f=128)[:, :, cols], tmpT)


def emit_ln(p, xt, out, G, Bt, tmp, eps):
    st, mv, rs = tmp
    for c in range(2):
        p.op('dve', lambda e: e.bn_stats(st[:, c * 6:(c + 1) * 6], xt[:, c * 512:(c + 1) * 512]), r=[xt], w=[st])
    p.op('dve', lambda e: e.bn_aggr(mv, st.rearrange("p (c s) -> p c s", s=6)), r=[st], w=[mv])
    p.act(rs, mv[:, 1:2], AF.Sqrt, bias=eps)
    p.op('dve', lambda e: e.reciprocal(rs, rs), r=[rs], w=[rs])
    p.ts('dve', out, xt, mv[:, 0:1], rs[:, 0:1], ALU.subtract, ALU.mult)
    p.tt('pool', out, out, G, ALU.mult)
    p.tt('pool', out, out, Bt, ALU.add)


def load_cast(p, dst, src, stage, chunk_cols, engs=('pool', 'act')):
    a, n = dst.shape[1], dst.shape[2]
    chunk_cols = min(chunk_cols, stage[0].shape[1])
    k = 0
    for i in range(a):
        for c0 in range(0, n, chunk_cols):
            c1 = min(n, c0 + chunk_cols)
            stg = stage[k % len(stage)]
            p.dma('sp', stg[:, 0:c1 - c0], src[:, i, c0:c1])
            p.copy(engs[k % len(engs)], dst[:, i, c0:c1], stg[:, 0:c1 - c0])
            k += 1


def emit_ln_in(cx, NTL):
    nc, p, PS = cx.nc, cx.p, cx.PS
    x = cx.inp("x", [NTL * 128, D]); g = cx.inp("g", [D]); b = cx.inp("b", [D])
    y = cx.out("y", [NTL * 128, D])
    hT_out = cx.over.get("hT_out")
    p.push()
    G = p.sb([128, D]); Bt = p.sb([128, D]); eps = p.sb([128, 1])
    p.memset('dve', eps, LN_EPS)
    p.dma('sp', G, g.partition_broadcast(128))
    p.dma('sp', Bt, b.partition_broadcast(128))
    xts = [p.sb([128, D]) for _ in range(2)]
    ots = [p.sb([128, D]) for _ in range(2)]
    tmp = (p.sb([128, 12]), p.sb([128, 2]), p.sb([128, 1]))
    if hT_out is not None:
        ident = p.sb([128, 128]); p.dma('sp', ident, cx.inp("ident", [128, 128]))
        tmpT = p.sb([128, 8, 128])
    for t in range(NTL):
        xt = xts[t % 2]; ot = ots[t % 2]
        p.dma('sp', xt, x[t * 128:(t + 1) * 128, :])
        emit_ln(p, xt, ot, G, Bt, tmp, eps)
        p.dma('sp', y[t * 128:(t + 1) * 128, :], ot)
        if hT_out is not None:
            emit_transposed(p, PS, ot, hT_out, slice(t * 128, (t + 1) * 128), ident, tmpT)
    p.pop()


def build_ln_in(NTL):
    return standalone(emit_ln_in, NTL)


def emit_post(cx, NTL, GT=8):
    nc, p, PS = cx.nc, cx.p, cx.PS
    NTOK = NTL * 128
    di = cx.inp
    h_d = di("h", [NTOK, D]); hT_d = di("hT", [D, NTOK])
    fused = "G_abd" in cx.over
    if fused:
        G_abd, G_c, rk_d = cx.over["G_abd"], cx.over["G_c"], cx.over["rk"]
        PWy, HALF, NSLOT = cx.over["PWy"], cx.over["HALF"], cx.over["NSLOT"]
    else:
        yT_d = di("yT", [D, NTOK])
    hT_out = cx.over.get("hT_out")
    wg_d = di("wg", [D, 4 * D]); wb_d = di("wb", [D, D]); wo_d = di("wo", [D, D])
    ln1g = di("ln1g", [D]); ln1b = di("ln1b", [D]); ln2g = di("ln2g", [D]); ln2b = di("ln2b", [D])
    wr_d = di("wr", [D, 20]); br_d = di("br", [20])
    mg_d = di("mg", [16, D, 256]); mu_d = di("mu", [16, D, 256]); md_d = di("md", [16, 256, D])
    id_d = di("ident", [128, 128])
    out_d = cx.out("out", [NTOK, D])
    h1_d = cx.scratch("h1s", [NTOK, D], F32)
    xT_d = cx.scratch("xTs", [128, 8, NTOK], BF16)
    p.push()
    ident = p.sb([128, 128]); p.dma('sp', ident, id_d)
    eps = p.sb([128, 1]); p.memset('dve', eps, LN_EPS)
    G1 = p.sb([128, D]); B1 = p.sb([128, D]); G2 = p.sb([128, D]); B2 = p.sb([128, D])
    for tdst, src in ((G1, ln1g), (B1, ln1b), (G2, ln2g), (B2, ln2b)):
        p.dma('sp', tdst, src.partition_broadcast(128))
    Wr = p.sb([128, 8, 20]); p.dma('sp', Wr, wr_d.rearrange("(c f) n -> f c n", f=128))
    Br = p.sb([128, 20]); p.dma('sp', Br, br_d.partition_broadcast(128))
    gate_all = p.sb([128, NTL, 16])
    lntmp = (p.sb([128, 12]), p.sb([128, 2]), p.sb([128, 1]))
    stage = [p.sb([128, 2048], name="stg%d" % i) for i in range(2)]

    p.push()
    Wg = p.sb([128, 8, 4 * D], BF16, name="Wg")
    Wb = p.sb([128, 8, D], BF16, name="Wb")
    Wo = p.sb([128, 8, D], BF16, name="Wo")
    load_cast(p, Wg, wg_d.rearrange("(c f) n -> f c n", f=128), stage, 2048)
    load_cast(p, Wb, wb_d.rearrange("(c f) n -> f c n", f=128), stage, 2048)
    load_cast(p, Wo, wo_d.rearrange("(c f) n -> f c n", f=128), stage, 2048)
    hTf = p.sb([128, 8, 128]); hTb = p.sb([128, 8, 128], BF16)
    yTf = p.sb([128, 8, 128]); yTb = p.sb([128, 8, 128], BF16)
    if fused:
        rk = p.sb([128, 2], name="rk"); p.dma('sp', rk, rk_d)
        ycand = [p.sb([128, D], name="ycand%d" % i) for i in range(2)]
        ytile = p.sb([128, D], name="ytile")
    ht = p.sb([128, D]); gsb = [p.sb([128, 512]) for _ in range(2)]
    merged = p.sb([128, D]); tmpm = p.sb([128, 512])
    mTb = p.sb([128, 8, 128], BF16)
    pre = p.sb([128, D]); h1 = p.sb([128, D])
    x32 = p.sb([128, 8, 128]); xb = p.sb([128, 8, 128], BF16)
    lg = p.sb([128, 20]); sm = p.sb([128, 16], name="smallr")
    gm4 = p.sb([128, 4]); ge4 = p.sb([128, 4]); em = p.sb([128, 16]); elm = p.sb([128, 16])
    top8 = p.sb([128, 8]); g0 = p.sb([128, 16]); g1 = p.sb([128, 16])
    for t in range(NTL):
        ts_ = slice(t * 128, (t + 1) * 128)
        p.dma('sp', hTf, hT_d.rearrange("(c f) n -> f c n", f=128)[:, :, ts_])
        p.dma('sp', ht, h_d[ts_, :])
        p.copy('pool', hTb, hTf)
        if not fused:
            p.dma('sp', yTf, yT_d.rearrange("(c f) n -> f c n", f=128)[:, :, ts_])
            p.copy('pool', yTb, yTf)
        else:
            for rc in range(2):
                r0 = 48 + HALF * rc + t * 128
                yc = ycand[rc].rearrange("p (m q c) -> p m q c", m=4, q=2)
                for q in range(2):
                    src = G_abd.rows(q, r0, 128).rearrange("p (m c) -> p m c", m=3)
                    p.dma('sp', yc[:, 0:2, q, :], src[:, 0:2, :])
                    p.dma('sp', yc[:, 3, q, :], src[:, 2, :])
                slot = (HALF // 256) * rc + t // 2
                if slot < NSLOT:
                    p.dma('sp', ycand[rc][:, 512:768], G_c.rows(t % 2, slot * 128, 128))
                else:
                    p.memset('pool', ycand[rc][:, 512:768], 0.0)
            p.ts('dve', ytile, ycand[0], rk[:, 0:1], None, ALU.mult)
            p.stt(ytile, ycand[1], rk[:, 1:2], ytile, ALU.mult, ALU.add)
            for c in range(8):
                pt = PS[4 + c // 4]
                p.tr(pt[:, (c % 4) * 128:(c % 4 + 1) * 128], ytile[:, c * 128:(c + 1) * 128], ident)
                if c % 4 == 3:
                    p.copy('act', yTb[:, c - 3:c + 1, :], pt.rearrange("p (a n) -> p a n", a=4))
        for i in range(4):
            for hf in range(2):
                pg = PS[hf]; py = PS[2 + hf]
                for c in range(8):
                    p.mm(pg, hTb[:, c, :], Wg[:, c, i * D + hf * 512: i * D + hf * 512 + 512], start=(c == 0), stop=(c == 7))
                p.act(gsb[hf], pg, AF.Sigmoid)
                for c in range(2):
                    p.mm(py, yTb[:, 2 * i + c, :], Wb[:, 2 * i + c, hf * 512:(hf + 1) * 512], start=(c == 0), stop=(c == 1))
                if i == 0:
                    p.tt('dve', merged[:, hf * 512:(hf + 1) * 512], gsb[hf], py, ALU.mult)
                else:
                    p.tt('dve', tmpm, gsb[hf], py, ALU.mult)
                    p.tt('pool', merged[:, hf * 512:(hf + 1) * 512], merged[:, hf * 512:(hf + 1) * 512], tmpm, ALU.add)
        for c in range(8):
            pt = PS[4 + c // 4]
            p.tr(pt[:, (c % 4) * 128:(c % 4 + 1) * 128], merged[:, c * 128:(c + 1) * 128], ident)
            if c % 4 == 3:
                p.copy('act', mTb[:, c - 3:c + 1, :], pt.rearrange("p (a n) -> p a n", a=4))
        for hf in range(2):
            po = PS[6 + hf]
            for c in range(8):
                p.mm(po, mTb[:, c, :], Wo[:, c, hf * 512:(hf + 1) * 512], start=(c == 0), stop=(c == 7))
            p.stt(pre[:, hf * 512:(hf + 1) * 512], ht[:, hf * 512:(hf + 1) * 512], DN_ALPHA, po, ALU.mult, ALU.add)
        emit_ln(p, pre, h1, G1, B1, lntmp, eps)
        p.dma('sp', h1_d[ts_, :], h1)
        for c in range(8):
            pt = PS[4 + c // 4]
            p.tr(pt[:, (c % 4) * 128:(c % 4 + 1) * 128], h1[:, c * 128:(c + 1) * 128], ident)
            if c % 4 == 3:
                p.copy('act', x32[:, c - 3:c + 1, :], pt.rearrange("p (a n) -> p a n", a=4))
        p.copy('pool', xb, x32)
        p.dma('sp', xT_d[:, :, ts_], xb)
        pr = PS[0]
        for c in range(8):
            p.mm(pr[:, 0:20], x32[:, c, :], Wr[:, c, :], start=(c == 0), stop=(c == 7))
        p.tt('dve', lg, pr[:, 0:20], Br, ALU.add)
        p.op('dve', lambda e: e.reduce_max(sm[:, 0:1], lg[:, 0:4], AX.X), r=[lg], w=[sm])
        p.ts('dve', sm[:, 1:2], sm[:, 0:1], -1.0, None, ALU.mult)
        p.act(ge4, lg[:, 0:4], AF.Exp, bias=sm[:, 1:2], accum_out=sm[:, 2:3])
        p.op('dve', lambda e: e.reciprocal(sm[:, 3:4], sm[:, 2:3]), r=[sm], w=[sm])
        p.ts('dve', gm4, lg[:, 0:4], sm[:, 0:1], None, ALU.is_ge)
        p.copy('dve', em.rearrange("p (g e) -> p g e", e=4), gm4.unsqueeze(2).to_broadcast([128, 4, 4]))
        p.ts('dve', em, em, 1e30, -1e30, ALU.mult, ALU.add)
        p.tt('dve', elm, lg[:, 4:20], em, ALU.add)
        p.op('dve', lambda e: e.max(top8, elm), r=[elm], w=[top8])
        p.tt('dve', sm[:, 4:5], top8[:, 1:2], top8[:, 0:1], ALU.subtract)
        p.act(sm[:, 5:6], sm[:, 4:5], AF.Sigmoid)
        p.ts('dve', sm[:, 6:7], sm[:, 5:6], -1.0, 1.0, ALU.mult, ALU.add)
        p.tt('dve', sm[:, 7:8], sm[:, 6:7], sm[:, 3:4], ALU.mult)
        p.tt('dve', sm[:, 8:9], sm[:, 5:6], sm[:, 3:4], ALU.mult)
        p.ts('dve', g0, elm, top8[:, 0:1], sm[:, 7:8], ALU.is_equal, ALU.mult)
        p.ts('dve', g1, elm, top8[:, 1:2], sm[:, 8:9], ALU.is_equal, ALU.mult)
        p.tt('dve', gate_all[:, t, :], g0, g1, ALU.add)

    p.pop()
    p.push()
    We_g = [p.sb([128, 8, 256], BF16, name="Weg%d" % i) for i in range(2)]
    We_u = [p.sb([128, 8, 256], BF16, name="Weu%d" % i) for i in range(2)]
    We_d = [p.sb([128, 2, D], BF16, name="Wed%d" % i) for i in range(2)]
    xg = p.sb([128, 8, GT * 128], BF16, name="xg")
    yacc = p.sb([128, GT, D], name="yacc")
    sg = [p.sb([128, 512], name="sg%d" % i) for i in range(2)]
    hid = [p.sb([128, 512], BF16, name="hid%d" % i) for i in range(2)]
    h1t = p.sb([128, D], name="h1t"); pre2 = p.sb([128, D], name="pre2"); o2 = p.sb([128, D], name="o2")
    tmpT2 = p.sb([128, 8, 128], name="tmpT2")
    k = 0
    for g0_ in range(0, NTL, GT):
        ntg = min(GT, NTL - g0_)
        p.dma('sp', xg[:, :, 0:ntg * 128], xT_d[:, :, g0_ * 128:(g0_ + ntg) * 128])
        for e_ in range(16):
            wgt, wut, wdt = We_g[k % 2], We_u[k % 2], We_d[k % 2]
            k += 1
            load_cast(p, wgt, mg_d[e_].rearrange("(c f) n -> f c n", f=128), stage, 2048)
            load_cast(p, wut, mu_d[e_].rearrange("(c f) n -> f c n", f=128), stage, 2048)
            load_cast(p, wdt, md_d[e_].rearrange("(c f) n -> f c n", f=128), stage, 2048)
            for tb in range(0, ntg, 4):
                nb = min(4, ntg - tb)
                ncol = nb * 128
                cs = slice(tb * 128, tb * 128 + ncol)
                for j in range(2):
                    pa = PS[j * 2]; pb = PS[j * 2 + 1]
                    for c in range(8):
                        p.mm(pa[:, 0:ncol], wgt[:, c, j * 128:(j + 1) * 128], xg[:, c, cs], start=(c == 0), stop=(c == 7))
                    for c in range(8):
                        p.mm(pb[:, 0:ncol], wut[:, c, j * 128:(j + 1) * 128], xg[:, c, cs], start=(c == 0), stop=(c == 7))
                    p.act(sg[j][:, 0:ncol], pa[:, 0:ncol], AF.Silu)
                    p.tt('dve', hid[j][:, 0:ncol], sg[j][:, 0:ncol], pb[:, 0:ncol], ALU.mult)
                for tt_ in range(nb):
                    tile_i = tb + tt_
                    for hf in range(2):
                        pd = PS[4 + (tt_ * 2 + hf) % 4]
                        for j in range(2):
                            p.mm(pd, hid[j][:, tt_ * 128:(tt_ + 1) * 128], wdt[:, j, hf * 512:(hf + 1) * 512], start=(j == 0), stop=(j == 1))
                        ya = yacc[:, tile_i, hf * 512:(hf + 1) * 512]
                        gcol = gate_all[:, g0_ + tile_i, e_:e_ + 1]
                        if e_ == 0:
                            p.ts('dve', ya, pd, gcol, None, ALU.mult)
                        else:
                            p.stt(ya, pd, gcol, ya, ALU.mult, ALU.add)
        for tt_ in range(ntg):
            ts_ = slice((g0_ + tt_) * 128, (g0_ + tt_ + 1) * 128)
            p.dma('sp', h1t, h1_d[ts_, :])
            p.stt(pre2, h1t, DN_ALPHA, yacc[:, tt_, :], ALU.mult, ALU.add)
            emit_ln(p, pre2, o2, G2, B2, lntmp, eps)
            p.dma('sp', out_d[ts_, :], o2)
            if hT_out is not None:
                emit_transposed(p, PS, o2, hT_out, ts_, ident, tmpT2)
    p.pop()
    p.pop()


def build_post(NTL, GT=8):
    return standalone(emit_post, NTL, GT)


def load_w_bf16(p, src_d, ncols, stage, name):
    W = p.sb([128, 8, ncols], BF16, name=name)
    load_cast(p, W, src_d.rearrange("(c f) n -> f c n", f=128), stage, 2048)
    return W


def load_hblock(p, hT_d, pos0, npos, hTf, hTb):
    p.dma('sp', hTf[:, :, 0:npos], hT_d.rearrange("(c f) n -> f c n", f=128)[:, :, pos0:pos0 + npos])
    p.copy('pool', hTb[:, :, 0:npos], hTf[:, :, 0:npos])


def proj_fm(p, ps_out, W, c0, ncols, hTb, n0, npos):
    for c in range(8):
        p.mm(ps_out, W[:, c, c0:c0 + ncols], hTb[:, c, n0:n0 + npos], start=(c == 0), stop=(c == 7))


def proj_tm(p, ps_out, W, c0, ncols, hTb, n0, npos):
    for c in range(8):
        p.mm(ps_out, hTb[:, c, n0:n0 + npos], W[:, c, c0:c0 + ncols], start=(c == 0), stop=(c == 7))


def emit_gla(cx, NCH, stop_at=99):
    nc, p, PS = cx.nc, cx.p, cx.PS
    P_ = NCH * 64
    BLK = 512
    di = cx.inp
    hT_d = di("hT", [D, P_])
    wqk_d = di("wqk", [D, 128]); wvo_d = di("wvo", [D, 256]); wa_d = di("wa", [D, 16])
    aup_d = di("aup", [16, 64]); ab_d = di("ab", [64, 1]); ng_d = di("ng", [128])
    cm_d = di("cmask", [P_]); mu_d = di("maskU2", [64, 128]); id_d = di("ident", [128, 128])
    y_d = cx.out("y", [P_, 128])
    p.push()
    stage = [p.sb([128, 2048], name="stg%d" % i) for i in range(2)]
    Wqk = load_w_bf16(p, wqk_d, 128, stage, "Wqk")
    Wvo = load_w_bf16(p, wvo_d, 256, stage, "Wvo")
    Wa = load_w_bf16(p, wa_d, 16, stage, "Wa")
    aup = p.sb([16, 64]); p.dma('sp', aup, aup_d)
    ab = [p.sb([32, 1]) for _ in range(2)]
    for h in range(2):
        p.dma('sp', ab[h], ab_d[h * 32:(h + 1) * 32, :])
    ng = p.sb([64, 128]); p.dma('sp', ng, ng_d.partition_broadcast(64))
    maskU = p.sb([64, 128]); p.dma('sp', maskU, mu_d)
    ident = p.sb([128, 128]); p.dma('sp', ident, id_d)
    eps6 = p.sb([64, 1]); p.memset('dve', eps6, 1e-6)
    S = [p.sb([32, 64], name="S%d" % h) for h in range(2)]
    for h in range(2):
        p.memset('dve', S[h], 0.0)
    hTf = p.sb([128, 8, BLK]); hTb = p.sb([128, 8, BLK], BF16)
    cm = p.sb([32, BLK]); xa = p.sb([16, BLK])
    mk = lambda nm: [p.sb([32, BLK], name=nm + str(h)) for h in range(2)]
    qT, kT, la, bT, eb, enb, ekb, qg, kg, ku = (mk(n) for n in ("qT", "kT", "la", "bT", "eb", "enb", "ekb", "qg", "kg", "ku"))
    dec = [p.sb([32, BLK // 64], name="dec%d" % h) for h in range(2)]
    vo = p.sb([64, 256]); sgo = p.sb([64, 128]); kut = p.sb([64, 64]); attm = p.sb([64, 128])
    o_sb = p.sb([64, 128]); sq = p.sb([64, 128]); ms = p.sb([64, 2]); yt = p.sb([64, 128])
    for b0 in range(0, P_, BLK):
        nb = min(BLK, P_ - b0)
        nch = nb // 64
        load_hblock(p, hT_d, b0, nb, hTf, hTb)
        p.dma('sp', cm[:, 0:nb], cm_d[b0:b0 + nb].partition_broadcast(32))
        proj_fm(p, PS[2][0:16, 0:nb], Wa, 0, 16, hTb, 0, nb)
        p.copy('act', xa[:, 0:nb], PS[2][0:16, 0:nb])
        for h in range(2):
            sl = (slice(None), slice(0, nb))
            proj_fm(p, PS[0][0:32, 0:nb], Wqk, h * 32, 32, hTb, 0, nb)
            p.op('act', lambda e: e.mul(qT[h][sl], PS[0][0:32, 0:nb], 32 ** -0.5), r=[PS[0]], w=[qT[h]])
            proj_fm(p, PS[1][0:32, 0:nb], Wqk, 64 + h * 32, 32, hTb, 0, nb)
            p.copy('act', kT[h][sl], PS[1][0:32, 0:nb])
            p.mm(PS[3][0:32, 0:nb], aup[:, h * 32:(h + 1) * 32], xa[:, 0:nb])
            p.act(la[h][sl], PS[3][0:32, 0:nb], AF.Sigmoid, bias=ab[h])
            p.act(la[h][sl], la[h][sl], AF.Ln)
            p.ts('pool', la[h][sl], la[h][sl], 1.0 / 16.0, None, ALU.mult)
            p.op('dve', lambda e: e.tensor_tensor_scan(bT[h][sl], cm[:, 0:nb], la[h][sl], 0.0, ALU.mult, ALU.add),
                 r=[cm, la[h]], w=[bT[h]])
            p.act(eb[h][sl], bT[h][sl], AF.Exp)
            p.act(enb[h][sl], bT[h][sl], AF.Exp, scale=-1.0)
            b3 = bT[h][sl].rearrange("p (c l) -> p c l", l=64)
            p.tt('dve', ekb[h][sl].rearrange("p (c l) -> p c l", l=64), b3[:, :, 63:64].to_broadcast([32, nch, 64]), b3, ALU.subtract)
            p.act(ekb[h][sl], ekb[h][sl], AF.Exp)
            p.act(dec[h][:, 0:nch], b3[:, :, 63], AF.Exp)
            p.tt('dve', qg[h][sl], qT[h][sl], eb[h][sl], ALU.mult)
            p.tt('pool', kg[h][sl], kT[h][sl], enb[h][sl], ALU.mult)
            p.tt('pool', ku[h][sl], kT[h][sl], ekb[h][sl], ALU.mult)
        for ci in range(nch):
            cs = slice(ci * 64, ci * 64 + 64)
            proj_tm(p, PS[4][0:64, 0:256], Wvo, 0, 256, hTb, ci * 64, 64)
            p.copy('act', vo[:, 0:128], PS[4][0:64, 0:128])
            p.act(sgo, PS[4][0:64, 128:256], AF.Silu)
            for h in range(2):
                p.tr(PS[5][0:64, h * 32:h * 32 + 32], ku[h][:, cs], ident[0:32, 0:32])
            p.copy('act', kut, PS[5][0:64, 0:64])
            for h in range(2):
                p.mm(PS[6][0:64, h * 64:h * 64 + 64], kg[h][:, cs], qg[h][:, cs])
            p.tt('dve', attm, PS[6][0:64, 0:128], maskU, ALU.mult)
            for h in range(2):
                p.mm(PS[7][0:64, h * 64:h * 64 + 64], attm[:, h * 64:h * 64 + 64], vo[:, h * 64:h * 64 + 64], start=True, stop=False)
                p.mm(PS[7][0:64, h * 64:h * 64 + 64], qg[h][:, cs], S[h], start=False, stop=True)
            for h in range(2):
                p.mm(PS[5][0:32, 128 + h * 64:128 + h * 64 + 64], kut[:, h * 32:h * 32 + 32], vo[:, h * 64:h * 64 + 64])
                p.stt(S[h], S[h], dec[h][:, ci:ci + 1], PS[5][0:32, 128 + h * 64:128 + h * 64 + 64], ALU.mult, ALU.add)
            p.copy('act', o_sb, PS[7][0:64, 0:128])
            p.tt('dve', sq, o_sb, o_sb, ALU.mult)
            p.op('dve', lambda e: e.reduce_sum(ms, sq.rearrange("p (h d) -> p h d", d=64), AX.X), r=[sq], w=[ms])
            p.act(ms, ms, AF.Sqrt, bias=eps6, scale=1.0 / 64.0)
            p.op('dve', lambda e: e.reciprocal(ms, ms), r=[ms], w=[ms])
            p.tt('dve', yt.rearrange("p (h d) -> p h d", d=64), o_sb.rearrange("p (h d) -> p h d", d=64),
                 ms.unsqueeze(2).to_broadcast([64, 2, 64]), ALU.mult)
            p.tt('pool', yt, yt, ng, ALU.mult)
            p.tt('pool', yt, yt, sgo, ALU.mult)
            p.dma('sp', y_d[b0 + ci * 64:b0 + ci * 64 + 64, :], yt)
    p.pop()


def build_gla(NCH, stop_at=99):
    return standalone(emit_gla, NCH, stop_at)


def emit_mlstm(cx, NCH):
    nc, p, PS = cx.nc, cx.p, cx.PS
    P_ = NCH * 64
    BLK = 512
    di = cx.inp
    hT_d = di("hT", [D, P_])
    wq_d = di("wq", [D, 128]); wk_d = di("wk", [D, 128]); wvo_d = di("wvo", [D, 256])
    wi_d = di("wi", [D, 128]); wf_d = di("wf", [D, 128])
    cwq_d = di("cwq", [128, 4]); cwk_d = di("cwk", [128, 4]); cbq_d = di("cbq", [128, 1]); cbk_d = di("cbk", [128, 1])
    ib_d = di("ib", [128, 1]); fb_d = di("fb", [128, 1]); ng_d = di("ng", [128])
    cm_d = di("cmask", [P_]); pm_d = di("pm01", [P_]); pn_d = di("pmneg", [P_])
    ml_d = di("maskL", [64, 64]); id_d = di("ident", [128, 128])
    y_d = cx.out("y", [P_, 128])
    p.push()
    stage = [p.sb([128, 2048], name="stg%d" % i) for i in range(2)]
    Wq = load_w_bf16(p, wq_d, 128, stage, "Wq"); Wk = load_w_bf16(p, wk_d, 128, stage, "Wk")
    Wvo = load_w_bf16(p, wvo_d, 256, stage, "Wvo")
    Wi = load_w_bf16(p, wi_d, 128, stage, "Wi"); Wf = load_w_bf16(p, wf_d, 128, stage, "Wf")
    def ld(src, shape, nm):
        t = p.sb(shape, name=nm); p.dma('sp', t, src); return t
    cw = {}
    for h in range(2):
        hs = slice(h * 64, h * 64 + 64)
        cw['q', h] = (ld(cwq_d[hs, :], [64, 4], "cwq%d" % h), ld(cbq_d[hs, :], [64, 1], "cbq%d" % h))
        cw['k', h] = (ld(cwk_d[hs, :], [64, 4], "cwk%d" % h), ld(cbk_d[hs, :], [64, 1], "cbk%d" % h))
    ib = [ld(ib_d[h * 64:h * 64 + 64, :], [64, 1], "ib%d" % h) for h in range(2)]
    fb = [ld(fb_d[h * 64:h * 64 + 64, :], [64, 1], "fb%d" % h) for h in range(2)]
    ng = p.sb([64, 128]); p.dma('sp', ng, ng_d.partition_broadcast(64))
    maskL = ld(ml_d, [64, 64], "maskL"); ident = ld(id_d, [128, 128], "ident")
    eps5 = p.sb([64, 1]); p.memset('dve', eps5, 1e-5)
    Cst = [p.sb([64, 65], name="C%d" % h) for h in range(2)]
    mst = [p.sb([64, 1], name="m%d" % h) for h in range(2)]
    for h in range(2):
        p.memset('dve', Cst[h], 0.0); p.memset('dve', mst[h], 0.0)
    hTf = p.sb([128, 8, BLK]); hTb = p.sb([128, 8, BLK], BF16)
    cm = p.sb([64, BLK]); pm = p.sb([64, BLK]); pn = p.sb([64, BLK])
    mk = lambda nm, w=BLK: [p.sb([64, w], name=nm + str(h)) for h in range(2)]
    qpre = mk("qpre", BLK + 3); kpre = mk("kpre", BLK + 3)
    for h in range(2):
        p.memset('dve', qpre[h], 0.0); p.memset('dve', kpre[h], 0.0)
    acc = mk("acc"); qT = mk("qT"); kT = mk("kT"); liR = mk("liR"); lfR = mk("lfR"); bR = mk("bR"); gR = mk("gR")
    vo1 = p.sb([64, 2, 65]); sgo = p.sb([64, 128]); hh = p.sb([64, 128]); yt = p.sb([64, 128])
    for h in range(2):
        p.memset('dve', vo1[:, h, 64:65], 1.0)
    bcol = p.sb([64, 1]); dl = p.sb([64, 64]); sm = p.sb([64, 12], name="msm"); sw = p.sb([64, 64]); swT = p.sb([64, 64])
    t2 = p.sb([64, 65]); nd = p.sb([64, 65]); wl = p.sb([64, 64]); kw = p.sb([64, 64]); kwt = p.sb([64, 64]); cl = p.sb([64, 65])
    bst = p.sb([64, 6]); bmv = p.sb([64, 2])
    for b0 in range(0, P_, BLK):
        nb = min(BLK, P_ - b0)
        nch = nb // 64
        sl = (slice(None), slice(0, nb))
        load_hblock(p, hT_d, b0, nb, hTf, hTb)
        for tdst, src in ((cm, cm_d), (pm, pm_d), (pn, pn_d)):
            p.dma('sp', tdst[:, 0:nb], src[b0:b0 + nb].partition_broadcast(64))
        for h in range(2):
            for (W, pre, outT, key, scl) in ((Wq, qpre[h], qT[h], 'q', 1.0), (Wk, kpre[h], kT[h], 'k', 0.125)):
                proj_fm(p, PS[0][0:64, 0:nb], W, h * 64, 64, hTb, 0, nb)
                p.copy('act', pre[:, 3:3 + nb], PS[0][0:64, 0:nb])
                cwt, cbt = cw[key, h]
                p.ts('dve', acc[h][sl], pre[:, 3:3 + nb], cwt[:, 3:4], None, ALU.mult)
                for j in range(3):
                    p.stt(acc[h][sl], pre[:, j:j + nb], cwt[:, j:j + 1], acc[h][sl], ALU.mult, ALU.add)
                p.act(outT[sl], acc[h][sl], AF.Silu, bias=cbt)
                if scl != 1.0:
                    p.ts('pool', outT[sl], outT[sl], scl, None, ALU.mult)
                p.copy('pool', pre[:, 0:3], pre[:, nb:nb + 3])
            proj_fm(p, PS[1][0:64, 0:nb], Wi, h * 64, 64, hTb, 0, nb)
            p.stt(liR[h][sl], PS[1][0:64, 0:nb], ib[h], pn[:, 0:nb], ALU.add, ALU.add)
            proj_fm(p, PS[2][0:64, 0:nb], Wf, h * 64, 64, hTb, 0, nb)
            p.act(lfR[h][sl], PS[2][0:64, 0:nb], AF.Sigmoid, bias=fb[h])
            p.act(lfR[h][sl], lfR[h][sl], AF.Ln)
            p.tt('pool', lfR[h][sl], lfR[h][sl], pm[:, 0:nb], ALU.mult)
            p.op('dve', lambda e: e.tensor_tensor_scan(bR[h][sl], cm[:, 0:nb], lfR[h][sl], 0.0, ALU.mult, ALU.add),
                 r=[cm, lfR[h]], w=[bR[h]])
            p.tt('dve', gR[h][sl], liR[h][sl], bR[h][sl], ALU.subtract)
        for ci in range(nch):
            cs = slice(ci * 64, ci * 64 + 64)
            proj_tm(p, PS[3][0:64, 0:256], Wvo, 0, 256, hTb, ci * 64, 64)
            p.copy('act', vo1[:, :, 0:64], PS[3][0:64, 0:128].rearrange("p (h d) -> p h d", d=64))
            p.act(sgo, PS[3][0:64, 128:256], AF.Sigmoid)
            for h in range(2):
                C = Cst[h]; m = mst[h]
                p.tr(PS[4][0:64, 0:64], bR[h][:, cs], ident[0:64, 0:64])
                p.copy('act', bcol, PS[4][0:64, 0:1])
                p.stt(dl, gR[h][:, cs], bcol, maskL, ALU.add, ALU.add)
                p.op('dve', lambda e: e.reduce_max(sm[:, 0:1], dl, AX.X), r=[dl], w=[sm])
                p.tt('dve', sm[:, 1:2], bcol, m, ALU.add)
                p.tt('dve', sm[:, 2:3], sm[:, 0:1], sm[:, 1:2], ALU.max)
                p.ts('dve', sm[:, 3:4], sm[:, 2:3], -1.0, None, ALU.mult)
                p.act(sw, dl, AF.Exp, bias=sm[:, 3:4])
                p.mm(PS[5][0:64, 0:64], qT[h][:, cs], kT[h][:, cs])
                p.tt('dve', sw, sw, PS[5][0:64, 0:64], ALU.mult)
                p.tr(PS[4][0:64, 64:128], sw, ident[0:64, 0:64])
                p.copy('act', swT, PS[4][0:64, 64:128])
                p.mm(PS[6][0:64, 0:65], swT, vo1[:, h, :])
                p.mm(PS[6][0:64, 128:193], qT[h][:, cs], C)
                p.tt('dve', sm[:, 4:5], sm[:, 1:2], sm[:, 2:3], ALU.subtract)
                p.act(sm[:, 5:6], sm[:, 4:5], AF.Exp)
                p.act(t2, PS[6][0:64, 128:193], AF.Copy, scale=sm[:, 5:6])
                p.tt('dve', nd, PS[6][0:64, 0:65], t2, ALU.add)
                p.ts('dve', sm[:, 6:7], nd[:, 64:65], -1.0, None, ALU.mult)
                p.tt('dve', sm[:, 6:7], sm[:, 6:7], nd[:, 64:65], ALU.max)
                p.act(sm[:, 7:8], sm[:, 2:3], AF.Exp, scale=-1.0)
                p.tt('dve', sm[:, 8:9], sm[:, 6:7], sm[:, 7:8], ALU.max)
                p.op('dve', lambda e: e.reciprocal(sm[:, 9:10], sm[:, 8:9]), r=[sm], w=[sm])
                p.ts('dve', hh[:, h * 64:h * 64 + 64], nd[:, 0:64], sm[:, 9:10], None, ALU.mult)
                blast = bR[h][:, ci * 64 + 63:ci * 64 + 64]
                p.op('dve', lambda e: e.reduce_max(sm[:, 10:11], gR[h][:, cs], AX.X), r=[gR[h]], w=[sm])
                p.ts('dve', sm[:, 11:12], sm[:, 10:11], -1.0, None, ALU.mult)
                p.tt('dve', sm[:, 10:11], sm[:, 10:11], blast, ALU.add)
                p.act(wl, gR[h][:, cs], AF.Exp, bias=sm[:, 11:12], scale=1.0)
                p.tt('dve', kw, kT[h][:, cs], wl, ALU.mult)
                p.tr(PS[7][0:64, 0:64], kw, ident[0:64, 0:64])
                p.copy('act', kwt, PS[7][0:64, 0:64])
                p.mm(PS[7][0:64, 128:193], kwt, vo1[:, h, :])
                p.tt('dve', sm[:, 0:1], blast, m, ALU.add)
                p.tt('dve', sm[:, 1:2], sm[:, 0:1], sm[:, 10:11], ALU.max)
                p.tt('dve', sm[:, 2:3], sm[:, 0:1], sm[:, 1:2], ALU.subtract)
                p.act(sm[:, 2:3], sm[:, 2:3], AF.Exp)
                p.tt('dve', sm[:, 3:4], sm[:, 10:11], sm[:, 1:2], ALU.subtract)
                p.act(sm[:, 3:4], sm[:, 3:4], AF.Exp)
                p.act(cl, PS[7][0:64, 128:193], AF.Copy, scale=sm[:, 3:4])
                p.stt(C, C, sm[:, 2:3], cl, ALU.mult, ALU.add)
                p.copy('dve', m, sm[:, 1:2])
            p.tt('dve', hh, hh, sgo, ALU.mult)
            for h in range(2):
                hs = slice(h * 64, h * 64 + 64)
                p.op('dve', lambda e: e.bn_stats(bst, hh[:, hs]), r=[hh], w=[bst])
                p.op('dve', lambda e: e.bn_aggr(bmv, bst), r=[bst], w=[bmv])
                p.act(bmv[:, 1:2], bmv[:, 1:2], AF.Sqrt, bias=eps5)
                p.op('dve', lambda e: e.reciprocal(bmv[:, 1:2], bmv[:, 1:2]), r=[bmv], w=[bmv])
                p.ts('dve', yt[:, hs], hh[:, hs], bmv[:, 0:1], bmv[:, 1:2], ALU.subtract, ALU.mult)
            p.tt('pool', yt, yt, ng, ALU.mult)
            p.dma('sp', y_d[b0 + ci * 64:b0 + ci * 64 + 64, :], yt)
    p.pop()


def build_mlstm(NCH):
    return standalone(emit_mlstm, NCH)


def emit_rwkv(cx, NCH, NSTEP):
    nc, p, PS = cx.nc, cx.p, cx.PS
    P_ = NCH * 64
    BLK = 512
    SUB = 32
    di = cx.inp
    hT_d = di("hT", [D, P_])
    w_d = di("w", [D, 640]); mu_d = di("mu", [128, 6])
    wup_d = di("wup", [64, 128]); aup_d = di("aup", [64, 128]); gup_d = di("gup", [128, 128])
    cols_d = di("cols", [128, 8])
    gng_d = di("gng", [128]); gnb_d = di("gnb", [128]); bo_d = di("blockones", [128, 128]); id_d = di("ident", [128, 128])
    y_d = cx.out("y", [P_, 128])
    scr = lambda n: cx.scratch(n, [P_ + 1, 128])
    k2s, nkas, vs, bons, gs, yraw = (scr(n) for n in ("k2s", "nkas", "vs", "bons", "gs", "yraw"))
    p.push()
    decT = p.sb([128, P_], name="decT"); kkT = p.sb([128, P_], name="kkT"); rTh = p.sb([128, P_ + 1], name="rTh")
    gng = p.sb([128, 128]); p.dma('sp', gng, gng_d.partition_broadcast(128))
    gnb = p.sb([128, 128]); p.dma('sp', gnb, gnb_d.partition_broadcast(128))
    epsg = p.sb([128, 1]); p.memset('dve', epsg, 64e-5)
    p.push()
    stage = [p.sb([128, 2048], name="stg%d" % i) for i in range(2)]
    W = load_w_bf16(p, w_d, 640, stage, "W")
    def ld(src, shape, nm):
        t = p.sb(shape, name=nm); p.dma('sp', t, src); return t
    mu = ld(mu_d, [128, 6], "mu"); wup = ld(wup_d, [64, 128], "wup"); aup = ld(aup_d, [64, 128], "aup")
    gup = ld(gup_d, [128, 128], "gup"); cols = ld(cols_d, [128, 8], "cols")
    bones = ld(bo_d, [128, 128], "bones"); ident = ld(id_d, [128, 128], "ident")
    omka = p.sb([128, 1]); p.ts('dve', omka, cols[:, 3:4], -1.0, 1.0, ALU.mult, ALU.add)
    p.memset('dve', rTh[:, 0:1], 0.0)
    hTf = p.sb([128, 8, BLK]); hTb = p.sb([128, 8, BLK], BF16)
    nrows = [128, 128, 128, 64, 64, 128]
    pre = [p.sb([nrows[i], BLK + 1], name="pre%d" % i) for i in range(6)]
    lp = [p.sb([nrows[i], BLK], name="lp%d" % i) for i in range(6)]
    for i in range(6):
        p.memset('dve', pre[i][:, 0:1], 0.0)
    dtmp = p.sb([128, BLK]); a_t = p.sb([128, BLK]); t1 = p.sb([128, BLK]); k2 = p.sb([128, BLK]); nka = p.sb([128, BLK])
    g_t = p.sb([128, BLK]); bon = p.sb([128, BLK]); tok = p.sb([128, 128], name="tokst")
    for b0 in range(0, P_, BLK):
        nb = min(BLK, P_ - b0)
        sl = (slice(None), slice(0, nb))
        load_hblock(p, hT_d, b0, nb, hTf, hTb)
        c0 = 0
        for i in range(6):
            nr = nrows[i]
            proj_fm(p, PS[i % 2][0:nr, 0:nb], W, c0, nr, hTb, 0, nb)
            c0 += nr
            p.copy('act', pre[i][:, 1:1 + nb], PS[i % 2][0:nr, 0:nb])
            p.tt('dve', dtmp[0:nr, 0:nb], pre[i][:, 0:nb], pre[i][:, 1:1 + nb], ALU.subtract)
            p.stt(lp[i][sl], dtmp[0:nr, 0:nb], mu[0:nr, i:i + 1], pre[i][:, 1:1 + nb], ALU.mult, ALU.add)
            p.copy('pool', pre[i][:, 0:1], pre[i][:, nb:nb + 1])
        r_, k_, v_, xw, xa, xg = lp
        bs = slice(b0, b0 + nb)
        p.copy('pool', rTh[:, 1 + b0:1 + b0 + nb], r_[sl])
        p.act(xw[sl], xw[sl], AF.Tanh)
        p.mm(PS[2][:, 0:nb], wup, xw[sl])
        p.act(t1[sl], PS[2][:, 0:nb], AF.Sigmoid, bias=cols[:, 0:1])
        p.act(decT[:, bs], t1[sl], AF.Exp, scale=-float(np.exp(-0.5)))
        p.mm(PS[3][:, 0:nb], aup, xa[sl])
        p.act(a_t[sl], PS[3][:, 0:nb], AF.Sigmoid, bias=cols[:, 1:2])
        p.act(xg[sl], xg[sl], AF.Sigmoid)
        p.mm(PS[2][:, 0:nb], gup, xg[sl])
        p.copy('act', g_t[sl], PS[2][:, 0:nb])
        p.ts('dve', t1[sl], k_[sl], cols[:, 2:3], None, ALU.mult)
        p.tt('pool', dtmp[sl], t1[sl], t1[sl], ALU.mult)
        p.mm(PS[3][:, 0:nb], bones, dtmp[sl])
        p.act(dtmp[sl], PS[3][:, 0:nb], AF.Sqrt)
        p.ts('dve', dtmp[sl], dtmp[sl], 1e-12, None, ALU.max)
        p.op('dve', lambda e: e.reciprocal(dtmp[sl], dtmp[sl]), r=[dtmp], w=[dtmp])
        p.tt('dve', kkT[:, bs], t1[sl], dtmp[sl], ALU.mult)
        p.ts('dve', t1[sl], a_t[sl], cols[:, 3:4], omka, ALU.mult, ALU.add)
        p.tt('dve', k2[sl], k_[sl], t1[sl], ALU.mult)
        p.stt(nka[sl], kkT[:, bs], -1.0, a_t[sl], ALU.mult, ALU.mult)
        p.stt(t1[sl], r_[sl], cols[:, 4:5], k2[sl], ALU.mult, ALU.mult)
        p.mm(PS[2][:, 0:nb], bones, t1[sl])
        p.tt('dve', bon[sl], PS[2][:, 0:nb], v_[sl], ALU.mult)
        for (src, dst) in ((k2, k2s), (nka, nkas), (v_, vs), (bon, bons), (g_t, gs)):
            for j in range(nb // 128):
                p.tr(PS[4 + j % 2][:, 0:128], src[:, j * 128:(j + 1) * 128], ident)
                p.copy('act', tok, PS[4 + j % 2][:, 0:128])
                p.dma('sp', dst[b0 + j * 128:b0 + (j + 1) * 128, :], tok)
    p.pop()
    ST = p.sb([128, 64], name="ST"); p.memset('dve', ST, 0.0)
    KVl = p.sb([2, SUB, 128], name="KVl"); KAl = p.sb([4, SUB, 128], name="KAl"); Vr = p.sb([2, SUB, 64], name="Vr")
    Ycp = p.sb([4, SUB, 64], name="Ycp"); L1 = p.sb([128, SUB, 4], name="L1")
    p.memset('dve', KVl, 0.0); p.memset('dve', KAl, 0.0); p.memset('dve', L1, 0.0)
    for s0 in range(0, NSTEP, SUB):
        ns = min(SUB, NSTEP - s0)
        for h in range(2):
            hc = slice(h * 64, h * 64 + 64)
            p.dma('sp', KVl[h:h + 1, 0:ns, hc], k2s[s0:s0 + ns, hc].unsqueeze(0))
            p.dma('sp', KAl[2 * h:2 * h + 1, 0:ns, hc], nkas[s0:s0 + ns, hc].unsqueeze(0))
            p.dma('sp', Vr[h:h + 1, 0:ns, :], vs[s0:s0 + ns, hc].unsqueeze(0))
            p.copy('pool', L1[hc, 0:ns, 2 * h], kkT[hc, s0:s0 + ns])
            p.copy('pool', L1[hc, 0:ns, 2 * h + 1], rTh[hc, s0:s0 + ns])
        for s in range(ns):
            t = s0 + s
            pa = PS[t % 2]; pb = PS[2 + t % 2]
            p.mm(pa[0:4, 0:64], L1[:, s, :], ST)
            p.copy('act', Ycp[:, s, :], pa[0:4, 0:64])
            p.mm(pb[:, 0:64], KVl[:, s, :], Vr[:, s, :], start=True, stop=False)
            p.mm(pb[:, 0:64], KAl[:, s, :], Ycp[:, s, :], start=False, stop=True)
            p.stt(ST, ST, decT[:, t:t + 1], pb[:, 0:64], ALU.mult, ALU.add)
        for h in range(2):
            p.dma('sp', yraw[s0:s0 + ns, h * 64:h * 64 + 64].unsqueeze(0), Ycp[2 * h + 1:2 * h + 2, 0:ns, :])
    yt = p.sb([128, 128]); bt = p.sb([128, 128]); gt = p.sb([128, 128]); ot = p.sb([128, 128])
    bst = p.sb([128, 6]); bmv = p.sb([128, 2])
    for t0 in range(0, P_, 128):
        if t0 + 1 >= NSTEP:
            break
        nt = min(128, NSTEP - 1 - t0)
        p.dma('sp', yt[0:nt, :], yraw[t0 + 1:t0 + 1 + nt, :])
        p.dma('sp', bt[0:nt, :], bons[t0:t0 + nt, :])
        p.dma('sp', gt[0:nt, :], gs[t0:t0 + nt, :])
        for h in range(2):
            hs = slice(h * 64, h * 64 + 64)
            p.op('dve', lambda e: e.bn_stats(bst[0:nt, :], yt[0:nt, hs]), r=[yt], w=[bst])
            p.op('dve', lambda e: e.bn_aggr(bmv[0:nt, :], bst[0:nt, :]), r=[bst], w=[bmv])
            p.act(bmv[0:nt, 1:2], bmv[0:nt, 1:2], AF.Sqrt, bias=epsg[0:nt, :])
            p.op('dve', lambda e: e.reciprocal(bmv[0:nt, 1:2], bmv[0:nt, 1:2]), r=[bmv], w=[bmv])
            p.ts('dve', ot[0:nt, hs], yt[0:nt, hs], bmv[0:nt, 0:1], bmv[0:nt, 1:2], ALU.subtract, ALU.mult)
        p.tt('pool', ot[0:nt, :], ot[0:nt, :], gng[0:nt, :], ALU.mult)
        p.tt('pool', ot[0:nt, :], ot[0:nt, :], gnb[0:nt, :], ALU.add)
        p.tt('dve', ot[0:nt, :], ot[0:nt, :], bt[0:nt, :], ALU.add)
        p.tt('dve', ot[0:nt, :], ot[0:nt, :], gt[0:nt, :], ALU.mult)
        p.dma('sp', y_d[t0:t0 + nt, :], ot[0:nt, :])
    p.pop()


def build_rwkv(NCH, NSTEP):
    return standalone(emit_rwkv, NCH, NSTEP)


def emit_dsa(cx, NKT, NSLOT, NROUND):
    nc, p, PS = cx.nc, cx.p, cx.PS
    TK = NKT * 128
    NBq = NSLOT
    di = cx.inp
    hT_d = di("hT", [D, TK])
    fused = "rk" in cx.over
    if not fused:
        hTq_d = di("hTq", [D, NSLOT * 128])
    wq_d = di("wq", [D, 256]); wckv_d = di("wckv", [D, 128]); widx_d = di("widx", [D, 296])
    kvg_d = di("kvg", [128]); wuk_d = di("wuk", [128, 64]); wuv_d = di("wuv", [128, 64])
    b3_d = di("B3raw", [3, 4, 128, 128]); mA_d = di("maskA", [128, 128]); mB_d = di("maskB", [128, 128]); c31_d = di("c31", [128, 4])
    id_d = di("ident", [128, 128])
    y_d = cx.out("y", [NBq * 128, 256])
    p.push()
    stage = [p.sb([128, 512], name="stg%d" % i) for i in range(2)]
    Wq = load_w_bf16(p, wq_d, 256, stage, "Wq"); Wc = load_w_bf16(p, wckv_d, 128, stage, "Wc")
    Widx = p.sb([128, 8, 296], name="Widx"); p.dma('sp', Widx, widx_d.rearrange("(c f) n -> f c n", f=128))
    def ld(src, shape, nm):
        t = p.sb(shape, name=nm); p.dma('sp', t, src); return t
    kvg = p.sb([128, 128]); p.dma('sp', kvg, kvg_d.partition_broadcast(128))
    wuk = ld(wuk_d, [128, 64], "wuk"); wuv = ld(wuv_d, [128, 64], "wuv")
    c31 = ld(c31_d, [128, 4], "c31"); maskA = ld(mA_d, [128, 128], "maskA"); maskB = ld(mB_d, [128, 128], "maskB"); ident = ld(id_d, [128, 128], "ident")
    Badj = [p.sb([128, 4, 128], name="Badj%d" % k) for k in range(3)]
    for k in range(3):
        p.dma('sp', Badj[k], b3_d[k].rearrange("h i s -> i h s"))
        for h in range(4):
            p.ts('dve', Badj[k][:, h, :], Badj[k][:, h, :], c31[:, h:h + 1], None, ALU.subtract)
    eps6 = p.sb([128, 1]); p.memset('dve', eps6, 1e-6)
    kiT = p.sb([32, TK], name="kiT"); kT = p.sb([64, TK], BF16, name="kT"); v_all = p.sb([128, NKT, 64], BF16, name="v_all")
    score = p.sb([128, TK], name="score"); wk = p.sb([128, TK], name="wk")
    hTf = p.sb([128, 8, 128]); hTb = p.sb([128, 8, 128], BF16)
    ct = p.sb([128, 128]); sq = p.sb([128, 128]); cT = p.sb([128, 128]); sm = p.sb([128, 8], name="dsm")
    for kt in range(NKT):
        ks = slice(kt * 128, kt * 128 + 128)
        load_hblock(p, hT_d, kt * 128, 128, hTf, hTb)
        for c in range(8):
            p.mm(PS[0][0:32, 0:128], Widx[:, c, 256:288], hTf[:, c, :], start=(c == 0), stop=(c == 7))
        p.copy('act', kiT[:, ks], PS[0][0:32, 0:128])
        proj_tm(p, PS[1][:, 0:128], Wc, 0, 128, hTb, 0, 128)
        p.copy('act', ct, PS[1][:, 0:128])
        p.tt('dve', sq, ct, ct, ALU.mult)
        p.op('dve', lambda e: e.reduce_sum(sm[:, 0:1], sq, AX.X), r=[sq], w=[sm])
        p.act(sm[:, 0:1], sm[:, 0:1], AF.Sqrt, bias=eps6, scale=1.0 / 128.0)
        p.op('dve', lambda e: e.reciprocal(sm[:, 0:1], sm[:, 0:1]), r=[sm], w=[sm])
        p.stt(ct, ct, sm[:, 0:1], kvg, ALU.mult, ALU.mult)
        p.tr(PS[2][:, 0:128], ct, ident)
        p.copy('act', cT, PS[2][:, 0:128])
        p.mm(PS[3][0:64, 0:128], wuk, cT)
        p.copy('act', kT[:, ks], PS[3][0:64, 0:128])
        p.mm(PS[3][:, 128:192], cT, wuv)
        p.copy('act', v_all[:, kt, :], PS[3][:, 128:192])
    qT = p.sb([64, 4, 128], BF16, name="qT"); qiT = p.sb([32, 8, 128], name="qiT"); wi = p.sb([128, 8], name="wi")
    if fused:
        hTf2 = p.sb([128, 8, 128], name="hTf2"); rkq = p.sb([128, 2], name="rkq"); p.dma('sp', rkq, cx.over["rk"])
    rel = [p.sb([128, 512], name="rel%d" % i) for i in range(2)]
    m8 = p.sb([128, 8], name="m8"); PT = [p.sb([128, 128], BF16, name="PT%d" % i) for i in range(2)]
    yt = p.sb([128, 256], name="yt")
    for bi in range(NSLOT):
        S = min((2 * bi + 2) * 128, TK)
        jA = 2 * bi
        if not fused:
            load_hblock(p, hTq_d, bi * 128, 128, hTf, hTb)
        else:
            hsrc = hT_d.rearrange("(c f) n -> f c n", f=128)
            p.dma('sp', hTf, hsrc[:, :, jA * 128:jA * 128 + 128])
            if (jA + 2) * 128 <= TK:
                p.dma('sp', hTf2, hsrc[:, :, (jA + 1) * 128:(jA + 2) * 128])
            else:
                p.memset('pool', hTf2, 0.0)
            p.ts('dve', hTf, hTf, rkq[:, 0:1], None, ALU.mult)
            p.stt(hTf, hTf2, rkq[:, 1:2], hTf, ALU.mult, ALU.add)
            p.copy('pool', hTb, hTf)
        for h in range(4):
            proj_fm(p, PS[0][0:64, 0:128], Wq, h * 64, 64, hTb, 0, 128)
            p.op('act', lambda e: e.mul(qT[:, h, :], PS[0][0:64, 0:128], 0.125), r=[PS[0]], w=[qT])
        for hi in range(8):
            for c in range(8):
                p.mm(PS[1][0:32, 0:128], Widx[:, c, hi * 32:(hi + 1) * 32], hTf[:, c, :], start=(c == 0), stop=(c == 7))
            p.copy('act', qiT[:, hi, :], PS[1][0:32, 0:128])
        for c in range(8):
            p.mm(PS[2][:, 0:8], hTf[:, c, :], Widx[:, c, 288:296], start=(c == 0), stop=(c == 7))
        p.op('act', lambda e: e.mul(wi, PS[2][:, 0:8], 1.0 / 16.0), r=[PS[2]], w=[wi])
        for k0 in range(0, S, 512):
            kn = min(512, S - k0)
            for hi in range(8):
                pb = PS[3 + hi % 2]
                p.mm(pb[:, 0:kn], qiT[:, hi, :], kiT[:, k0:k0 + kn])
                r_ = rel[hi % 2]
                p.act(r_[:, 0:kn], pb[:, 0:kn], AF.Relu)
                if hi == 0:
                    p.ts('dve', score[:, k0:k0 + kn], r_[:, 0:kn], wi[:, 0:1], None, ALU.mult)
                else:
                    p.stt(score[:, k0:k0 + kn], r_[:, 0:kn], wi[:, hi:hi + 1], score[:, k0:k0 + kn], ALU.mult, ALU.add)
        p.memset('dve', score[:, 0:N_META], 1e30)
        p.tt('dve', score[:, jA * 128:jA * 128 + 128], score[:, jA * 128:jA * 128 + 128], maskA, ALU.min)
        if (jA + 2) * 128 <= S:
            p.tt('dve', score[:, (jA + 1) * 128:(jA + 2) * 128], score[:, (jA + 1) * 128:(jA + 2) * 128], maskB, ALU.min)
        if S > NROUND * 8:
            p.copy('pool', wk[:, 0:S], score[:, 0:S])
            for r in range(NROUND):
                p.op('dve', lambda e: e.max(m8, wk[:, 0:S]), r=[wk], w=[m8])
                if r < NROUND - 1:
                    p.op('dve', lambda e: e.match_replace(wk[:, 0:S], m8, wk[:, 0:S], -3e38), r=[m8, wk], w=[wk])
            p.ts('dve', sm[:, 1:2], m8[:, 7:8], -1e29, None, ALU.max)
        else:
            p.memset('dve', sm[:, 1:2], -1e29)
        p.ts('dve', wk[:, 0:S], score[:, 0:S], sm[:, 1:2], 1.0, ALU.is_ge, ALU.subtract)
        lg = score
        for h in range(4):
            for k0 in range(0, S, 512):
                kn = min(512, S - k0)
                pb = PS[5 + (k0 // 512) % 2]
                p.mm(pb[:, 0:kn], qT[:, h, :], kT[:, k0:k0 + kn])
                p.stt(lg[:, k0:k0 + kn], wk[:, k0:k0 + kn], 1e30, pb[:, 0:kn], ALU.mult, ALU.add)
            for k in range(3):
                jb = jA - 1 + k
                if jb >= 0 and (jb + 1) * 128 <= S:
                    p.tt('dve', lg[:, jb * 128:(jb + 1) * 128], lg[:, jb * 128:(jb + 1) * 128], Badj[k][:, h, :], ALU.add)
            p.op('dve', lambda e: e.reduce_max(sm[:, 2:3], lg[:, 0:S], AX.X), r=[lg], w=[sm])
            p.ts('dve', sm[:, 3:4], sm[:, 2:3], -1.0, None, ALU.mult)
            p.act(lg[:, 0:S], lg[:, 0:S], AF.Exp, bias=sm[:, 3:4], accum_out=sm[:, 4:5])
            nkb = S // 128
            for kb in range(nkb):
                pt = PS[1 + kb % 2]
                p.tr(pt[:, 0:128], lg[:, kb * 128:(kb + 1) * 128], ident)
                p.copy('act' if kb % 2 == 0 else 'dve', PT[kb % 2], pt[:, 0:128])
                p.mm(PS[7][:, 0:64], PT[kb % 2], v_all[:, kb, :], start=(kb == 0), stop=(kb == nkb - 1))
            p.op('dve', lambda e: e.reciprocal(sm[:, 5:6], sm[:, 4:5]), r=[sm], w=[sm])
            p.ts('dve', yt[:, h * 64:(h + 1) * 64], PS[7][:, 0:64], sm[:, 5:6], None, ALU.mult)
        p.dma('sp', y_d[bi * 128:(bi + 1) * 128, :], yt)
    p.pop()


def build_dsa(NKT, NSLOT, NROUND):
    return standalone(emit_dsa, NKT, NSLOT, NROUND)


OFFS = [0, 1024, 1808, 2488, 3520, 7616]


def _c(a):
    return np.ascontiguousarray(a, dtype=np.float32)


def _t5_bucket(n):
    n = np.maximum(n, 0)
    me = 16
    large = me + (np.log(np.maximum(n, 1).astype(np.float32) / me) / np.log(128 / me) * (32 - me)).astype(np.int32)
    return np.where(n < me, n, np.minimum(large, 31))


def _run(nc, in_maps):
    res = run_bass_kernel_spmd(nc, in_maps, core_ids=list(range(NCORES)))
    return res.results


def _gla_inputs(inp, l, hp, hT, NCH):
    w_in = inp['w_in'][l]; c0 = OFFS[1]
    heads = [2 * hp, 2 * hp + 1]
    qcols = np.concatenate([np.arange(c0 + hh * 32, c0 + hh * 32 + 32) for hh in heads])
    kcols = qcols + 128
    vcols = np.concatenate([np.arange(c0 + 256 + hh * 64, c0 + 256 + hh * 64 + 64) for hh in heads])
    acols = np.arange(c0 + 512, c0 + 528)
    ocols = vcols + 256 + 16
    return dict(hT=hT, wqk=_c(w_in[:, np.concatenate([qcols, kcols])]), wvo=_c(w_in[:, np.concatenate([vcols, ocols])]),
                wa=_c(w_in[:, acols]), aup=_c(inp['gla_a_up'][l][:, hp * 64:(hp + 1) * 64]),
                ab=_c(inp['gla_a_b'][l][hp * 64:(hp + 1) * 64, None]), ng=_c(np.tile(inp['gla_norm_g'][l], 2)),
                cmask=(np.arange(NCH * 64) % 64 != 0).astype(np.float32),
                maskU2=_c(np.tile(np.triu(np.ones((64, 64), np.float32)), (1, 2))), ident=np.eye(128, dtype=np.float32))


def _mlstm_inputs(inp, l, hp, hT, NCH):
    w_in = inp['w_in'][l]; c0 = OFFS[3]
    heads = [2 * hp, 2 * hp + 1]
    hc = np.concatenate([np.arange(hh * 64, hh * 64 + 64) for hh in heads])
    P = NCH * 64; pos = np.arange(P); real = (pos >= 48)
    return dict(hT=hT, wq=_c(w_in[:, c0 + hc]), wk=_c(w_in[:, c0 + 256 + hc]),
                wvo=_c(w_in[:, np.concatenate([c0 + 512 + hc, c0 + 776 + hc])]),
                wi=_c(np.repeat(w_in[:, [c0 + 768 + hh for hh in heads]], 64, axis=1)),
                wf=_c(np.repeat(w_in[:, [c0 + 772 + hh for hh in heads]], 64, axis=1)),
                cwq=_c(inp['mlstm_conv_w'][l][:, hc].T), cwk=_c(inp['mlstm_conv_w'][l][:, 256 + hc].T),
                cbq=_c(inp['mlstm_conv_b'][l][hc, None]), cbk=_c(inp['mlstm_conv_b'][l][256 + hc, None]),
                ib=_c(np.repeat(inp['mlstm_i_b'][l][heads], 64)[:, None]), fb=_c(np.repeat(inp['mlstm_f_b'][l][heads], 64)[:, None]),
                ng=_c(inp['mlstm_norm_g'][l][hc]), cmask=(pos % 64 != 0).astype(np.float32),
                pm01=real.astype(np.float32), pmneg=np.where(real, 0.0, -1e30).astype(np.float32),
                maskL=np.where(np.tril(np.ones((64, 64))) > 0, 0.0, -1e30).astype(np.float32), ident=np.eye(128, dtype=np.float32))


def _rwkv_inputs(inp, l, hp, hT):
    w_in = inp['w_in'][l]
    hc = np.arange(hp * 128, hp * 128 + 128)
    colsel = np.concatenate([hc, 256 + hc, 512 + hc, np.arange(768, 832), np.arange(832, 896), np.arange(896, 1024)])
    mu = inp['rwkv_mu'][l]
    mu6 = np.zeros((128, 6), np.float32)
    mu6[:, 0] = mu[hc]; mu6[:, 1] = mu[256 + hc]; mu6[:, 2] = mu[512 + hc]
    mu6[:64, 3] = mu[768:832]; mu6[:64, 4] = mu[832:896]; mu6[:, 5] = mu[896:1024]
    cols = np.zeros((128, 8), np.float32)
    cols[:, 0] = inp['rwkv_w0'][l][hc]; cols[:, 1] = inp['rwkv_a0'][l][hc]; cols[:, 2] = inp['rwkv_k_k'][l][hc]
    cols[:, 3] = inp['rwkv_k_a'][l][hc]; cols[:, 4] = inp['rwkv_r_k'][l].reshape(-1)[hc]
    bo = np.zeros((128, 128), np.float32); bo[:64, :64] = 1; bo[64:, 64:] = 1
    return dict(hT=hT, w=_c(w_in[:, colsel]), mu=mu6, wup=_c(inp['rwkv_w_up'][l][:, hc]), aup=_c(inp['rwkv_a_up'][l][:, hc]),
                gup=_c(inp['rwkv_g_up'][l][:, hc]), cols=cols, gng=_c(inp['rwkv_gn_g'][l][hc]), gnb=_c(inp['rwkv_gn_b'][l][hc]),
                blockones=bo, ident=np.eye(128, dtype=np.float32))


def _dsa_inputs(inp, l, half, hT_tok, NSLOT):
    w_in = inp['w_in'][l]; c0 = OFFS[2]; rb = inp['rel_bias']
    i = np.arange(128)[:, None]; s = np.arange(128)[None, :]
    bd = _t5_bucket(i - s); bp = _t5_bucket(128 + i - s)
    Draw = np.stack([np.where(s <= i, rb[bd, hh], rb[31, hh]) for hh in range(4)]).astype(np.float32)
    Praw = np.stack([rb[bp, hh] for hh in range(4)]).astype(np.float32)
    c31t = np.stack([np.full((128, 128), rb[31, hh]) for hh in range(4)]).astype(np.float32)
    B3 = np.stack([Praw, Draw, c31t]) if half == 0 else np.stack([c31t, Praw, Draw])
    mm = np.where(s <= i, 3e38, -1e30).astype(np.float32)
    maskA = mm if half == 0 else np.full((128, 128), 3e38, np.float32)
    maskB = np.full((128, 128), -1e30, np.float32) if half == 0 else mm
    hTq = np.zeros((1024, NSLOT * 128), np.float32)
    for ii in range(NSLOT):
        j = 2 * ii + half
        if j * 128 < hT_tok.shape[1]:
            hTq[:, ii * 128:(ii + 1) * 128] = hT_tok[:, j * 128:(j + 1) * 128]
    return dict(hT=hT_tok, hTq=hTq, B3raw=_c(B3), maskA=maskA, maskB=maskB,
                wq=_c(w_in[:, c0:c0 + 256]), wckv=_c(w_in[:, c0 + 256:c0 + 384]), widx=_c(w_in[:, c0 + 384:c0 + 680]),
                kvg=_c(inp['dsa_kv_norm_g'][l]), wuk=_c(inp['dsa_w_uk'][l]), wuv=_c(inp['dsa_w_uv'][l]),
                c31=_c(np.tile(rb[31][None, :], (128, 1))), ident=np.eye(128, dtype=np.float32))


def build_fused(NTL, NCH, NSTEP, NKT, NSLOT, NROUND, HALF):
    nc, p, PS = new_prog()
    NTOK = NTL * 128; P_ = NCH * 64; TK = NKT * 128
    PW = max(48 + HALF + NTOK, 48 + TK, P_)
    PWy = PW
    groups = [[2 * g, 2 * g + 1] for g in range(NCORES // 2)]
    ext = lambda n, s_: nc.dram_tensor(n, list(s_), F32, kind="ExternalInput").ap()
    itn = lambda n, s_: nc.dram_tensor(n, list(s_), F32, kind="Internal").ap()
    ident_d = ext("ident", [128, 128]); rk_d = ext("rk", [128, 2]); x_d = ext("x", [NTOK, D])
    out_d = nc.dram_tensor("out", [NTOK, D], F32, kind="ExternalOutput").ap()
    h_own = itn("h_own", [NTOK, D]); hT_own = itn("hT_own", [D, NTOK])
    hT_pos = itn("hT_pos", [D, PW]); Y_abd = itn("Y_abd", [PWy, 384]); Y_c = itn("Y_c", [NSLOT * 128, 256])

    class Gathered:
        def __init__(self, name, src, bounds):
            self.src, self.bounds = src, bounds
            self.bufs = [itn("%s_%d" % (name, k), [2 * (b1 - b0), src.shape[1]]) for k, (b0, b1) in enumerate(bounds)]

        def gather(self):
            for (b0, b1), g in zip(self.bounds, self.bufs):
                p.coll("AllGather", self.src[b0:b1, :], g, groups)

        def rows(self, q, r0, n):
            for (b0, b1), g in zip(self.bounds, self.bufs):
                if b0 <= r0 and r0 + n <= b1:
                    return g[q * (b1 - b0) + r0 - b0:q * (b1 - b0) + r0 - b0 + n, :]
            raise ValueError("row range straddles gather chunks")

    def bounds_of(total, step, first=None):
        bs = []
        b0 = 0
        nxt = first if first is not None else step
        while b0 < total:
            b1 = min(total, nxt)
            bs.append((b0, b1)); b0 = b1; nxt = b1 + step
        return bs
    G_h = Gathered("G_h", hT_own, bounds_of(D, 64))
    G_abd = Gathered("G_abd", Y_abd, bounds_of(PWy, 1024, 48 + 1024))
    G_c = Gathered("G_c", Y_c, bounds_of(NSLOT * 128, 1024))
    p.push()
    z = p.sb([128, 512], name="zeros"); p.memset('dve', z, 0.0)
    for c in range(8):
        p.dma('sp', hT_pos[c * 128:(c + 1) * 128, 0:48], z[:, 0:48])
    for r0 in range(0, PWy, 128):
        n = min(128, PWy - r0)
        p.dma('sp', Y_abd[r0:r0 + n, :], z[0:n, 0:384])
    p.pop()
    emit_ln_in(Ctx(nc, p, PS, "ln_", dict(x=x_d, y=h_own, hT_out=hT_own, ident=ident_d)), NTL)
    for l in range(DEPTH):
        pre = "l%d_" % l
        G_h.gather()
        for q in range(2):
            for (b0, b1) in G_h.bounds:
                p.dma('sp', hT_pos[b0:b1, 48 + HALF * q:48 + HALF * q + NTOK], G_h.rows(q, b0, b1 - b0))
        emit_rwkv(Ctx(nc, p, PS, pre + "rwkv_", dict(hT=hT_pos[:, 0:P_], y=Y_abd[0:P_, 0:128], ident=ident_d)), NCH, NSTEP)
        emit_gla(Ctx(nc, p, PS, pre + "gla_", dict(hT=hT_pos[:, 0:P_], y=Y_abd[0:P_, 128:256], ident=ident_d)), NCH)
        emit_mlstm(Ctx(nc, p, PS, pre + "mlstm_", dict(hT=hT_pos[:, 0:P_], y=Y_abd[0:P_, 256:384], ident=ident_d)), NCH)
        emit_dsa(Ctx(nc, p, PS, pre + "dsa_", dict(hT=hT_pos[:, 48:48 + TK], y=Y_c, ident=ident_d, rk=rk_d)), NKT, NSLOT, NROUND)
        G_abd.gather()
        G_c.gather()
        last = (l == DEPTH - 1)
        over = dict(h=h_own, hT=hT_own, G_abd=G_abd, G_c=G_c, rk=rk_d, PWy=PWy, HALF=HALF, NSLOT=NSLOT, ident=ident_d,
                    out=(out_d if last else h_own))
        if not last:
            over["hT_out"] = hT_own
        emit_post(Ctx(nc, p, PS, pre + "post_", over), NTL, 17)
    p.finish()
    return nc


def kernel(**inputs):
    inp = {k: np.asarray(v) for k, v in inputs.items()}
    x = inp['x'].astype(np.float32)
    B, S, _ = x.shape
    T = S + N_META
    HALF = S // 2
    NTL = -(-(T - HALF) // 128)
    NTOK = NTL * 128
    NCH = -(-(T + 48 + 1) // 64); NCH += NCH % 2
    NSTEP = 48 + T + 1
    NKT = -(-T // 128)
    NSLOT = (NKT + 1) // 2
    NROUND = min(256, S // 4) // 8
    assert B * 2 == NCORES and (HALF // 128) % 2 == 0
    nc = build_fused(NTL, NCH, NSTEP, NKT, NSLOT, NROUND, HALF)
    hcat = np.concatenate([np.broadcast_to(inp['meta'][None].astype(np.float32), (B, N_META, D)), x], 1)
    maps = []
    for c in range(NCORES):
        b, r = c // 2, c % 2
        xo = np.zeros((NTOK, D), np.float32)
        seg = hcat[b, r * HALF:min(T, r * HALF + NTOK)] if r == 1 else hcat[b, 0:HALF]
        xo[:len(seg)] = seg
        m = dict(x=xo, ident=np.eye(128, dtype=np.float32), rk=np.tile(np.array([[1.0 - r, float(r)]], np.float32), (128, 1)))
        m["ln_g"] = _c(inp['ln_in_g']); m["ln_b"] = _c(inp['ln_in_b'])
        dummy = np.zeros((D, 1), np.float32)
        for l in range(DEPTH):
            pre = "l%d_" % l
            for nm, d in (("rwkv_", _rwkv_inputs(inp, l, r, dummy)), ("gla_", _gla_inputs(inp, l, r, dummy, NCH)),
                          ("mlstm_", _mlstm_inputs(inp, l, r, dummy, NCH)), ("dsa_", _dsa_inputs(inp, l, r, dummy, 0))):
                for k, v in d.items():
                    if k not in ("hT", "hTq", "ident"):
                        m[pre + nm + k] = v
            wr = _c(np.concatenate([inp['moe_w_grp'][l], inp['moe_w_rt'][l]], 1))
            br = _c(np.concatenate([inp['moe_b_grp'][l], inp['moe_b_rt'][l]], 0))
            post = dict(wg=_c(inp['w_in'][l][:, OFFS[4]:OFFS[5]]), wb=_c(inp['w_branch'][l].reshape(D, D)), wo=_c(inp['w_out'][l]),
                        ln1g=_c(inp['ln1_g'][l]), ln1b=_c(inp['ln1_b'][l]), ln2g=_c(inp['ln2_g'][l]), ln2b=_c(inp['ln2_b'][l]),
                        wr=wr, br=br, mg=_c(inp['moe_w_gate'][l]), mu=_c(inp['moe_w_up'][l]), md=_c(inp['moe_w_down'][l]))
            for k, v in post.items():
                m[pre + "post_" + k] = v
        maps.append(m)
    res = _run(nc, maps)
    out = np.zeros((B, T, D), np.float32)
    for c in range(NCORES):
        b, r = c // 2, c % 2
        n = HALF if r == 0 else T - HALF
        out[b, r * HALF:r * HALF + n] = res[c]["out"][:n]
    return np.ascontiguousarray(out[:, N_META:])


def kernel_unfused(**inputs):
    inp = {k: np.asarray(v) for k, v in inputs.items()}
    x = inp['x'].astype(np.float32)
    B, S, _ = x.shape
    T = S + N_META
    NTOKC = (B * T) // NCORES
    NTL = -(-NTOKC // 128)
    NCH = -(-(T + 48) // 64); NCH += NCH % 2
    if NCH * 64 < 48 + T + 1:
        NCH += 2
    P = NCH * 64
    NSTEP = 48 + T + 1
    NKT = -(-T // 128)
    NSLOT = (NKT + 1) // 2
    NROUND = min(256, S // 4) // 8
    ident = np.eye(128, dtype=np.float32)

    def tok_split(a):
        out = []
        for c in range(NCORES):
            sh = np.zeros((NTL * 128, a.shape[1]), np.float32)
            sh[:NTOKC] = a[c * NTOKC:(c + 1) * NTOKC]
            out.append(sh)
        return out

    def tok_merge(res, key):
        return np.concatenate([r[key][:NTOKC] for r in res], 0)

    hcat = np.concatenate([np.broadcast_to(inp['meta'][None], (B, N_META, D)), x], 1).reshape(B * T, D)
    res = _run(build_ln_in(NTL), [dict(x=s_, g=_c(inp['ln_in_g']), b=_c(inp['ln_in_b'])) for s_ in tok_split(hcat)])
    h = tok_merge(res, "y")

    for l in range(DEPTH):
        hb = h.reshape(B, T, D)
        hTpos = []
        hTtok = []
        for b in range(B):
            a = np.zeros((D, P), np.float32); a[:, 48:48 + T] = hb[b].T; hTpos.append(a)
            a2 = np.zeros((D, NKT * 128), np.float32); a2[:, :T] = hb[b].T; hTtok.append(a2)
        y = np.zeros((B, T, D), np.float32)
        res = _run(build_rwkv(NCH, NSTEP), [_rwkv_inputs(inp, l, c % 2, hTpos[c // 2]) for c in range(NCORES)])
        for c in range(NCORES):
            y[c // 2, :, (c % 2) * 128:(c % 2) * 128 + 128] = res[c]["y"][48:48 + T]
        res = _run(build_gla(NCH), [_gla_inputs(inp, l, c % 2, hTpos[c // 2], NCH) for c in range(NCORES)])
        for c in range(NCORES):
            y[c // 2, :, 256 + (c % 2) * 128:256 + (c % 2) * 128 + 128] = res[c]["y"][48:48 + T]
        res = _run(build_mlstm(NCH), [_mlstm_inputs(inp, l, c % 2, hTpos[c // 2], NCH) for c in range(NCORES)])
        for c in range(NCORES):
            y[c // 2, :, 768 + (c % 2) * 128:768 + (c % 2) * 128 + 128] = res[c]["y"][48:48 + T]
        res = _run(build_dsa(NKT, NSLOT, NROUND), [_dsa_inputs(inp, l, c % 2, hTtok[c // 2], NSLOT) for c in range(NCORES)])
        for c in range(NCORES):
            half = c % 2
            for ii in range(NSLOT):
                j = 2 * ii + half
                if j * 128 >= T:
                    continue
                n = min(128, T - j * 128)
                y[c // 2, j * 128:j * 128 + n, 512:768] = res[c]["y"][ii * 128:ii * 128 + n]
        yf = y.reshape(B * T, D)
        hs = tok_split(h); ys = tok_split(yf)
        wr = _c(np.concatenate([inp['moe_w_grp'][l], inp['moe_w_rt'][l]], 1))
        br = _c(np.concatenate([inp['moe_b_grp'][l], inp['moe_b_rt'][l]], 0))
        common = dict(wg=_c(inp['w_in'][l][:, OFFS[4]:OFFS[5]]), wb=_c(inp['w_branch'][l].reshape(D, D)), wo=_c(inp['w_out'][l]),
                      ln1g=_c(inp['ln1_g'][l]), ln1b=_c(inp['ln1_b'][l]), ln2g=_c(inp['ln2_g'][l]), ln2b=_c(inp['ln2_b'][l]),
                      wr=wr, br=br, mg=_c(inp['moe_w_gate'][l]), mu=_c(inp['moe_w_up'][l]), md=_c(inp['moe_w_down'][l]), ident=ident)
        maps = [dict(h=hs[c], hT=_c(hs[c].T), yT=_c(ys[c].T), **common) for c in range(NCORES)]
        res = _run(build_post(NTL, 8), maps)
        h = tok_merge(res, "out")
    return np.ascontiguousarray(h.reshape(B, T, D)[:, N_META:]).astype(np.float32)
```

```python
import numpy as np
import concourse.bass as bass
import concourse.mybir as mybir
from concourse.bass_utils import run_bass_kernel_spmd
from contextlib import ExitStack

F32 = mybir.dt.float32
BF16 = mybir.dt.bfloat16
ALU = mybir.AluOpType
AF = mybir.ActivationFunctionType
AX = mybir.AxisListType

D = 1024
DEPTH = 2
N_META = 16
DN_ALPHA = (2 * DEPTH) ** 0.25
LN_EPS = 1e-5
NCORES = 8

ENGS = ('pe', 'act', 'dve', 'pool', 'sp')
N_DMA_SEMS = 16
CC_INC = 1


class Prog:
    def __init__(self, nc):
        self.nc = nc
        self.E = {'pe': nc.tensor, 'act': nc.scalar, 'dve': nc.vector, 'pool': nc.gpsimd, 'sp': nc.sync}
        self.sem = {e: nc.alloc_semaphore("s_" + e) for e in ENGS}
        self.cnt = {e: 0 for e in ENGS}
        self.dsem = [nc.alloc_semaphore("d_%d" % i) for i in range(N_DMA_SEMS)]
        self.dcnt = [0] * N_DMA_SEMS
        self.dnext = 0
        self.known = {e: {} for e in ENGS}
        self.last_w = {}
        self.readers = {}
        self.semobj = {}
        for e in ENGS:
            self.semobj['s_' + e] = self.sem[e]
        for i in range(N_DMA_SEMS):
            self.semobj['d_%d' % i] = self.dsem[i]
        self.n_ins = 0
        self.n_wait = 0
        self._uid = 0
        self.stacks = []
        self.bg = None

    def sb(self, shape, dt=F32, name=None):
        self._uid += 1
        nm = (name or "t") + "_%d" % self._uid
        if self.stacks:
            return self.stacks[-1].enter_context(self.nc.sbuf_tensor(nm, list(shape), dt)).ap()
        return self.nc.alloc_sbuf_tensor(nm, list(shape), dt).ap()

    def push(self):
        self.stacks.append(ExitStack())

    def pop(self):
        self.barrier()
        self.stacks.pop().close()

    def barrier(self):
        for e in ENGS:
            for f in ENGS:
                if f != e and self.cnt[f] > self.known[e].get('s_' + f, 0):
                    self.E[e].wait_ge(self.sem[f], self.cnt[f])
                    self.known[e]['s_' + f] = self.cnt[f]
            for i in range(N_DMA_SEMS):
                sn = 'd_%d' % i
                if self.dcnt[i] > self.known[e].get(sn, 0):
                    self.E[e].wait_ge(self.dsem[i], self.dcnt[i])
                    self.known[e][sn] = self.dcnt[i]

    def ps(self, shape, dt=F32, name=None):
        self._uid += 1
        return self.nc.alloc_psum_tensor(name or ("p%d" % self._uid), list(shape), dt).ap()

    @staticmethod
    def key_of(x):
        if isinstance(x, (str, tuple)):
            return x
        return x.tensor.name

    def _deps(self, eng, reads, writes):
        need = {}

        def add(sn, v, prod_eng):
            if prod_eng == eng and eng == 'pe':
                return
            if need.get(sn, 0) < v:
                need[sn] = v
        for k in reads:
            w = self.last_w.get(k)
            if w is not None:
                add(*w)
        for k in writes:
            w = self.last_w.get(k)
            if w is not None:
                add(*w)
            for (sn, v, pe) in self.readers.get(k, {}).values():
                if pe == eng:
                    continue
                add(sn, v, pe)
        for sn, v in need.items():
            if self.known[eng].get(sn, 0) < v:
                self.E[eng].wait_ge(self.semobj[sn], v)
                self.known[eng][sn] = v
                self.n_wait += 1

    def _record(self, reads, writes, tok):
        for k in reads:
            self.readers.setdefault(k, {})[tok[0]] = tok
        for k in writes:
            self.last_w[k] = tok
            self.readers[k] = {}

    def op(self, eng, fn, r=(), w=()):
        reads = [self.key_of(x) for x in r]
        writes = [self.key_of(x) for x in w]
        self._deps(eng, reads, writes)
        ins = fn(self.E[eng])
        self.cnt[eng] += 1
        ins.then_inc(self.sem[eng], 1)
        tok = ('s_' + eng, self.cnt[eng], eng)
        self._record(reads, writes, tok)
        self.n_ins += 1
        return ins

    def bgsteps(self, n):
        for _ in range(n):
            if self.bg is None:
                return
            try:
                next(self.bg)
            except StopIteration:
                self.bg = None

    def drain_bg(self):
        while self.bg is not None:
            self.bgsteps(1)

    def dma(self, eng, out, in_, r=None, w=None, **kw):
        reads = [self.key_of(x) for x in (r if r is not None else [in_])]
        writes = [self.key_of(x) for x in (w if w is not None else [out])]
        self._deps(eng, reads, writes)
        i = self.dnext
        self.dnext = (self.dnext + 1) % N_DMA_SEMS
        sn = 'd_%d' % i
        if self.known[eng].get(sn, 0) < self.dcnt[i]:
            self.E[eng].wait_ge(self.dsem[i], self.dcnt[i])
            self.known[eng][sn] = self.dcnt[i]
        ins = self.E[eng].dma_start(out=out, in_=in_, **kw)
        self.dcnt[i] += 16
        ins.then_inc(self.dsem[i], 16)
        tok = (sn, self.dcnt[i], 'dma')
        self._record(reads, writes, tok)
        self.n_ins += 1
        return ins

    def coll(self, kind, in_, out, groups):
        eng = 'pool'
        reads = [self.key_of(in_)]
        writes = [self.key_of(out)]
        self._deps(eng, reads, writes)
        i = self.dnext
        self.dnext = (self.dnext + 1) % N_DMA_SEMS
        sn = 'd_%d' % i
        if self.known[eng].get(sn, 0) < self.dcnt[i]:
            self.E[eng].wait_ge(self.dsem[i], self.dcnt[i])
            self.known[eng][sn] = self.dcnt[i]
        ins = self.nc.gpsimd.collective_compute(kind, ALU.bypass, replica_groups=groups, ins=[in_.opt()], outs=[out.opt()])
        self.dcnt[i] += CC_INC
        ins.then_inc(self.dsem[i], CC_INC)
        tok = (sn, self.dcnt[i], 'dma')
        self._record(reads, writes, tok)
        self.n_ins += 1
        return ins

    def finish(self, eng='sp'):
        for i in range(N_DMA_SEMS):
            if self.dcnt[i] > 0:
                self.E[eng].wait_ge(self.dsem[i], self.dcnt[i])
        for e in ENGS:
            if self.cnt[e] > 0 and e != eng:
                self.E[eng].wait_ge(self.sem[e], self.cnt[e])

    def mm(self, out, lhsT, rhs, start=True, stop=True, **kw):
        return self.op('pe', lambda e: e.matmul(out, lhsT, rhs, start=start, stop=stop, **kw),
                       r=[lhsT, rhs], w=[out])

    def tr(self, out, in_, ident):
        return self.op('pe', lambda e: e.transpose(out, in_, ident), r=[in_, ident], w=[out])

    def act(self, out, in_, func, bias=None, scale=1.0, accum_out=None, eng='act'):
        r = [in_]
        w = [out]
        kw = {}
        if bias is not None:
            kw['bias'] = bias
            if not isinstance(bias, (int, float)):
                r.append(bias)
        if not isinstance(scale, (int, float)):
            r.append(scale)
        if accum_out is not None:
            kw['accum_out'] = accum_out
            w.append(accum_out)
        return self.op('act', lambda e: e.activation(out, in_, func, scale=scale, **kw), r=r, w=w)

    def tt(self, eng, out, a, b, op):
        return self.op(eng, lambda e: e.tensor_tensor(out, a, b, op), r=[a, b], w=[out])

    def ts(self, eng, out, a, s1, s2, op0, op1=None, accum_out=None):
        r = [a] + [s for s in (s1, s2) if s is not None and not isinstance(s, (int, float))]
        w = [out] + ([accum_out] if accum_out is not None else [])
        kw = {}
        if accum_out is not None:
            kw['accum_out'] = accum_out
        if op1 is None:
            return self.op(eng, lambda e: e.tensor_scalar(out, a, s1, None, op0, **kw), r=r, w=w)
        return self.op(eng, lambda e: e.tensor_scalar(out, a, s1, s2, op0, op1, **kw), r=r, w=w)

    def stt(self, out, a, s, b, op0, op1, accum_out=None):
        r = [a, b] + ([s] if not isinstance(s, (int, float)) else [])
        w = [out] + ([accum_out] if accum_out is not None else [])
        kw = {}
        if accum_out is not None:
            kw['accum_out'] = accum_out
        return self.op('dve', lambda e: e.scalar_tensor_tensor(out, a, s, b, op0, op1, **kw), r=r, w=w)

    def copy(self, eng, out, in_):
        if eng == 'act':
            return self.op('act', lambda e: e.copy(out, in_), r=[in_], w=[out])
        return self.op(eng, lambda e: e.tensor_copy(out, in_), r=[in_], w=[out])

    def memset(self, eng, out, v):
        return self.op(eng, lambda e: e.memset(out, v), w=[out])


class Ctx:
    def __init__(self, nc, p, PS, prefix="", over=None):
        self.nc, self.p, self.PS, self.prefix, self.over = nc, p, PS, prefix, dict(over or {})

    def inp(self, n, s):
        if n in self.over:
            return self.over[n]
        return self.nc.dram_tensor(self.prefix + n, list(s), F32, kind="ExternalInput").ap()

    def out(self, n, s):
        if n in self.over:
            return self.over[n]
        return self.nc.dram_tensor(self.prefix + n, list(s), F32, kind="ExternalOutput").ap()

    def scratch(self, n, s, dt=F32):
        return self.nc.dram_tensor(self.prefix + n, list(s), dt, kind="Internal").ap()


def new_prog():
    nc = bass.Bass("TRN2", target_bir_lowering=False)
    p = Prog(nc)
    PS = [p.ps([128, 512], name="bank%d" % i) for i in range(8)]
    return nc, p, PS


def standalone(emit, *a, **k):
    nc, p, PS = new_prog()
    emit(Ctx(nc, p, PS), *a, **k)
    p.finish()
    return nc


def emit_transposed(p, PS, src, dstT_d, cols, ident, tmpT):
    for c in range(8):
        pt = PS[4 + c // 4]
        p.tr(pt[:, (c % 4) * 128:(c % 4 + 1) * 128], src[:, c * 128:(c + 1) * 128], ident)
        if c % 4 == 3:
            p.copy('act', tmpT[:, c - 3:c + 1, :], pt.rearrange("p (a n) -> p a n", a=4))
    p.dma('sp', dstT_d.rearrange("(c f) n -> f c n", f=128)[:, :, cols], tmpT)


def emit_ln(p, xt, out, G, Bt, tmp, eps):
    st, mv, rs = tmp
    for c in range(2):
        p.op('dve', lambda e: e.bn_stats(st[:, c * 6:(c + 1) * 6], xt[:, c * 512:(c + 1) * 512]), r=[xt], w=[st])
    p.op('dve', lambda e: e.bn_aggr(mv, st.rearrange("p (c s) -> p c s", s=6)), r=[st], w=[mv])
    p.act(rs, mv[:, 1:2], AF.Sqrt, bias=eps)
    p.op('dve', lambda e: e.reciprocal(rs, rs), r=[rs], w=[rs])
    p.ts('dve', out, xt, mv[:, 0:1], rs[:, 0:1], ALU.subtract, ALU.mult)
    p.tt('pool', out, out, G, ALU.mult)
    p.tt('pool', out, out, Bt, ALU.add)


def load_cast(p, dst, src, stage, chunk_cols, engs=('pool', 'act')):
    a, n = dst.shape[1], dst.shape[2]
    chunk_cols = min(chunk_cols, stage[0].shape[1])
    k = 0
    for i in range(a):
        for c0 in range(0, n, chunk_cols):
            c1 = min(n, c0 + chunk_cols)
            stg = stage[k % len(stage)]
            p.dma('sp', stg[:, 0:c1 - c0], src[:, i, c0:c1])
            p.copy(engs[k % len(engs)], dst[:, i, c0:c1], stg[:, 0:c1 - c0])
            k += 1


def emit_ln_in(cx, NTL):
    nc, p, PS = cx.nc, cx.p, cx.PS
    x = cx.inp("x", [NTL * 128, D]); g = cx.inp("g", [D]); b = cx.inp("b", [D])
    y = cx.out("y", [NTL * 128, D])
    hT_out = cx.over.get("hT_out")
    p.push()
    G = p.sb([128, D]); Bt = p.sb([128, D]); eps = p.sb([128, 1])
    p.memset('dve', eps, LN_EPS)
    p.dma('sp', G, g.partition_broadcast(128))
    p.dma('sp', Bt, b.partition_broadcast(128))
    xts = [p.sb([128, D]) for _ in range(2)]
    ots = [p.sb([128, D]) for _ in range(2)]
    tmp = (p.sb([128, 12]), p.sb([128, 2]), p.sb([128, 1]))
    if hT_out is not None:
        ident = p.sb([128, 128]); p.dma('sp', ident, cx.inp("ident", [128, 128]))
        tmpT = p.sb([128, 8, 128])
    for t in range(NTL):
        xt = xts[t % 2]; ot = ots[t % 2]
        p.dma('sp', xt, x[t * 128:(t + 1) * 128, :])
        emit_ln(p, xt, ot, G, Bt, tmp, eps)
        p.dma('sp', y[t * 128:(t + 1) * 128, :], ot)
        if hT_out is not None:
            emit_transposed(p, PS, ot, hT_out, slice(t * 128, (t + 1) * 128), ident, tmpT)
    p.pop()


def build_ln_in(NTL):
    return standalone(emit_ln_in, NTL)


def emit_post(cx, NTL, GT=8):
    nc, p, PS = cx.nc, cx.p, cx.PS
    NTOK = NTL * 128
    di = cx.inp
    h_d = di("h", [NTOK, D]); hT_d = di("hT", [D, NTOK])
    fused = "G_abd" in cx.over
    if fused:
        G_abd, G_c, rk_d = cx.over["G_abd"], cx.over["G_c"], cx.over["rk"]
        PWy, HALF, NSLOT = cx.over["PWy"], cx.over["HALF"], cx.over["NSLOT"]
    else:
        yT_d = di("yT", [D, NTOK])
    hT_out = cx.over.get("hT_out")
    wg_d = di("wg", [D, 4 * D]); wb_d = di("wb", [D, D]); wo_d = di("wo", [D, D])
    ln1g = di("ln1g", [D]); ln1b = di("ln1b", [D]); ln2g = di("ln2g", [D]); ln2b = di("ln2b", [D])
    wr_d = di("wr", [D, 20]); br_d = di("br", [20])
    mg_d = di("mg", [16, D, 256]); mu_d = di("mu", [16, D, 256]); md_d = di("md", [16, 256, D])
    id_d = di("ident", [128, 128])
    out_d = cx.out("out", [NTOK, D])
    h1_d = cx.scratch("h1s", [NTOK, D], F32)
    xT_d = cx.scratch("xTs", [128, 8, NTOK], BF16)
    p.push()
    ident = p.sb([128, 128]); p.dma('sp', ident, id_d)
    eps = p.sb([128, 1]); p.memset('dve', eps, LN_EPS)
    G1 = p.sb([128, D]); B1 = p.sb([128, D]); G2 = p.sb([128, D]); B2 = p.sb([128, D])
    for tdst, src in ((G1, ln1g), (B1, ln1b), (G2, ln2g), (B2, ln2b)):
        p.dma('sp', tdst, src.partition_broadcast(128))
    Wr = p.sb([128, 8, 20]); p.dma('sp', Wr, wr_d.rearrange("(c f) n -> f c n", f=128))
    Br = p.sb([128, 20]); p.dma('sp', Br, br_d.partition_broadcast(128))
    gate_all = p.sb([128, NTL, 16])
    lntmp = (p.sb([128, 12]), p.sb([128, 2]), p.sb([128, 1]))
    stage = [p.sb([128, 2048], name="stg%d" % i) for i in range(2)]

    p.push()
    Wg = p.sb([128, 8, 4 * D], BF16, name="Wg")
    Wb = p.sb([128, 8, D], BF16, name="Wb")
    Wo = p.sb([128, 8, D], BF16, name="Wo")
    load_cast(p, Wg, wg_d.rearrange("(c f) n -> f c n", f=128), stage, 2048)
    load_cast(p, Wb, wb_d.rearrange("(c f) n -> f c n", f=128), stage, 2048)
    load_cast(p, Wo, wo_d.rearrange("(c f) n -> f c n", f=128), stage, 2048)
    hTf = p.sb([128, 8, 128]); hTb = p.sb([128, 8, 128], BF16)
    yTf = p.sb([128, 8, 128]); yTb = p.sb([128, 8, 128], BF16)
    if fused:
        rk = p.sb([128, 2], name="rk"); p.dma('sp', rk, rk_d)
        ycand = [p.sb([128, D], name="ycand%d" % i) for i in range(2)]
        ytile = p.sb([128, D], name="ytile")
    ht = p.sb([128, D]); gsb = [p.sb([128, 512]) for _ in range(2)]
    merged = p.sb([128, D]); tmpm = p.sb([128, 512])
    mTb = p.sb([128, 8, 128], BF16)
    pre = p.sb([128, D]); h1 = p.sb([128, D])
    x32 = p.sb([128, 8, 128]); xb = p.sb([128, 8, 128], BF16)
    lg = p.sb([128, 20]); sm = p.sb([128, 16], name="smallr")
    gm4 = p.sb([128, 4]); ge4 = p.sb([128, 4]); em = p.sb([128, 16]); elm = p.sb([128, 16])
    top8 = p.sb([128, 8]); g0 = p.sb([128, 16]); g1 = p.sb([128, 16])
    for t in range(NTL):
        ts_ = slice(t * 128, (t + 1) * 128)
        p.dma('sp', hTf, hT_d.rearrange("(c f) n -> f c n", f=128)[:, :, ts_])
        p.dma('sp', ht, h_d[ts_, :])
        p.copy('pool', hTb, hTf)
        if not fused:
            p.dma('sp', yTf, yT_d.rearrange("(c f) n -> f c n", f=128)[:, :, ts_])
            p.copy('pool', yTb, yTf)
        else:
            for rc in range(2):
                r0 = 48 + HALF * rc + t * 128
                yc = ycand[rc].rearrange("p (m q c) -> p m q c", m=4, q=2)
                for q in range(2):
                    src = G_abd.rows(q, r0, 128).rearrange("p (m c) -> p m c", m=3)
                    p.dma('sp', yc[:, 0:2, q, :], src[:, 0:2, :])
                    p.dma('sp', yc[:, 3, q, :], src[:, 2, :])
                slot = (HALF // 256) * rc + t // 2
                if slot < NSLOT:
                    p.dma('sp', ycand[rc][:, 512:768], G_c.rows(t % 2, slot * 128, 128))
                else:
                    p.memset('pool', ycand[rc][:, 512:768], 0.0)
            p.ts('dve', ytile, ycand[0], rk[:, 0:1], None, ALU.mult)
            p.stt(ytile, ycand[1], rk[:, 1:2], ytile, ALU.mult, ALU.add)
            for c in range(8):
                pt = PS[4 + c // 4]
                p.tr(pt[:, (c % 4) * 128:(c % 4 + 1) * 128], ytile[:, c * 128:(c + 1) * 128], ident)
                if c % 4 == 3:
                    p.copy('act', yTb[:, c - 3:c + 1, :], pt.rearrange("p (a n) -> p a n", a=4))
        for i in range(4):
            for hf in range(2):
                pg = PS[hf]; py = PS[2 + hf]
                for c in range(8):
                    p.mm(pg, hTb[:, c, :], Wg[:, c, i * D + hf * 512: i * D + hf * 512 + 512], start=(c == 0), stop=(c == 7))
                p.act(gsb[hf], pg, AF.Sigmoid)
                for c in range(2):
                    p.mm(py, yTb[:, 2 * i + c, :], Wb[:, 2 * i + c, hf * 512:(hf + 1) * 512], start=(c == 0), stop=(c == 1))
                if i == 0:
                    p.tt('dve', merged[:, hf * 512:(hf + 1) * 512], gsb[hf], py, ALU.mult)
                else:
                    p.tt('dve', tmpm, gsb[hf], py, ALU.mult)
                    p.tt('pool', merged[:, hf * 512:(hf + 1) * 512], merged[:, hf * 512:(hf + 1) * 512], tmpm, ALU.add)
        for c in range(8):
            pt = PS[4 + c // 4]
            p.tr(pt[:, (c % 4) * 128:(c % 4 + 1) * 128], merged[:, c * 128:(c + 1) * 128], ident)
            if c % 4 == 3:
                p.copy('act', mTb[:, c - 3:c + 1, :], pt.rearrange("p (a n) -> p a n", a=4))
        for hf in range(2):
            po = PS[6 + hf]
            for c in range(8):
                p.mm(po, mTb[:, c, :], Wo[:, c, hf * 512:(hf + 1) * 512], start=(c == 0), stop=(c == 7))
            p.stt(pre[:, hf * 512:(hf + 1) * 512], ht[:, hf * 512:(hf + 1) * 512], DN_ALPHA, po, ALU.mult, ALU.add)
        emit_ln(p, pre, h1, G1, B1, lntmp, eps)
        p.dma('sp', h1_d[ts_, :], h1)
        for c in range(8):
            pt = PS[4 + c // 4]
            p.tr(pt[:, (c % 4) * 128:(c % 4 + 1) * 128], h1[:, c * 128:(c + 1) * 128], ident)
            if c % 4 == 3:
                p.copy('act', x32[:, c - 3:c + 1, :], pt.rearrange("p (a n) -> p a n", a=4))
        p.copy('pool', xb, x32)
        p.dma('sp', xT_d[:, :, ts_], xb)
        pr = PS[0]
        for c in range(8):
            p.mm(pr[:, 0:20], x32[:, c, :], Wr[:, c, :], start=(c == 0), stop=(c == 7))
        p.tt('dve', lg, pr[:, 0:20], Br, ALU.add)
        p.op('dve', lambda e: e.reduce_max(sm[:, 0:1], lg[:, 0:4], AX.X), r=[lg], w=[sm])
        p.ts('dve', sm[:, 1:2], sm[:, 0:1], -1.0, None, ALU.mult)
        p.act(ge4, lg[:, 0:4], AF.Exp, bias=sm[:, 1:2], accum_out=sm[:, 2:3])
        p.op('dve', lambda e: e.reciprocal(sm[:, 3:4], sm[:, 2:3]), r=[sm], w=[sm])
        p.ts('dve', gm4, lg[:, 0:4], sm[:, 0:1], None, ALU.is_ge)
        p.copy('dve', em.rearrange("p (g e) -> p g e", e=4), gm4.unsqueeze(2).to_broadcast([128, 4, 4]))
        p.ts('dve', em, em, 1e30, -1e30, ALU.mult, ALU.add)
        p.tt('dve', elm, lg[:, 4:20], em, ALU.add)
        p.op('dve', lambda e: e.max(top8, elm), r=[elm], w=[top8])
        p.tt('dve', sm[:, 4:5], top8[:, 1:2], top8[:, 0:1], ALU.subtract)
        p.act(sm[:, 5:6], sm[:, 4:5], AF.Sigmoid)
        p.ts('dve', sm[:, 6:7], sm[:, 5:6], -1.0, 1.0, ALU.mult, ALU.add)
        p.tt('dve', sm[:, 7:8], sm[:, 6:7], sm[:, 3:4], ALU.mult)
        p.tt('dve', sm[:, 8:9], sm[:, 5:6], sm[:, 3:4], ALU.mult)
        p.ts('dve', g0, elm, top8[:, 0:1], sm[:, 7:8], ALU.is_equal, ALU.mult)
        p.ts('dve', g1, elm, top8[:, 1:2], sm[:, 8:9], ALU.is_equal, ALU.mult)
        p.tt('dve', gate_all[:, t, :], g0, g1, ALU.add)

    p.pop()
    p.push()
    We_g = [p.sb([128, 8, 256], BF16, name="Weg%d" % i) for i in range(2)]
    We_u = [p.sb([128, 8, 256], BF16, name="Weu%d" % i) for i in range(2)]
    We_d = [p.sb([128, 2, D], BF16, name="Wed%d" % i) for i in range(2)]
    xg = p.sb([128, 8, GT * 128], BF16, name="xg")
    yacc = p.sb([128, GT, D], name="yacc")
    sg = [p.sb([128, 512], name="sg%d" % i) for i in range(2)]
    hid = [p.sb([128, 512], BF16, name="hid%d" % i) for i in range(2)]
    h1t = p.sb([128, D], name="h1t"); pre2 = p.sb([128, D], name="pre2"); o2 = p.sb([128, D], name="o2")
    tmpT2 = p.sb([128, 8, 128], name="tmpT2")
    k = 0
    for g0_ in range(0, NTL, GT):
        ntg = min(GT, NTL - g0_)
        p.dma('sp', xg[:, :, 0:ntg * 128], xT_d[:, :, g0_ * 128:(g0_ + ntg) * 128])
        for e_ in range(16):
            wgt, wut, wdt = We_g[k % 2], We_u[k % 2], We_d[k % 2]
            k += 1
            load_cast(p, wgt, mg_d[e_].rearrange("(c f) n -> f c n", f=128), stage, 2048)
            load_cast(p, wut, mu_d[e_].rearrange("(c f) n -> f c n", f=128), stage, 2048)
            load_cast(p, wdt, md_d[e_].rearrange("(c f) n -> f c n", f=128), stage, 2048)
            for tb in range(0, ntg, 4):
                nb = min(4, ntg - tb)
                ncol = nb * 128
                cs = slice(tb * 128, tb * 128 + ncol)
                for j in range(2):
                    pa = PS[j * 2]; pb = PS[j * 2 + 1]
                    for c in range(8):
                        p.mm(pa[:, 0:ncol], wgt[:, c, j * 128:(j + 1) * 128], xg[:, c, cs], start=(c == 0), stop=(c == 7))
                    for c in range(8):
                        p.mm(pb[:, 0:ncol], wut[:, c, j * 128:(j + 1) * 128], xg[:, c, cs], start=(c == 0), stop=(c == 7))
                    p.act(sg[j][:, 0:ncol], pa[:, 0:ncol], AF.Silu)
                    p.tt('dve', hid[j][:, 0:ncol], sg[j][:, 0:ncol], pb[:, 0:ncol], ALU.mult)
                for tt_ in range(nb):
                    tile_i = tb + tt_
                    for hf in range(2):
                        pd = PS[4 + (tt_ * 2 + hf) % 4]
                        for j in range(2):
                            p.mm(pd, hid[j][:, tt_ * 128:(tt_ + 1) * 128], wdt[:, j, hf * 512:(hf + 1) * 512], start=(j == 0), stop=(j == 1))
                        ya = yacc[:, tile_i, hf * 512:(hf + 1) * 512]
                        gcol = gate_all[:, g0_ + tile_i, e_:e_ + 1]
                        if e_ == 0:
                            p.ts('dve', ya, pd, gcol, None, ALU.mult)
                        else:
                            p.stt(ya, pd, gcol, ya, ALU.mult, ALU.add)
        for tt_ in range(ntg):
            ts_ = slice((g0_ + tt_) * 128, (g0_ + tt_ + 1) * 128)
            p.dma('sp', h1t, h1_d[ts_, :])
            p.stt(pre2, h1t, DN_ALPHA, yacc[:, tt_, :], ALU.mult, ALU.add)
            emit_ln(p, pre2, o2, G2, B2, lntmp, eps)
            p.dma('sp', out_d[ts_, :], o2)
            if hT_out is not None:
                emit_transposed(p, PS, o2, hT_out, ts_, ident, tmpT2)
    p.pop()
    p.pop()


def build_post(NTL, GT=8):
    return standalone(emit_post, NTL, GT)


def load_w_bf16(p, src_d, ncols, stage, name):
    W = p.sb([128, 8, ncols], BF16, name=name)
    load_cast(p, W, src_d.rearrange("(c f) n -> f c n", f=128), stage, 2048)
    return W


def load_hblock(p, hT_d, pos0, npos, hTf, hTb):
    p.dma('sp', hTf[:, :, 0:npos], hT_d.rearrange("(c f) n -> f c n", f=128)[:, :, pos0:pos0 + npos])
    p.copy('pool', hTb[:, :, 0:npos], hTf[:, :, 0:npos])


def proj_fm(p, ps_out, W, c0, ncols, hTb, n0, npos):
    for c in range(8):
        p.mm(ps_out, W[:, c, c0:c0 + ncols], hTb[:, c, n0:n0 + npos], start=(c == 0), stop=(c == 7))


def proj_tm(p, ps_out, W, c0, ncols, hTb, n0, npos):
    for c in range(8):
        p.mm(ps_out, hTb[:, c, n0:n0 + npos], W[:, c, c0:c0 + ncols], start=(c == 0), stop=(c == 7))


def gla_gen(cx, NCH, stop_at=99):
    nc, p, PS = cx.nc, cx.p, cx.PS
    P_ = NCH * 64
    BLK = 512
    di = cx.inp
    hT_d = di("hT", [D, P_])
    wqk_d = di("wqk", [D, 128]); wvo_d = di("wvo", [D, 256]); wa_d = di("wa", [D, 16])
    aup_d = di("aup", [16, 64]); ab_d = di("ab", [64, 1]); ng_d = di("ng", [128])
    cm_d = di("cmask", [P_]); mu_d = di("maskU2", [64, 128]); id_d = di("ident", [128, 128])
    y_d = cx.out("y", [P_, 128])
    p.push()
    stage = [p.sb([128, 2048], name="stg%d" % i) for i in range(2)]
    Wqk = load_w_bf16(p, wqk_d, 128, stage, "Wqk")
    Wvo = load_w_bf16(p, wvo_d, 256, stage, "Wvo")
    Wa = load_w_bf16(p, wa_d, 16, stage, "Wa")
    aup = p.sb([16, 64]); p.dma('sp', aup, aup_d)
    ab = [p.sb([32, 1]) for _ in range(2)]
    for h in range(2):
        p.dma('sp', ab[h], ab_d[h * 32:(h + 1) * 32, :])
    ng = p.sb([64, 128]); p.dma('sp', ng, ng_d.partition_broadcast(64))
    maskU = p.sb([64, 128]); p.dma('sp', maskU, mu_d)
    ident = p.sb([128, 128]); p.dma('sp', ident, id_d)
    eps6 = p.sb([64, 1]); p.memset('dve', eps6, 1e-6)
    S = [p.sb([32, 64], name="S%d" % h) for h in range(2)]
    for h in range(2):
        p.memset('dve', S[h], 0.0)
    hTf = p.sb([128, 8, BLK]); hTb = p.sb([128, 8, BLK], BF16)
    cm = p.sb([32, BLK]); xa = p.sb([16, BLK])
    mk = lambda nm: [p.sb([32, BLK], name=nm + str(h)) for h in range(2)]
    qT, kT, la, bT, eb, enb, ekb, qg, kg, ku = (mk(n) for n in ("qT", "kT", "la", "bT", "eb", "enb", "ekb", "qg", "kg", "ku"))
    dec = [p.sb([32, BLK // 64], name="dec%d" % h) for h in range(2)]
    vo = p.sb([64, 256]); sgo = p.sb([64, 128]); kut = p.sb([64, 64]); attm = p.sb([64, 128])
    o_sb = p.sb([64, 128]); sq = p.sb([64, 128]); ms = p.sb([64, 2]); yt = p.sb([64, 128])
    yield
    for b0 in range(0, P_, BLK):
        nb = min(BLK, P_ - b0)
        nch = nb // 64
        load_hblock(p, hT_d, b0, nb, hTf, hTb)
        p.dma('sp', cm[:, 0:nb], cm_d[b0:b0 + nb].partition_broadcast(32))
        proj_fm(p, PS[2][0:16, 0:nb], Wa, 0, 16, hTb, 0, nb)
        p.copy('act', xa[:, 0:nb], PS[2][0:16, 0:nb])
        for h in range(2):
            sl = (slice(None), slice(0, nb))
            proj_fm(p, PS[0][0:32, 0:nb], Wqk, h * 32, 32, hTb, 0, nb)
            p.op('act', lambda e: e.mul(qT[h][sl], PS[0][0:32, 0:nb], 32 ** -0.5), r=[PS[0]], w=[qT[h]])
            proj_fm(p, PS[1][0:32, 0:nb], Wqk, 64 + h * 32, 32, hTb, 0, nb)
            p.copy('act', kT[h][sl], PS[1][0:32, 0:nb])
            p.mm(PS[3][0:32, 0:nb], aup[:, h * 32:(h + 1) * 32], xa[:, 0:nb])
            p.act(la[h][sl], PS[3][0:32, 0:nb], AF.Sigmoid, bias=ab[h])
            p.act(la[h][sl], la[h][sl], AF.Ln)
            p.ts('pool', la[h][sl], la[h][sl], 1.0 / 16.0, None, ALU.mult)
            p.op('dve', lambda e: e.tensor_tensor_scan(bT[h][sl], cm[:, 0:nb], la[h][sl], 0.0, ALU.mult, ALU.add),
                 r=[cm, la[h]], w=[bT[h]])
            p.act(eb[h][sl], bT[h][sl], AF.Exp)
            p.act(enb[h][sl], bT[h][sl], AF.Exp, scale=-1.0)
            b3 = bT[h][sl].rearrange("p (c l) -> p c l", l=64)
            p.tt('dve', ekb[h][sl].rearrange("p (c l) -> p c l", l=64), b3[:, :, 63:64].to_broadcast([32, nch, 64]), b3, ALU.subtract)
            p.act(ekb[h][sl], ekb[h][sl], AF.Exp)
            p.act(dec[h][:, 0:nch], b3[:, :, 63], AF.Exp)
            p.tt('dve', qg[h][sl], qT[h][sl], eb[h][sl], ALU.mult)
            p.tt('pool', kg[h][sl], kT[h][sl], enb[h][sl], ALU.mult)
            p.tt('pool', ku[h][sl], kT[h][sl], ekb[h][sl], ALU.mult)
        for ci in range(nch):
            cs = slice(ci * 64, ci * 64 + 64)
            proj_tm(p, PS[4][0:64, 0:256], Wvo, 0, 256, hTb, ci * 64, 64)
            p.copy('act', vo[:, 0:128], PS[4][0:64, 0:128])
            p.act(sgo, PS[4][0:64, 128:256], AF.Silu)
            for h in range(2):
                p.tr(PS[5][0:64, h * 32:h * 32 + 32], ku[h][:, cs], ident[0:32, 0:32])
            p.copy('act', kut, PS[5][0:64, 0:64])
            for h in range(2):
                p.mm(PS[6][0:64, h * 64:h * 64 + 64], kg[h][:, cs], qg[h][:, cs])
            p.tt('dve', attm, PS[6][0:64, 0:128], maskU, ALU.mult)
            for h in range(2):
                p.mm(PS[7][0:64, h * 64:h * 64 + 64], attm[:, h * 64:h * 64 + 64], vo[:, h * 64:h * 64 + 64], start=True, stop=False)
                p.mm(PS[7][0:64, h * 64:h * 64 + 64], qg[h][:, cs], S[h], start=False, stop=True)
            for h in range(2):
                p.mm(PS[5][0:32, 128 + h * 64:128 + h * 64 + 64], kut[:, h * 32:h * 32 + 32], vo[:, h * 64:h * 64 + 64])
                p.stt(S[h], S[h], dec[h][:, ci:ci + 1], PS[5][0:32, 128 + h * 64:128 + h * 64 + 64], ALU.mult, ALU.add)
            p.copy('act', o_sb, PS[7][0:64, 0:128])
            p.tt('dve', sq, o_sb, o_sb, ALU.mult)
            p.op('dve', lambda e: e.reduce_sum(ms, sq.rearrange("p (h d) -> p h d", d=64), AX.X), r=[sq], w=[ms])
            p.act(ms, ms, AF.Sqrt, bias=eps6, scale=1.0 / 64.0)
            p.op('dve', lambda e: e.reciprocal(ms, ms), r=[ms], w=[ms])
            p.tt('dve', yt.rearrange("p (h d) -> p h d", d=64), o_sb.rearrange("p (h d) -> p h d", d=64),
                 ms.unsqueeze(2).to_broadcast([64, 2, 64]), ALU.mult)
            p.tt('pool', yt, yt, ng, ALU.mult)
            p.tt('pool', yt, yt, sgo, ALU.mult)
            p.dma('sp', y_d[b0 + ci * 64:b0 + ci * 64 + 64, :], yt)
            yield
    p.pop()


def emit_gla(cx, NCH, stop_at=99):
    for _ in gla_gen(cx, NCH, stop_at):
        pass


def build_gla(NCH, stop_at=99):
    return standalone(emit_gla, NCH, stop_at)


def emit_mlstm(cx, NCH):
    nc, p, PS = cx.nc, cx.p, cx.PS
    P_ = NCH * 64
    BLK = 512
    di = cx.inp
    hT_d = di("hT", [D, P_])
    wq_d = di("wq", [D, 128]); wk_d = di("wk", [D, 128]); wvo_d = di("wvo", [D, 256])
    wi_d = di("wi", [D, 128]); wf_d = di("wf", [D, 128])
    cwq_d = di("cwq", [128, 4]); cwk_d = di("cwk", [128, 4]); cbq_d = di("cbq", [128, 1]); cbk_d = di("cbk", [128, 1])
    ib_d = di("ib", [128, 1]); fb_d = di("fb", [128, 1]); ng_d = di("ng", [128])
    cm_d = di("cmask", [P_]); pm_d = di("pm01", [P_]); pn_d = di("pmneg", [P_])
    ml_d = di("maskL", [64, 64]); id_d = di("ident", [128, 128])
    y_d = cx.out("y", [P_, 128])
    p.push()
    stage = [p.sb([128, 2048], name="stg%d" % i) for i in range(2)]
    Wq = load_w_bf16(p, wq_d, 128, stage, "Wq"); Wk = load_w_bf16(p, wk_d, 128, stage, "Wk")
    Wvo = load_w_bf16(p, wvo_d, 256, stage, "Wvo")
    Wi = load_w_bf16(p, wi_d, 128, stage, "Wi"); Wf = load_w_bf16(p, wf_d, 128, stage, "Wf")
    def ld(src, shape, nm):
        t = p.sb(shape, name=nm); p.dma('sp', t, src); return t
    cw = {}
    for h in range(2):
        hs = slice(h * 64, h * 64 + 64)
        cw['q', h] = (ld(cwq_d[hs, :], [64, 4], "cwq%d" % h), ld(cbq_d[hs, :], [64, 1], "cbq%d" % h))
        cw['k', h] = (ld(cwk_d[hs, :], [64, 4], "cwk%d" % h), ld(cbk_d[hs, :], [64, 1], "cbk%d" % h))
    ib = [ld(ib_d[h * 64:h * 64 + 64, :], [64, 1], "ib%d" % h) for h in range(2)]
    fb = [ld(fb_d[h * 64:h * 64 + 64, :], [64, 1], "fb%d" % h) for h in range(2)]
    ng = p.sb([64, 128]); p.dma('sp', ng, ng_d.partition_broadcast(64))
    maskL = ld(ml_d, [64, 64], "maskL"); ident = ld(id_d, [128, 128], "ident")
    eps5 = p.sb([64, 1]); p.memset('dve', eps5, 1e-5)
    Cst = [p.sb([64, 65], name="C%d" % h) for h in range(2)]
    mst = [p.sb([64, 1], name="m%d" % h) for h in range(2)]
    for h in range(2):
        p.memset('dve', Cst[h], 0.0); p.memset('dve', mst[h], 0.0)
    hTf = p.sb([128, 8, BLK]); hTb = p.sb([128, 8, BLK], BF16)
    cm = p.sb([64, BLK]); pm = p.sb([64, BLK]); pn = p.sb([64, BLK])
    mk = lambda nm, w=BLK: [p.sb([64, w], name=nm + str(h)) for h in range(2)]
    qpre = mk("qpre", BLK + 3); kpre = mk("kpre", BLK + 3)
    for h in range(2):
        p.memset('dve', qpre[h], 0.0); p.memset('dve', kpre[h], 0.0)
    acc = mk("acc"); qT = mk("qT"); kT = mk("kT"); liR = mk("liR"); lfR = mk("lfR"); bR = mk("bR"); gR = mk("gR")
    vo1 = p.sb([64, 2, 65]); sgo = p.sb([64, 128]); hh = p.sb([64, 128]); yt = p.sb([64, 128])
    for h in range(2):
        p.memset('dve', vo1[:, h, 64:65], 1.0)
    bcol = p.sb([64, 1]); dl = p.sb([64, 64]); sm = p.sb([64, 12], name="msm"); sw = p.sb([64, 64]); swT = p.sb([64, 64])
    t2 = p.sb([64, 65]); nd = p.sb([64, 65]); wl = p.sb([64, 64]); kw = p.sb([64, 64]); kwt = p.sb([64, 64]); cl = p.sb([64, 65])
    bst = p.sb([64, 6]); bmv = p.sb([64, 2])
    for b0 in range(0, P_, BLK):
        nb = min(BLK, P_ - b0)
        nch = nb // 64
        sl = (slice(None), slice(0, nb))
        load_hblock(p, hT_d, b0, nb, hTf, hTb)
        for tdst, src in ((cm, cm_d), (pm, pm_d), (pn, pn_d)):
            p.dma('sp', tdst[:, 0:nb], src[b0:b0 + nb].partition_broadcast(64))
        for h in range(2):
            for (W, pre, outT, key, scl) in ((Wq, qpre[h], qT[h], 'q', 1.0), (Wk, kpre[h], kT[h], 'k', 0.125)):
                proj_fm(p, PS[0][0:64, 0:nb], W, h * 64, 64, hTb, 0, nb)
                p.copy('act', pre[:, 3:3 + nb], PS[0][0:64, 0:nb])
                cwt, cbt = cw[key, h]
                p.ts('dve', acc[h][sl], pre[:, 3:3 + nb], cwt[:, 3:4], None, ALU.mult)
                for j in range(3):
                    p.stt(acc[h][sl], pre[:, j:j + nb], cwt[:, j:j + 1], acc[h][sl], ALU.mult, ALU.add)
                p.act(outT[sl], acc[h][sl], AF.Silu, bias=cbt)
                if scl != 1.0:
                    p.ts('pool', outT[sl], outT[sl], scl, None, ALU.mult)
                p.copy('pool', pre[:, 0:3], pre[:, nb:nb + 3])
            proj_fm(p, PS[1][0:64, 0:nb], Wi, h * 64, 64, hTb, 0, nb)
            p.stt(liR[h][sl], PS[1][0:64, 0:nb], ib[h], pn[:, 0:nb], ALU.add, ALU.add)
            proj_fm(p, PS[2][0:64, 0:nb], Wf, h * 64, 64, hTb, 0, nb)
            p.act(lfR[h][sl], PS[2][0:64, 0:nb], AF.Sigmoid, bias=fb[h])
            p.act(lfR[h][sl], lfR[h][sl], AF.Ln)
            p.tt('pool', lfR[h][sl], lfR[h][sl], pm[:, 0:nb], ALU.mult)
            p.op('dve', lambda e: e.tensor_tensor_scan(bR[h][sl], cm[:, 0:nb], lfR[h][sl], 0.0, ALU.mult, ALU.add),
                 r=[cm, lfR[h]], w=[bR[h]])
            p.tt('dve', gR[h][sl], liR[h][sl], bR[h][sl], ALU.subtract)
        for ci in range(nch):
            cs = slice(ci * 64, ci * 64 + 64)
            proj_tm(p, PS[3][0:64, 0:256], Wvo, 0, 256, hTb, ci * 64, 64)
            p.copy('act', vo1[:, :, 0:64], PS[3][0:64, 0:128].rearrange("p (h d) -> p h d", d=64))
            p.act(sgo, PS[3][0:64, 128:256], AF.Sigmoid)
            for h in range(2):
                C = Cst[h]; m = mst[h]
                p.tr(PS[4][0:64, 0:64], bR[h][:, cs], ident[0:64, 0:64])
                p.copy('act', bcol, PS[4][0:64, 0:1])
                p.stt(dl, gR[h][:, cs], bcol, maskL, ALU.add, ALU.add)
                p.op('dve', lambda e: e.reduce_max(sm[:, 0:1], dl, AX.X), r=[dl], w=[sm])
                p.tt('dve', sm[:, 1:2], bcol, m, ALU.add)
                p.tt('dve', sm[:, 2:3], sm[:, 0:1], sm[:, 1:2], ALU.max)
                p.ts('dve', sm[:, 3:4], sm[:, 2:3], -1.0, None, ALU.mult)
                p.act(sw, dl, AF.Exp, bias=sm[:, 3:4])
                p.mm(PS[5][0:64, 0:64], qT[h][:, cs], kT[h][:, cs])
                p.tt('dve', sw, sw, PS[5][0:64, 0:64], ALU.mult)
                p.tr(PS[4][0:64, 64:128], sw, ident[0:64, 0:64])
                p.copy('act', swT, PS[4][0:64, 64:128])
                p.mm(PS[6][0:64, 0:65], swT, vo1[:, h, :])
                p.mm(PS[6][0:64, 128:193], qT[h][:, cs], C)
                p.tt('dve', sm[:, 4:5], sm[:, 1:2], sm[:, 2:3], ALU.subtract)
                p.act(sm[:, 5:6], sm[:, 4:5], AF.Exp)
                p.act(t2, PS[6][0:64, 128:193], AF.Copy, scale=sm[:, 5:6])
                p.tt('dve', nd, PS[6][0:64, 0:65], t2, ALU.add)
                p.ts('dve', sm[:, 6:7], nd[:, 64:65], -1.0, None, ALU.mult)
                p.tt('dve', sm[:, 6:7], sm[:, 6:7], nd[:, 64:65], ALU.max)
                p.act(sm[:, 7:8], sm[:, 2:3], AF.Exp, scale=-1.0)
                p.tt('dve', sm[:, 8:9], sm[:, 6:7], sm[:, 7:8], ALU.max)
                p.op('dve', lambda e: e.reciprocal(sm[:, 9:10], sm[:, 8:9]), r=[sm], w=[sm])
                p.ts('dve', hh[:, h * 64:h * 64 + 64], nd[:, 0:64], sm[:, 9:10], None, ALU.mult)
                blast = bR[h][:, ci * 64 + 63:ci * 64 + 64]
                p.op('dve', lambda e: e.reduce_max(sm[:, 10:11], gR[h][:, cs], AX.X), r=[gR[h]], w=[sm])
                p.ts('dve', sm[:, 11:12], sm[:, 10:11], -1.0, None, ALU.mult)
                p.tt('dve', sm[:, 10:11], sm[:, 10:11], blast, ALU.add)
                p.act(wl, gR[h][:, cs], AF.Exp, bias=sm[:, 11:12], scale=1.0)
                p.tt('dve', kw, kT[h][:, cs], wl, ALU.mult)
                p.tr(PS[7][0:64, 0:64], kw, ident[0:64, 0:64])
                p.copy('act', kwt, PS[7][0:64, 0:64])
                p.mm(PS[7][0:64, 128:193], kwt, vo1[:, h, :])
                p.tt('dve', sm[:, 0:1], blast, m, ALU.add)
                p.tt('dve', sm[:, 1:2], sm[:, 0:1], sm[:, 10:11], ALU.max)
                p.tt('dve', sm[:, 2:3], sm[:, 0:1], sm[:, 1:2], ALU.subtract)
                p.act(sm[:, 2:3], sm[:, 2:3], AF.Exp)
                p.tt('dve', sm[:, 3:4], sm[:, 10:11], sm[:, 1:2], ALU.subtract)
                p.act(sm[:, 3:4], sm[:, 3:4], AF.Exp)
                p.act(cl, PS[7][0:64, 128:193], AF.Copy, scale=sm[:, 3:4])
                p.stt(C, C, sm[:, 2:3], cl, ALU.mult, ALU.add)
                p.copy('dve', m, sm[:, 1:2])
            p.tt('dve', hh, hh, sgo, ALU.mult)
            for h in range(2):
                hs = slice(h * 64, h * 64 + 64)
                p.op('dve', lambda e: e.bn_stats(bst, hh[:, hs]), r=[hh], w=[bst])
                p.op('dve', lambda e: e.bn_aggr(bmv, bst), r=[bst], w=[bmv])
                p.act(bmv[:, 1:2], bmv[:, 1:2], AF.Sqrt, bias=eps5)
                p.op('dve', lambda e: e.reciprocal(bmv[:, 1:2], bmv[:, 1:2]), r=[bmv], w=[bmv])
                p.ts('dve', yt[:, hs], hh[:, hs], bmv[:, 0:1], bmv[:, 1:2], ALU.subtract, ALU.mult)
            p.tt('pool', yt, yt, ng, ALU.mult)
            p.dma('sp', y_d[b0 + ci * 64:b0 + ci * 64 + 64, :], yt)
            p.bgsteps(1)
    p.pop()


def build_mlstm(NCH):
    return standalone(emit_mlstm, NCH)


def emit_rwkv(cx, NCH, NSTEP):
    nc, p, PS = cx.nc, cx.p, cx.PS
    P_ = NCH * 64
    BLK = 512
    SUB = 32
    di = cx.inp
    hT_d = di("hT", [D, P_])
    w_d = di("w", [D, 640]); mu_d = di("mu", [128, 6])
    wup_d = di("wup", [64, 128]); aup_d = di("aup", [64, 128]); gup_d = di("gup", [128, 128])
    cols_d = di("cols", [128, 8])
    gng_d = di("gng", [128]); gnb_d = di("gnb", [128]); bo_d = di("blockones", [128, 128]); id_d = di("ident", [128, 128])
    y_d = cx.out("y", [P_, 128])
    scr = lambda n: cx.scratch(n, [P_ + 1, 128])
    k2s, nkas, vs, bons, gs, yraw = (scr(n) for n in ("k2s", "nkas", "vs", "bons", "gs", "yraw"))
    p.push()
    decT = p.sb([128, P_], name="decT"); kkT = p.sb([128, P_], name="kkT"); rTh = p.sb([128, P_ + 1], name="rTh")
    gng = p.sb([128, 128]); p.dma('sp', gng, gng_d.partition_broadcast(128))
    gnb = p.sb([128, 128]); p.dma('sp', gnb, gnb_d.partition_broadcast(128))
    epsg = p.sb([128, 1]); p.memset('dve', epsg, 64e-5)
    p.push()
    stage = [p.sb([128, 2048], name="stg%d" % i) for i in range(2)]
    W = load_w_bf16(p, w_d, 640, stage, "W")
    def ld(src, shape, nm):
        t = p.sb(shape, name=nm); p.dma('sp', t, src); return t
    mu = ld(mu_d, [128, 6], "mu"); wup = ld(wup_d, [64, 128], "wup"); aup = ld(aup_d, [64, 128], "aup")
    gup = ld(gup_d, [128, 128], "gup"); cols = ld(cols_d, [128, 8], "cols")
    bones = ld(bo_d, [128, 128], "bones"); ident = ld(id_d, [128, 128], "ident")
    omka = p.sb([128, 1]); p.ts('dve', omka, cols[:, 3:4], -1.0, 1.0, ALU.mult, ALU.add)
    p.memset('dve', rTh[:, 0:1], 0.0)
    hTf = p.sb([128, 8, BLK]); hTb = p.sb([128, 8, BLK], BF16)
    nrows = [128, 128, 128, 64, 64, 128]
    pre = [p.sb([nrows[i], BLK + 1], name="pre%d" % i) for i in range(6)]
    lp = [p.sb([nrows[i], BLK], name="lp%d" % i) for i in range(6)]
    for i in range(6):
        p.memset('dve', pre[i][:, 0:1], 0.0)
    dtmp = p.sb([128, BLK]); a_t = p.sb([128, BLK]); t1 = p.sb([128, BLK]); k2 = p.sb([128, BLK]); nka = p.sb([128, BLK])
    g_t = p.sb([128, BLK]); bon = p.sb([128, BLK]); tok = p.sb([128, 128], name="tokst")
    for b0 in range(0, P_, BLK):
        nb = min(BLK, P_ - b0)
        sl = (slice(None), slice(0, nb))
        load_hblock(p, hT_d, b0, nb, hTf, hTb)
        c0 = 0
        for i in range(6):
            nr = nrows[i]
            proj_fm(p, PS[i % 2][0:nr, 0:nb], W, c0, nr, hTb, 0, nb)
            c0 += nr
            p.copy('act', pre[i][:, 1:1 + nb], PS[i % 2][0:nr, 0:nb])
            p.tt('dve', dtmp[0:nr, 0:nb], pre[i][:, 0:nb], pre[i][:, 1:1 + nb], ALU.subtract)
            p.stt(lp[i][sl], dtmp[0:nr, 0:nb], mu[0:nr, i:i + 1], pre[i][:, 1:1 + nb], ALU.mult, ALU.add)
            p.copy('pool', pre[i][:, 0:1], pre[i][:, nb:nb + 1])
        r_, k_, v_, xw, xa, xg = lp
        bs = slice(b0, b0 + nb)
        p.copy('pool', rTh[:, 1 + b0:1 + b0 + nb], r_[sl])
        p.act(xw[sl], xw[sl], AF.Tanh)
        p.mm(PS[2][:, 0:nb], wup, xw[sl])
        p.act(t1[sl], PS[2][:, 0:nb], AF.Sigmoid, bias=cols[:, 0:1])
        p.act(decT[:, bs], t1[sl], AF.Exp, scale=-float(np.exp(-0.5)))
        p.mm(PS[3][:, 0:nb], aup, xa[sl])
        p.act(a_t[sl], PS[3][:, 0:nb], AF.Sigmoid, bias=cols[:, 1:2])
        p.act(xg[sl], xg[sl], AF.Sigmoid)
        p.mm(PS[2][:, 0:nb], gup, xg[sl])
        p.copy('act', g_t[sl], PS[2][:, 0:nb])
        p.ts('dve', t1[sl], k_[sl], cols[:, 2:3], None, ALU.mult)
        p.tt('pool', dtmp[sl], t1[sl], t1[sl], ALU.mult)
        p.mm(PS[3][:, 0:nb], bones, dtmp[sl])
        p.act(dtmp[sl], PS[3][:, 0:nb], AF.Sqrt)
        p.ts('dve', dtmp[sl], dtmp[sl], 1e-12, None, ALU.max)
        p.op('dve', lambda e: e.reciprocal(dtmp[sl], dtmp[sl]), r=[dtmp], w=[dtmp])
        p.tt('dve', kkT[:, bs], t1[sl], dtmp[sl], ALU.mult)
        p.ts('dve', t1[sl], a_t[sl], cols[:, 3:4], omka, ALU.mult, ALU.add)
        p.tt('dve', k2[sl], k_[sl], t1[sl], ALU.mult)
        p.stt(nka[sl], kkT[:, bs], -1.0, a_t[sl], ALU.mult, ALU.mult)
        p.stt(t1[sl], r_[sl], cols[:, 4:5], k2[sl], ALU.mult, ALU.mult)
        p.mm(PS[2][:, 0:nb], bones, t1[sl])
        p.tt('dve', bon[sl], PS[2][:, 0:nb], v_[sl], ALU.mult)
        for (src, dst) in ((k2, k2s), (nka, nkas), (v_, vs), (bon, bons), (g_t, gs)):
            for j in range(nb // 128):
                p.tr(PS[4 + j % 2][:, 0:128], src[:, j * 128:(j + 1) * 128], ident)
                p.copy('act', tok, PS[4 + j % 2][:, 0:128])
                p.dma('sp', dst[b0 + j * 128:b0 + (j + 1) * 128, :], tok)
    p.pop()
    ST = p.sb([128, 64], name="ST"); p.memset('dve', ST, 0.0)
    sets = []
    for i in range(2):
        d = dict(KVl=p.sb([2, SUB, 128], name="KVl%d" % i), KAl=p.sb([4, SUB, 128], name="KAl%d" % i),
                 Vr=p.sb([2, SUB, 64], name="Vr%d" % i), Ycp=p.sb([4, SUB, 64], name="Ycp%d" % i),
                 L1=p.sb([128, SUB, 4], name="L1%d" % i))
        p.memset('dve', d["KVl"], 0.0); p.memset('dve', d["KAl"], 0.0); p.memset('dve', d["L1"], 0.0)
        sets.append(d)
    nblk = -(-NSTEP // SUB)

    def load_blk(k):
        s0 = k * SUB
        ns = min(SUB, NSTEP - s0)
        d = sets[k % 2]
        for h in range(2):
            hc = slice(h * 64, h * 64 + 64)
            p.dma('sp', d["KVl"][h:h + 1, 0:ns, hc], k2s[s0:s0 + ns, hc].unsqueeze(0))
            p.dma('sp', d["KAl"][2 * h:2 * h + 1, 0:ns, hc], nkas[s0:s0 + ns, hc].unsqueeze(0))
            p.dma('sp', d["Vr"][h:h + 1, 0:ns, :], vs[s0:s0 + ns, hc].unsqueeze(0))
            p.copy('pool', d["L1"][hc, 0:ns, 2 * h], kkT[hc, s0:s0 + ns])
            p.copy('pool', d["L1"][hc, 0:ns, 2 * h + 1], rTh[hc, s0:s0 + ns])

    load_blk(0)
    for k in range(nblk):
        s0 = k * SUB
        ns = min(SUB, NSTEP - s0)
        if k + 1 < nblk:
            load_blk(k + 1)
        d = sets[k % 2]
        KVl, KAl, Vr, Ycp, L1 = d["KVl"], d["KAl"], d["Vr"], d["Ycp"], d["L1"]
        for s in range(ns):
            t = s0 + s
            pa = PS[t % 2]; pb = PS[2 + t % 2]
            p.mm(pa[0:4, 0:64], L1[:, s, :], ST)
            p.copy('act', Ycp[:, s, :], pa[0:4, 0:64])
            p.mm(pb[:, 0:64], KVl[:, s, :], Vr[:, s, :], start=True, stop=False)
            p.mm(pb[:, 0:64], KAl[:, s, :], Ycp[:, s, :], start=False, stop=True)
            p.stt(ST, ST, decT[:, t:t + 1], pb[:, 0:64], ALU.mult, ALU.add)
        for h in range(2):
            p.dma('sp', yraw[s0:s0 + ns, h * 64:h * 64 + 64].unsqueeze(0), Ycp[2 * h + 1:2 * h + 2, 0:ns, :])
    yt = p.sb([128, 128]); bt = p.sb([128, 128]); gt = p.sb([128, 128]); ot = p.sb([128, 128])
    bst = p.sb([128, 6]); bmv = p.sb([128, 2])
    for t0 in range(0, P_, 128):
        if t0 + 1 >= NSTEP:
            break
        nt = min(128, NSTEP - 1 - t0)
        p.dma('sp', yt[0:nt, :], yraw[t0 + 1:t0 + 1 + nt, :])
        p.dma('sp', bt[0:nt, :], bons[t0:t0 + nt, :])
        p.dma('sp', gt[0:nt, :], gs[t0:t0 + nt, :])
        for h in range(2):
            hs = slice(h * 64, h * 64 + 64)
            p.op('dve', lambda e: e.bn_stats(bst[0:nt, :], yt[0:nt, hs]), r=[yt], w=[bst])
            p.op('dve', lambda e: e.bn_aggr(bmv[0:nt, :], bst[0:nt, :]), r=[bst], w=[bmv])
            p.act(bmv[0:nt, 1:2], bmv[0:nt, 1:2], AF.Sqrt, bias=epsg[0:nt, :])
            p.op('dve', lambda e: e.reciprocal(bmv[0:nt, 1:2], bmv[0:nt, 1:2]), r=[bmv], w=[bmv])
            p.ts('dve', ot[0:nt, hs], yt[0:nt, hs], bmv[0:nt, 0:1], bmv[0:nt, 1:2], ALU.subtract, ALU.mult)
        p.tt('pool', ot[0:nt, :], ot[0:nt, :], gng[0:nt, :], ALU.mult)
        p.tt('pool', ot[0:nt, :], ot[0:nt, :], gnb[0:nt, :], ALU.add)
        p.tt('dve', ot[0:nt, :], ot[0:nt, :], bt[0:nt, :], ALU.add)
        p.tt('dve', ot[0:nt, :], ot[0:nt, :], gt[0:nt, :], ALU.mult)
        p.dma('sp', y_d[t0:t0 + nt, :], ot[0:nt, :])
    p.pop()


def build_rwkv(NCH, NSTEP):
    return standalone(emit_rwkv, NCH, NSTEP)


def emit_dsa(cx, NKT, NSLOT, NROUND):
    nc, p, PS = cx.nc, cx.p, cx.PS
    TK = NKT * 128
    NBq = NSLOT
    di = cx.inp
    hT_d = di("hT", [D, TK])
    fused = "rk" in cx.over
    if not fused:
        hTq_d = di("hTq", [D, NSLOT * 128])
    wq_d = di("wq", [D, 256]); wckv_d = di("wckv", [D, 128]); widx_d = di("widx", [D, 296])
    kvg_d = di("kvg", [128]); wuk_d = di("wuk", [128, 64]); wuv_d = di("wuv", [128, 64])
    b3_d = di("B3raw", [3, 4, 128, 128]); mA_d = di("maskA", [128, 128]); mB_d = di("maskB", [128, 128]); c31_d = di("c31", [128, 4])
    id_d = di("ident", [128, 128])
    y_d = cx.out("y", [NBq * 128, 256])
    p.push()
    stage = [p.sb([128, 512], name="stg%d" % i) for i in range(2)]
    Wq = load_w_bf16(p, wq_d, 256, stage, "Wq"); Wc = load_w_bf16(p, wckv_d, 128, stage, "Wc")
    Widx = p.sb([128, 8, 296], name="Widx"); p.dma('sp', Widx, widx_d.rearrange("(c f) n -> f c n", f=128))
    def ld(src, shape, nm):
        t = p.sb(shape, name=nm); p.dma('sp', t, src); return t
    kvg = p.sb([128, 128]); p.dma('sp', kvg, kvg_d.partition_broadcast(128))
    wuk = ld(wuk_d, [128, 64], "wuk"); wuv = ld(wuv_d, [128, 64], "wuv")
    c31 = ld(c31_d, [128, 4], "c31"); maskA = ld(mA_d, [128, 128], "maskA"); maskB = ld(mB_d, [128, 128], "maskB"); ident = ld(id_d, [128, 128], "ident")
    Badj = [p.sb([128, 4, 128], name="Badj%d" % k) for k in range(3)]
    for k in range(3):
        p.dma('sp', Badj[k], b3_d[k].rearrange("h i s -> i h s"))
        for h in range(4):
            p.ts('dve', Badj[k][:, h, :], Badj[k][:, h, :], c31[:, h:h + 1], None, ALU.subtract)
    eps6 = p.sb([128, 1]); p.memset('dve', eps6, 1e-6)
    kiT = p.sb([32, TK], name="kiT"); kT = p.sb([64, TK], BF16, name="kT"); v_all = p.sb([128, NKT, 64], BF16, name="v_all")
    score = p.sb([128, TK], name="score"); wk = p.sb([128, TK], name="wk")
    hTf = p.sb([128, 8, 128]); hTb = p.sb([128, 8, 128], BF16)
    ct = p.sb([128, 128]); sq = p.sb([128, 128]); cT = p.sb([128, 128]); sm = p.sb([128, 8], name="dsm")
    for kt in range(NKT):
        ks = slice(kt * 128, kt * 128 + 128)
        load_hblock(p, hT_d, kt * 128, 128, hTf, hTb)
        for c in range(8):
            p.mm(PS[0][0:32, 0:128], Widx[:, c, 256:288], hTf[:, c, :], start=(c == 0), stop=(c == 7))
        p.copy('act', kiT[:, ks], PS[0][0:32, 0:128])
        proj_tm(p, PS[1][:, 0:128], Wc, 0, 128, hTb, 0, 128)
        p.copy('act', ct, PS[1][:, 0:128])
        p.tt('dve', sq, ct, ct, ALU.mult)
        p.op('dve', lambda e: e.reduce_sum(sm[:, 0:1], sq, AX.X), r=[sq], w=[sm])
        p.act(sm[:, 0:1], sm[:, 0:1], AF.Sqrt, bias=eps6, scale=1.0 / 128.0)
        p.op('dve', lambda e: e.reciprocal(sm[:, 0:1], sm[:, 0:1]), r=[sm], w=[sm])
        p.stt(ct, ct, sm[:, 0:1], kvg, ALU.mult, ALU.mult)
        p.tr(PS[2][:, 0:128], ct, ident)
        p.copy('act', cT, PS[2][:, 0:128])
        p.mm(PS[3][0:64, 0:128], wuk, cT)
        p.copy('act', kT[:, ks], PS[3][0:64, 0:128])
        p.mm(PS[3][:, 128:192], cT, wuv)
        p.copy('act', v_all[:, kt, :], PS[3][:, 128:192])
    qT = p.sb([64, 4, 128], BF16, name="qT"); qiT = p.sb([32, 8, 128], name="qiT"); wi = p.sb([128, 8], name="wi")
    if fused:
        hTf2 = p.sb([128, 8, 128], name="hTf2"); rkq = p.sb([128, 2], name="rkq"); p.dma('sp', rkq, cx.over["rk"])
    rel = [p.sb([128, 512], name="rel%d" % i) for i in range(2)]
    m8 = p.sb([128, 8], name="m8"); PT = [p.sb([128, 128], BF16, name="PT%d" % i) for i in range(2)]
    yt = p.sb([128, 256], name="yt")
    for bi in range(NSLOT):
        S = min((2 * bi + 2) * 128, TK)
        jA = 2 * bi
        if not fused:
            load_hblock(p, hTq_d, bi * 128, 128, hTf, hTb)
        else:
            hsrc = hT_d.rearrange("(c f) n -> f c n", f=128)
            p.dma('sp', hTf, hsrc[:, :, jA * 128:jA * 128 + 128])
            if (jA + 2) * 128 <= TK:
                p.dma('sp', hTf2, hsrc[:, :, (jA + 1) * 128:(jA + 2) * 128])
            else:
                p.memset('pool', hTf2, 0.0)
            p.ts('dve', hTf, hTf, rkq[:, 0:1], None, ALU.mult)
            p.stt(hTf, hTf2, rkq[:, 1:2], hTf, ALU.mult, ALU.add)
            p.copy('pool', hTb, hTf)
        for h in range(4):
            proj_fm(p, PS[0][0:64, 0:128], Wq, h * 64, 64, hTb, 0, 128)
            p.op('act', lambda e: e.mul(qT[:, h, :], PS[0][0:64, 0:128], 0.125), r=[PS[0]], w=[qT])
        for hi in range(8):
            for c in range(8):
                p.mm(PS[1][0:32, 0:128], Widx[:, c, hi * 32:(hi + 1) * 32], hTf[:, c, :], start=(c == 0), stop=(c == 7))
            p.copy('act', qiT[:, hi, :], PS[1][0:32, 0:128])
        for c in range(8):
            p.mm(PS[2][:, 0:8], hTf[:, c, :], Widx[:, c, 288:296], start=(c == 0), stop=(c == 7))
        p.op('act', lambda e: e.mul(wi, PS[2][:, 0:8], 1.0 / 16.0), r=[PS[2]], w=[wi])
        for k0 in range(0, S, 512):
            kn = min(512, S - k0)
            for hi in range(8):
                pb = PS[3 + hi % 2]
                p.mm(pb[:, 0:kn], qiT[:, hi, :], kiT[:, k0:k0 + kn])
                r_ = rel[hi % 2]
                p.act(r_[:, 0:kn], pb[:, 0:kn], AF.Relu)
                if hi == 0:
                    p.ts('dve', score[:, k0:k0 + kn], r_[:, 0:kn], wi[:, 0:1], None, ALU.mult)
                else:
                    p.stt(score[:, k0:k0 + kn], r_[:, 0:kn], wi[:, hi:hi + 1], score[:, k0:k0 + kn], ALU.mult, ALU.add)
        p.memset('dve', score[:, 0:N_META], 1e30)
        p.tt('dve', score[:, jA * 128:jA * 128 + 128], score[:, jA * 128:jA * 128 + 128], maskA, ALU.min)
        if (jA + 2) * 128 <= S:
            p.tt('dve', score[:, (jA + 1) * 128:(jA + 2) * 128], score[:, (jA + 1) * 128:(jA + 2) * 128], maskB, ALU.min)
        if S > NROUND * 8:
            p.copy('pool', wk[:, 0:S], score[:, 0:S])
            for r in range(NROUND):
                p.op('dve', lambda e: e.max(m8, wk[:, 0:S]), r=[wk], w=[m8])
                if r < NROUND - 1:
                    p.op('dve', lambda e: e.match_replace(wk[:, 0:S], m8, wk[:, 0:S], -3e38), r=[m8, wk], w=[wk])
            p.ts('dve', sm[:, 1:2], m8[:, 7:8], -1e29, None, ALU.max)
        else:
            p.memset('dve', sm[:, 1:2], -1e29)
        p.ts('dve', wk[:, 0:S], score[:, 0:S], sm[:, 1:2], 1.0, ALU.is_ge, ALU.subtract)
        lg = score
        for h in range(4):
            for k0 in range(0, S, 512):
                kn = min(512, S - k0)
                pb = PS[5 + (k0 // 512) % 2]
                p.mm(pb[:, 0:kn], qT[:, h, :], kT[:, k0:k0 + kn])
                p.stt(lg[:, k0:k0 + kn], wk[:, k0:k0 + kn], 1e30, pb[:, 0:kn], ALU.mult, ALU.add)
            for k in range(3):
                jb = jA - 1 + k
                if jb >= 0 and (jb + 1) * 128 <= S:
                    p.tt('dve', lg[:, jb * 128:(jb + 1) * 128], lg[:, jb * 128:(jb + 1) * 128], Badj[k][:, h, :], ALU.add)
            p.op('dve', lambda e: e.reduce_max(sm[:, 2:3], lg[:, 0:S], AX.X), r=[lg], w=[sm])
            p.ts('dve', sm[:, 3:4], sm[:, 2:3], -1.0, None, ALU.mult)
            p.act(lg[:, 0:S], lg[:, 0:S], AF.Exp, bias=sm[:, 3:4], accum_out=sm[:, 4:5])
            nkb = S // 128
            for kb in range(nkb):
                pt = PS[1 + kb % 2]
                p.tr(pt[:, 0:128], lg[:, kb * 128:(kb + 1) * 128], ident)
                p.copy('act' if kb % 2 == 0 else 'dve', PT[kb % 2], pt[:, 0:128])
                p.mm(PS[7][:, 0:64], PT[kb % 2], v_all[:, kb, :], start=(kb == 0), stop=(kb == nkb - 1))
            p.op('dve', lambda e: e.reciprocal(sm[:, 5:6], sm[:, 4:5]), r=[sm], w=[sm])
            p.ts('dve', yt[:, h * 64:(h + 1) * 64], PS[7][:, 0:64], sm[:, 5:6], None, ALU.mult)
        p.dma('sp', y_d[bi * 128:(bi + 1) * 128, :], yt)
    p.pop()


def build_dsa(NKT, NSLOT, NROUND):
    return standalone(emit_dsa, NKT, NSLOT, NROUND)


OFFS = [0, 1024, 1808, 2488, 3520, 7616]


def _c(a):
    return np.ascontiguousarray(a, dtype=np.float32)


def _t5_bucket(n):
    n = np.maximum(n, 0)
    me = 16
    large = me + (np.log(np.maximum(n, 1).astype(np.float32) / me) / np.log(128 / me) * (32 - me)).astype(np.int32)
    return np.where(n < me, n, np.minimum(large, 31))


def _run(nc, in_maps):
    res = run_bass_kernel_spmd(nc, in_maps, core_ids=list(range(NCORES)))
    return res.results


def _gla_inputs(inp, l, hp, hT, NCH):
    w_in = inp['w_in'][l]; c0 = OFFS[1]
    heads = [2 * hp, 2 * hp + 1]
    qcols = np.concatenate([np.arange(c0 + hh * 32, c0 + hh * 32 + 32) for hh in heads])
    kcols = qcols + 128
    vcols = np.concatenate([np.arange(c0 + 256 + hh * 64, c0 + 256 + hh * 64 + 64) for hh in heads])
    acols = np.arange(c0 + 512, c0 + 528)
    ocols = vcols + 256 + 16
    return dict(hT=hT, wqk=_c(w_in[:, np.concatenate([qcols, kcols])]), wvo=_c(w_in[:, np.concatenate([vcols, ocols])]),
                wa=_c(w_in[:, acols]), aup=_c(inp['gla_a_up'][l][:, hp * 64:(hp + 1) * 64]),
                ab=_c(inp['gla_a_b'][l][hp * 64:(hp + 1) * 64, None]), ng=_c(np.tile(inp['gla_norm_g'][l], 2)),
                cmask=(np.arange(NCH * 64) % 64 != 0).astype(np.float32),
                maskU2=_c(np.tile(np.triu(np.ones((64, 64), np.float32)), (1, 2))), ident=np.eye(128, dtype=np.float32))


def _mlstm_inputs(inp, l, hp, hT, NCH):
    w_in = inp['w_in'][l]; c0 = OFFS[3]
    heads = [2 * hp, 2 * hp + 1]
    hc = np.concatenate([np.arange(hh * 64, hh * 64 + 64) for hh in heads])
    P = NCH * 64; pos = np.arange(P); real = (pos >= 48)
    return dict(hT=hT, wq=_c(w_in[:, c0 + hc]), wk=_c(w_in[:, c0 + 256 + hc]),
                wvo=_c(w_in[:, np.concatenate([c0 + 512 + hc, c0 + 776 + hc])]),
                wi=_c(np.repeat(w_in[:, [c0 + 768 + hh for hh in heads]], 64, axis=1)),
                wf=_c(np.repeat(w_in[:, [c0 + 772 + hh for hh in heads]], 64, axis=1)),
                cwq=_c(inp['mlstm_conv_w'][l][:, hc].T), cwk=_c(inp['mlstm_conv_w'][l][:, 256 + hc].T),
                cbq=_c(inp['mlstm_conv_b'][l][hc, None]), cbk=_c(inp['mlstm_conv_b'][l][256 + hc, None]),
                ib=_c(np.repeat(inp['mlstm_i_b'][l][heads], 64)[:, None]), fb=_c(np.repeat(inp['mlstm_f_b'][l][heads], 64)[:, None]),
                ng=_c(inp['mlstm_norm_g'][l][hc]), cmask=(pos % 64 != 0).astype(np.float32),
                pm01=real.astype(np.float32), pmneg=np.where(real, 0.0, -1e30).astype(np.float32),
                maskL=np.where(np.tril(np.ones((64, 64))) > 0, 0.0, -1e30).astype(np.float32), ident=np.eye(128, dtype=np.float32))


def _rwkv_inputs(inp, l, hp, hT):
    w_in = inp['w_in'][l]
    hc = np.arange(hp * 128, hp * 128 + 128)
    colsel = np.concatenate([hc, 256 + hc, 512 + hc, np.arange(768, 832), np.arange(832, 896), np.arange(896, 1024)])
    mu = inp['rwkv_mu'][l]
    mu6 = np.zeros((128, 6), np.float32)
    mu6[:, 0] = mu[hc]; mu6[:, 1] = mu[256 + hc]; mu6[:, 2] = mu[512 + hc]
    mu6[:64, 3] = mu[768:832]; mu6[:64, 4] = mu[832:896]; mu6[:, 5] = mu[896:1024]
    cols = np.zeros((128, 8), np.float32)
    cols[:, 0] = inp['rwkv_w0'][l][hc]; cols[:, 1] = inp['rwkv_a0'][l][hc]; cols[:, 2] = inp['rwkv_k_k'][l][hc]
    cols[:, 3] = inp['rwkv_k_a'][l][hc]; cols[:, 4] = inp['rwkv_r_k'][l].reshape(-1)[hc]
    bo = np.zeros((128, 128), np.float32); bo[:64, :64] = 1; bo[64:, 64:] = 1
    return dict(hT=hT, w=_c(w_in[:, colsel]), mu=mu6, wup=_c(inp['rwkv_w_up'][l][:, hc]), aup=_c(inp['rwkv_a_up'][l][:, hc]),
                gup=_c(inp['rwkv_g_up'][l][:, hc]), cols=cols, gng=_c(inp['rwkv_gn_g'][l][hc]), gnb=_c(inp['rwkv_gn_b'][l][hc]),
                blockones=bo, ident=np.eye(128, dtype=np.float32))


def _dsa_inputs(inp, l, half, hT_tok, NSLOT):
    w_in = inp['w_in'][l]; c0 = OFFS[2]; rb = inp['rel_bias']
    i = np.arange(128)[:, None]; s = np.arange(128)[None, :]
    bd = _t5_bucket(i - s); bp = _t5_bucket(128 + i - s)
    Draw = np.stack([np.where(s <= i, rb[bd, hh], rb[31, hh]) for hh in range(4)]).astype(np.float32)
    Praw = np.stack([rb[bp, hh] for hh in range(4)]).astype(np.float32)
    c31t = np.stack([np.full((128, 128), rb[31, hh]) for hh in range(4)]).astype(np.float32)
    B3 = np.stack([Praw, Draw, c31t]) if half == 0 else np.stack([c31t, Praw, Draw])
    mm = np.where(s <= i, 3e38, -1e30).astype(np.float32)
    maskA = mm if half == 0 else np.full((128, 128), 3e38, np.float32)
    maskB = np.full((128, 128), -1e30, np.float32) if half == 0 else mm
    hTq = np.zeros((1024, NSLOT * 128), np.float32)
    for ii in range(NSLOT):
        j = 2 * ii + half
        if j * 128 < hT_tok.shape[1]:
            hTq[:, ii * 128:(ii + 1) * 128] = hT_tok[:, j * 128:(j + 1) * 128]
    return dict(hT=hT_tok, hTq=hTq, B3raw=_c(B3), maskA=maskA, maskB=maskB,
                wq=_c(w_in[:, c0:c0 + 256]), wckv=_c(w_in[:, c0 + 256:c0 + 384]), widx=_c(w_in[:, c0 + 384:c0 + 680]),
                kvg=_c(inp['dsa_kv_norm_g'][l]), wuk=_c(inp['dsa_w_uk'][l]), wuv=_c(inp['dsa_w_uv'][l]),
                c31=_c(np.tile(rb[31][None, :], (128, 1))), ident=np.eye(128, dtype=np.float32))


def build_fused(NTL, NCH, NSTEP, NKT, NSLOT, NROUND, HALF):
    nc, p, PS = new_prog()
    NTOK = NTL * 128; P_ = NCH * 64; TK = NKT * 128
    PW = max(48 + HALF + NTOK, 48 + TK, P_)
    PWy = PW
    groups = [[2 * g, 2 * g + 1] for g in range(NCORES // 2)]
    ext = lambda n, s_: nc.dram_tensor(n, list(s_), F32, kind="ExternalInput").ap()
    itn = lambda n, s_: nc.dram_tensor(n, list(s_), F32, kind="Internal").ap()
    ident_d = ext("ident", [128, 128]); rk_d = ext("rk", [128, 2]); x_d = ext("x", [NTOK, D])
    out_d = nc.dram_tensor("out", [NTOK, D], F32, kind="ExternalOutput").ap()
    h_own = itn("h_own", [NTOK, D]); hT_own = itn("hT_own", [D, NTOK])
    hT_pos = itn("hT_pos", [D, PW]); Y_abd = itn("Y_abd", [PWy, 384]); Y_c = itn("Y_c", [NSLOT * 128, 256])

    class Gathered:
        def __init__(self, name, src, bounds):
            self.src, self.bounds = src, bounds
            self.bufs = [itn("%s_%d" % (name, k), [2 * (b1 - b0), src.shape[1]]) for k, (b0, b1) in enumerate(bounds)]

        def gather(self):
            for (b0, b1), g in zip(self.bounds, self.bufs):
                p.coll("AllGather", self.src[b0:b1, :], g, groups)

        def rows(self, q, r0, n):
            for (b0, b1), g in zip(self.bounds, self.bufs):
                if b0 <= r0 and r0 + n <= b1:
                    return g[q * (b1 - b0) + r0 - b0:q * (b1 - b0) + r0 - b0 + n, :]
            raise ValueError("row range straddles gather chunks")

    def bounds_of(total, step, first=None):
        bs = []
        b0 = 0
        nxt = first if first is not None else step
        while b0 < total:
            b1 = min(total, nxt)
            bs.append((b0, b1)); b0 = b1; nxt = b1 + step
        return bs
    G_h = Gathered("G_h", hT_own, bounds_of(D, 64))
    G_abd = Gathered("G_abd", Y_abd, bounds_of(PWy, 1024, 48 + 1024))
    G_c = Gathered("G_c", Y_c, bounds_of(NSLOT * 128, 1024))
    p.push()
    z = p.sb([128, 512], name="zeros"); p.memset('dve', z, 0.0)
    for c in range(8):
        p.dma('sp', hT_pos[c * 128:(c + 1) * 128, 0:48], z[:, 0:48])
    for r0 in range(0, PWy, 128):
        n = min(128, PWy - r0)
        p.dma('sp', Y_abd[r0:r0 + n, :], z[0:n, 0:384])
    p.pop()
    emit_ln_in(Ctx(nc, p, PS, "ln_", dict(x=x_d, y=h_own, hT_out=hT_own, ident=ident_d)), NTL)
    for l in range(DEPTH):
        pre = "l%d_" % l
        G_h.gather()
        for q in range(2):
            for (b0, b1) in G_h.bounds:
                p.dma('sp', hT_pos[b0:b1, 48 + HALF * q:48 + HALF * q + NTOK], G_h.rows(q, b0, b1 - b0))
        emit_rwkv(Ctx(nc, p, PS, pre + "rwkv_", dict(hT=hT_pos[:, 0:P_], y=Y_abd[0:P_, 0:128], ident=ident_d)), NCH, NSTEP)
        gg = gla_gen(Ctx(nc, p, PS, pre + "gla_", dict(hT=hT_pos[:, 0:P_], y=Y_abd[0:P_, 128:256], ident=ident_d)), NCH)
        next(gg)
        p.bg = gg
        emit_mlstm(Ctx(nc, p, PS, pre + "mlstm_", dict(hT=hT_pos[:, 0:P_], y=Y_abd[0:P_, 256:384], ident=ident_d)), NCH)
        p.drain_bg()
        emit_dsa(Ctx(nc, p, PS, pre + "dsa_", dict(hT=hT_pos[:, 48:48 + TK], y=Y_c, ident=ident_d, rk=rk_d)), NKT, NSLOT, NROUND)
        G_abd.gather()
        G_c.gather()
        last = (l == DEPTH - 1)
        over = dict(h=h_own, hT=hT_own, G_abd=G_abd, G_c=G_c, rk=rk_d, PWy=PWy, HALF=HALF, NSLOT=NSLOT, ident=ident_d,
                    out=(out_d if last else h_own))
        if not last:
            over["hT_out"] = hT_own
        emit_post(Ctx(nc, p, PS, pre + "post_", over), NTL, 17)
    p.finish()
    return nc


def kernel(**inputs):
    inp = {k: np.asarray(v) for k, v in inputs.items()}
    x = inp['x'].astype(np.float32)
    B, S, _ = x.shape
    T = S + N_META
    HALF = S // 2
    NTL = -(-(T - HALF) // 128)
    NTOK = NTL * 128
    NCH = -(-(T + 48 + 1) // 64); NCH += NCH % 2
    NSTEP = 48 + T + 1
    NKT = -(-T // 128)
    NSLOT = (NKT + 1) // 2
    NROUND = min(256, S // 4) // 8
    assert B * 2 == NCORES and (HALF // 128) % 2 == 0
    nc = build_fused(NTL, NCH, NSTEP, NKT, NSLOT, NROUND, HALF)
    hcat = np.concatenate([np.broadcast_to(inp['meta'][None].astype(np.float32), (B, N_META, D)), x], 1)
    maps = []
    for c in range(NCORES):
        b, r = c // 2, c % 2
        xo = np.zeros((NTOK, D), np.float32)
        seg = hcat[b, r * HALF:min(T, r * HALF + NTOK)] if r == 1 else hcat[b, 0:HALF]
        xo[:len(seg)] = seg
        m = dict(x=xo, ident=np.eye(128, dtype=np.float32), rk=np.tile(np.array([[1.0 - r, float(r)]], np.float32), (128, 1)))
        m["ln_g"] = _c(inp['ln_in_g']); m["ln_b"] = _c(inp['ln_in_b'])
        dummy = np.zeros((D, 1), np.float32)
        for l in range(DEPTH):
            pre = "l%d_" % l
            for nm, d in (("rwkv_", _rwkv_inputs(inp, l, r, dummy)), ("gla_", _gla_inputs(inp, l, r, dummy, NCH)),
                          ("mlstm_", _mlstm_inputs(inp, l, r, dummy, NCH)), ("dsa_", _dsa_inputs(inp, l, r, dummy, 0))):
                for k, v in d.items():
                    if k not in ("hT", "hTq", "ident"):
                        m[pre + nm + k] = v
            wr = _c(np.concatenate([inp['moe_w_grp'][l], inp['moe_w_rt'][l]], 1))
            br = _c(np.concatenate([inp['moe_b_grp'][l], inp['moe_b_rt'][l]], 0))
            post = dict(wg=_c(inp['w_in'][l][:, OFFS[4]:OFFS[5]]), wb=_c(inp['w_branch'][l].reshape(D, D)), wo=_c(inp['w_out'][l]),
                        ln1g=_c(inp['ln1_g'][l]), ln1b=_c(inp['ln1_b'][l]), ln2g=_c(inp['ln2_g'][l]), ln2b=_c(inp['ln2_b'][l]),
                        wr=wr, br=br, mg=_c(inp['moe_w_gate'][l]), mu=_c(inp['moe_w_up'][l]), md=_c(inp['moe_w_down'][l]))
            for k, v in post.items():
                m[pre + "post_" + k] = v
        maps.append(m)
    res = _run(nc, maps)
    out = np.zeros((B, T, D), np.float32)
    for c in range(NCORES):
        b, r = c // 2, c % 2
        n = HALF if r == 0 else T - HALF
        out[b, r * HALF:r * HALF + n] = res[c]["out"][:n]
    return np.ascontiguousarray(out[:, N_META:])


def kernel_unfused(**inputs):
    inp = {k: np.asarray(v) for k, v in inputs.items()}
    x = inp['x'].astype(np.float32)
    B, S, _ = x.shape
    T = S + N_META
    NTOKC = (B * T) // NCORES
    NTL = -(-NTOKC // 128)
    NCH = -(-(T + 48) // 64); NCH += NCH % 2
    if NCH * 64 < 48 + T + 1:
        NCH += 2
    P = NCH * 64
    NSTEP = 48 + T + 1
    NKT = -(-T // 128)
    NSLOT = (NKT + 1) // 2
    NROUND = min(256, S // 4) // 8
    ident = np.eye(128, dtype=np.float32)

    def tok_split(a):
        out = []
        for c in range(NCORES):
            sh = np.zeros((NTL * 128, a.shape[1]), np.float32)
            sh[:NTOKC] = a[c * NTOKC:(c + 1) * NTOKC]
            out.append(sh)
        return out

    def tok_merge(res, key):
        return np.concatenate([r[key][:NTOKC] for r in res], 0)

    hcat = np.concatenate([np.broadcast_to(inp['meta'][None], (B, N_META, D)), x], 1).reshape(B * T, D)
    res = _run(build_ln_in(NTL), [dict(x=s_, g=_c(inp['ln_in_g']), b=_c(inp['ln_in_b'])) for s_ in tok_split(hcat)])
    h = tok_merge(res, "y")

    for l in range(DEPTH):
        hb = h.reshape(B, T, D)
        hTpos = []
        hTtok = []
        for b in range(B):
            a = np.zeros((D, P), np.float32); a[:, 48:48 + T] = hb[b].T; hTpos.append(a)
            a2 = np.zeros((D, NKT * 128), np.float32); a2[:, :T] = hb[b].T; hTtok.append(a2)
        y = np.zeros((B, T, D), np.float32)
        res = _run(build_rwkv(NCH, NSTEP), [_rwkv_inputs(inp, l, c % 2, hTpos[c // 2]) for c in range(NCORES)])
        for c in range(NCORES):
            y[c // 2, :, (c % 2) * 128:(c % 2) * 128 + 128] = res[c]["y"][48:48 + T]
        res = _run(build_gla(NCH), [_gla_inputs(inp, l, c % 2, hTpos[c // 2], NCH) for c in range(NCORES)])
        for c in range(NCORES):
            y[c // 2, :, 256 + (c % 2) * 128:256 + (c % 2) * 128 + 128] = res[c]["y"][48:48 + T]
        res = _run(build_mlstm(NCH), [_mlstm_inputs(inp, l, c % 2, hTpos[c // 2], NCH) for c in range(NCORES)])
        for c in range(NCORES):
            y[c // 2, :, 768 + (c % 2) * 128:768 + (c % 2) * 128 + 128] = res[c]["y"][48:48 + T]
        res = _run(build_dsa(NKT, NSLOT, NROUND), [_dsa_inputs(inp, l, c % 2, hTtok[c // 2], NSLOT) for c in range(NCORES)])
        for c in range(NCORES):
            half = c % 2
            for ii in range(NSLOT):
                j = 2 * ii + half
                if j * 128 >= T:
                    continue
                n = min(128, T - j * 128)
                y[c // 2, j * 128:j * 128 + n, 512:768] = res[c]["y"][ii * 128:ii * 128 + n]
        yf = y.reshape(B * T, D)
        hs = tok_split(h); ys = tok_split(yf)
        wr = _c(np.concatenate([inp['moe_w_grp'][l], inp['moe_w_rt'][l]], 1))
        br = _c(np.concatenate([inp['moe_b_grp'][l], inp['moe_b_rt'][l]], 0))
        common = dict(wg=_c(inp['w_in'][l][:, OFFS[4]:OFFS[5]]), wb=_c(inp['w_branch'][l].reshape(D, D)), wo=_c(inp['w_out'][l]),
                      ln1g=_c(inp['ln1_g'][l]), ln1b=_c(inp['ln1_b'][l]), ln2g=_c(inp['ln2_g'][l]), ln2b=_c(inp['ln2_b'][l]),
                      wr=wr, br=br, mg=_c(inp['moe_w_gate'][l]), mu=_c(inp['moe_w_up'][l]), md=_c(inp['moe_w_down'][l]), ident=ident)
        maps = [dict(h=hs[c], hT=_c(hs[c].T), yT=_c(ys[c].T), **common) for c in range(NCORES)]
        res = _run(build_post(NTL, 8), maps)
        h = tok_merge(res, "out")
    return np.ascontiguousarray(h.reshape(B, T, D)[:, N_META:]).astype(np.float32)
```

```python
import numpy as np
import concourse.bass as bass
import concourse.mybir as mybir
from concourse.bass_utils import run_bass_kernel_spmd
from contextlib import ExitStack

F32 = mybir.dt.float32
BF16 = mybir.dt.bfloat16
ALU = mybir.AluOpType
AF = mybir.ActivationFunctionType
AX = mybir.AxisListType

D = 1024
DEPTH = 2
N_META = 16
DN_ALPHA = (2 * DEPTH) ** 0.25
LN_EPS = 1e-5
NCORES = 8

ENGS = ('pe', 'act', 'dve', 'pool', 'sp')
N_DMA_SEMS = 16
CC_INC = 1


class Prog:
    def __init__(self, nc):
        self.nc = nc
        self.E = {'pe': nc.tensor, 'act': nc.scalar, 'dve': nc.vector, 'pool': nc.gpsimd, 'sp': nc.sync}
        self.sem = {e: nc.alloc_semaphore("s_" + e) for e in ENGS}
        self.cnt = {e: 0 for e in ENGS}
        self.dsem = [nc.alloc_semaphore("d_%d" % i) for i in range(N_DMA_SEMS)]
        self.dcnt = [0] * N_DMA_SEMS
        self.dnext = 0
        self.known = {e: {} for e in ENGS}
        self.last_w = {}
        self.readers = {}
        self.semobj = {}
        for e in ENGS:
            self.semobj['s_' + e] = self.sem[e]
        for i in range(N_DMA_SEMS):
            self.semobj['d_%d' % i] = self.dsem[i]
        self.n_ins = 0
        self.n_wait = 0
        self._uid = 0
        self.stacks = []
        self.bg = None

    def sb(self, shape, dt=F32, name=None):
        self._uid += 1
        nm = (name or "t") + "_%d" % self._uid
        if self.stacks:
            return self.stacks[-1].enter_context(self.nc.sbuf_tensor(nm, list(shape), dt)).ap()
        return self.nc.alloc_sbuf_tensor(nm, list(shape), dt).ap()

    def push(self):
        self.stacks.append(ExitStack())

    def pop(self):
        self.barrier()
        self.stacks.pop().close()

    def barrier(self):
        for e in ENGS:
            for f in ENGS:
                if f != e and self.cnt[f] > self.known[e].get('s_' + f, 0):
                    self.E[e].wait_ge(self.sem[f], self.cnt[f])
                    self.known[e]['s_' + f] = self.cnt[f]
            for i in range(N_DMA_SEMS):
                sn = 'd_%d' % i
                if self.dcnt[i] > self.known[e].get(sn, 0):
                    self.E[e].wait_ge(self.dsem[i], self.dcnt[i])
                    self.known[e][sn] = self.dcnt[i]

    def ps(self, shape, dt=F32, name=None):
        self._uid += 1
        return self.nc.alloc_psum_tensor(name or ("p%d" % self._uid), list(shape), dt).ap()

    @staticmethod
    def key_of(x):
        if isinstance(x, (str, tuple)):
            return x
        return x.tensor.name

    def _deps(self, eng, reads, writes):
        need = {}

        def add(sn, v, prod_eng):
            if prod_eng == eng and eng == 'pe':
                return
            if need.get(sn, 0) < v:
                need[sn] = v
        for k in reads:
            w = self.last_w.get(k)
            if w is not None:
                add(*w)
        for k in writes:
            w = self.last_w.get(k)
            if w is not None:
                add(*w)
            for (sn, v, pe) in self.readers.get(k, {}).values():
                if pe == eng:
                    continue
                add(sn, v, pe)
        for sn, v in need.items():
            if self.known[eng].get(sn, 0) < v:
                self.E[eng].wait_ge(self.semobj[sn], v)
                self.known[eng][sn] = v
                self.n_wait += 1

    def _record(self, reads, writes, tok):
        for k in reads:
            self.readers.setdefault(k, {})[tok[0]] = tok
        for k in writes:
            self.last_w[k] = tok
            self.readers[k] = {}

    def op(self, eng, fn, r=(), w=()):
        reads = [self.key_of(x) for x in r]
        writes = [self.key_of(x) for x in w]
        self._deps(eng, reads, writes)
        ins = fn(self.E[eng])
        self.cnt[eng] += 1
        ins.then_inc(self.sem[eng], 1)
        tok = ('s_' + eng, self.cnt[eng], eng)
        self._record(reads, writes, tok)
        self.n_ins += 1
        return ins

    def bgsteps(self, n):
        for _ in range(n):
            if self.bg is None:
                return
            try:
                next(self.bg)
            except StopIteration:
                self.bg = None

    def drain_bg(self):
        while self.bg is not None:
            self.bgsteps(1)

    def dma(self, eng, out, in_, r=None, w=None, **kw):
        reads = [self.key_of(x) for x in (r if r is not None else [in_])]
        writes = [self.key_of(x) for x in (w if w is not None else [out])]
        self._deps(eng, reads, writes)
        i = self.dnext
        self.dnext = (self.dnext + 1) % N_DMA_SEMS
        sn = 'd_%d' % i
        if self.known[eng].get(sn, 0) < self.dcnt[i]:
            self.E[eng].wait_ge(self.dsem[i], self.dcnt[i])
            self.known[eng][sn] = self.dcnt[i]
        ins = self.E[eng].dma_start(out=out, in_=in_, **kw)
        self.dcnt[i] += 16
        ins.then_inc(self.dsem[i], 16)
        tok = (sn, self.dcnt[i], 'dma')
        self._record(reads, writes, tok)
        self.n_ins += 1
        return ins

    def coll(self, kind, in_, out, groups):
        eng = 'pool'
        reads = [self.key_of(in_)]
        writes = [self.key_of(out)]
        self._deps(eng, reads, writes)
        i = self.dnext
        self.dnext = (self.dnext + 1) % N_DMA_SEMS
        sn = 'd_%d' % i
        if self.known[eng].get(sn, 0) < self.dcnt[i]:
            self.E[eng].wait_ge(self.dsem[i], self.dcnt[i])
            self.known[eng][sn] = self.dcnt[i]
        ins = self.nc.gpsimd.collective_compute(kind, ALU.bypass, replica_groups=groups, ins=[in_.opt()], outs=[out.opt()])
        self.dcnt[i] += CC_INC
        ins.then_inc(self.dsem[i], CC_INC)
        tok = (sn, self.dcnt[i], 'dma')
        self._record(reads, writes, tok)
        self.n_ins += 1
        return ins

    def finish(self, eng='sp'):
        for i in range(N_DMA_SEMS):
            if self.dcnt[i] > 0:
                self.E[eng].wait_ge(self.dsem[i], self.dcnt[i])
        for e in ENGS:
            if self.cnt[e] > 0 and e != eng:
                self.E[eng].wait_ge(self.sem[e], self.cnt[e])

    def mm(self, out, lhsT, rhs, start=True, stop=True, **kw):
        return self.op('pe', lambda e: e.matmul(out, lhsT, rhs, start=start, stop=stop, **kw),
                       r=[lhsT, rhs], w=[out])

    def tr(self, out, in_, ident):
        return self.op('pe', lambda e: e.transpose(out, in_, ident), r=[in_, ident], w=[out])

    def act(self, out, in_, func, bias=None, scale=1.0, accum_out=None, eng='act'):
        r = [in_]
        w = [out]
        kw = {}
        if bias is not None:
            kw['bias'] = bias
            if not isinstance(bias, (int, float)):
                r.append(bias)
        if not isinstance(scale, (int, float)):
            r.append(scale)
        if accum_out is not None:
            kw['accum_out'] = accum_out
            w.append(accum_out)
        return self.op('act', lambda e: e.activation(out, in_, func, scale=scale, **kw), r=r, w=w)

    def tt(self, eng, out, a, b, op):
        return self.op(eng, lambda e: e.tensor_tensor(out, a, b, op), r=[a, b], w=[out])

    def ts(self, eng, out, a, s1, s2, op0, op1=None, accum_out=None):
        r = [a] + [s for s in (s1, s2) if s is not None and not isinstance(s, (int, float))]
        w = [out] + ([accum_out] if accum_out is not None else [])
        kw = {}
        if accum_out is not None:
            kw['accum_out'] = accum_out
        if op1 is None:
            return self.op(eng, lambda e: e.tensor_scalar(out, a, s1, None, op0, **kw), r=r, w=w)
        return self.op(eng, lambda e: e.tensor_scalar(out, a, s1, s2, op0, op1, **kw), r=r, w=w)

    def stt(self, out, a, s, b, op0, op1, accum_out=None):
        r = [a, b] + ([s] if not isinstance(s, (int, float)) else [])
        w = [out] + ([accum_out] if accum_out is not None else [])
        kw = {}
        if accum_out is not None:
            kw['accum_out'] = accum_out
        return self.op('dve', lambda e: e.scalar_tensor_tensor(out, a, s, b, op0, op1, **kw), r=r, w=w)

    def copy(self, eng, out, in_):
        if eng == 'act':
            return self.op('act', lambda e: e.copy(out, in_), r=[in_], w=[out])
        return self.op(eng, lambda e: e.tensor_copy(out, in_), r=[in_], w=[out])

    def memset(self, eng, out, v):
        return self.op(eng, lambda e: e.memset(out, v), w=[out])


class Ctx:
    def __init__(self, nc, p, PS, prefix="", over=None):
        self.nc, self.p, self.PS, self.prefix, self.over = nc, p, PS, prefix, dict(over or {})

    def inp(self, n, s):
        if n in self.over:
            return self.over[n]
        return self.nc.dram_tensor(self.prefix + n, list(s), F32, kind="ExternalInput").ap()

    def out(self, n, s):
        if n in self.over:
            return self.over[n]
        return self.nc.dram_tensor(self.prefix + n, list(s), F32, kind="ExternalOutput").ap()

    def scratch(self, n, s, dt=F32):
        return self.nc.dram_tensor(self.prefix + n, list(s), dt, kind="Internal").ap()


def new_prog():
    nc = bass.Bass("TRN2", target_bir_lowering=False)
    p = Prog(nc)
    PS = [p.ps([128, 512], name="bank%d" % i) for i in range(8)]
    return nc, p, PS


def standalone(emit, *a, **k):
    nc, p, PS = new_prog()
    emit(Ctx(nc, p, PS), *a, **k)
    p.finish()
    return nc


def emit_transposed(p, PS, src, dstT_d, cols, ident, tmpT):
    for c in range(8):
        pt = PS[4 + c // 4]
        p.tr(pt[:, (c % 4) * 128:(c % 4 + 1) * 128], src[:, c * 128:(c + 1) * 128], ident)
        if c % 4 == 3:
            p.copy('act', tmpT[:, c - 3:c + 1, :], pt.rearrange("p (a n) -> p a n", a=4))
    p.dma('sp', dstT_d.rearrange("(c f) n -> f c n", f=128)[:, :, cols], tmpT)


def emit_ln(p, xt, out, G, Bt, tmp, eps):
    st, mv, rs = tmp
    for c in range(2):
        p.op('dve', lambda e: e.bn_stats(st[:, c * 6:(c + 1) * 6], xt[:, c * 512:(c + 1) * 512]), r=[xt], w=[st])
    p.op('dve', lambda e: e.bn_aggr(mv, st.rearrange("p (c s) -> p c s", s=6)), r=[st], w=[mv])
    p.act(rs, mv[:, 1:2], AF.Sqrt, bias=eps)
    p.op('dve', lambda e: e.reciprocal(rs, rs), r=[rs], w=[rs])
    p.ts('dve', out, xt, mv[:, 0:1], rs[:, 0:1], ALU.subtract, ALU.mult)
    p.tt('pool', out, out, G, ALU.mult)
    p.tt('pool', out, out, Bt, ALU.add)


def load_cast(p, dst, src, stage, chunk_cols, engs=('pool', 'act')):
    a, n = dst.shape[1], dst.shape[2]
    chunk_cols = min(chunk_cols, stage[0].shape[1])
    k = 0
    for i in range(a):
        for c0 in range(0, n, chunk_cols):
            c1 = min(n, c0 + chunk_cols)
            stg = stage[k % len(stage)]
            p.dma('sp', stg[:, 0:c1 - c0], src[:, i, c0:c1])
            p.copy(engs[k % len(engs)], dst[:, i, c0:c1], stg[:, 0:c1 - c0])
            k += 1


def emit_ln_in(cx, NTL):
    nc, p, PS = cx.nc, cx.p, cx.PS
    x = cx.inp("x", [NTL * 128, D]); g = cx.inp("g", [D]); b = cx.inp("b", [D])
    y = cx.out("y", [NTL * 128, D])
    hT_out = cx.over.get("hT_out")
    p.push()
    G = p.sb([128, D]); Bt = p.sb([128, D]); eps = p.sb([128, 1])
    p.memset('dve', eps, LN_EPS)
    p.dma('sp', G, g.partition_broadcast(128))
    p.dma('sp', Bt, b.partition_broadcast(128))
    xts = [p.sb([128, D]) for _ in range(2)]
    ots = [p.sb([128, D]) for _ in range(2)]
    tmp = (p.sb([128, 12]), p.sb([128, 2]), p.sb([128, 1]))
    if hT_out is not None:
        ident = p.sb([128, 128]); p.dma('sp', ident, cx.inp("ident", [128, 128]))
        tmpT = p.sb([128, 8, 128])
    for t in range(NTL):
        xt = xts[t % 2]; ot = ots[t % 2]
        p.dma('sp', xt, x[t * 128:(t + 1) * 128, :])
        emit_ln(p, xt, ot, G, Bt, tmp, eps)
        p.dma('sp', y[t * 128:(t + 1) * 128, :], ot)
        if hT_out is not None:
            emit_transposed(p, PS, ot, hT_out, slice(t * 128, (t + 1) * 128), ident, tmpT)
    p.pop()


def build_ln_in(NTL):
    return standalone(emit_ln_in, NTL)


def emit_post(cx, NTL, GT=8):
    nc, p, PS = cx.nc, cx.p, cx.PS
    NTOK = NTL * 128
    di = cx.inp
    h_d = di("h", [NTOK, D]); hT_d = di("hT", [D, NTOK])
    fused = "G_abd" in cx.over
    if fused:
        G_abd, G_c, rk_d = cx.over["G_abd"], cx.over["G_c"], cx.over["rk"]
        PWy, HALF, NSLOT = cx.over["PWy"], cx.over["HALF"], cx.over["NSLOT"]
    else:
        yT_d = di("yT", [D, NTOK])
    hT_out = cx.over.get("hT_out")
    wg_d = di("wg", [D, 4 * D]); wb_d = di("wb", [D, D]); wo_d = di("wo", [D, D])
    ln1g = di("ln1g", [D]); ln1b = di("ln1b", [D]); ln2g = di("ln2g", [D]); ln2b = di("ln2b", [D])
    wr_d = di("wr", [D, 20]); br_d = di("br", [20])
    mg_d = di("mg", [16, D, 256]); mu_d = di("mu", [16, D, 256]); md_d = di("md", [16, 256, D])
    id_d = di("ident", [128, 128])
    out_d = cx.out("out", [NTOK, D])
    h1_d = cx.scratch("h1s", [NTOK, D], F32)
    xT_d = cx.scratch("xTs", [128, 8, NTOK], BF16)
    p.push()
    ident = p.sb([128, 128]); p.dma('sp', ident, id_d)
    eps = p.sb([128, 1]); p.memset('dve', eps, LN_EPS)
    G1 = p.sb([128, D]); B1 = p.sb([128, D]); G2 = p.sb([128, D]); B2 = p.sb([128, D])
    for tdst, src in ((G1, ln1g), (B1, ln1b), (G2, ln2g), (B2, ln2b)):
        p.dma('sp', tdst, src.partition_broadcast(128))
    Wr = p.sb([128, 8, 20]); p.dma('sp', Wr, wr_d.rearrange("(c f) n -> f c n", f=128))
    Br = p.sb([128, 20]); p.dma('sp', Br, br_d.partition_broadcast(128))
    gate_all = p.sb([128, NTL, 16])
    lntmp = (p.sb([128, 12]), p.sb([128, 2]), p.sb([128, 1]))
    stage = [p.sb([128, 2048], name="stg%d" % i) for i in range(2)]

    p.push()
    Wg = p.sb([128, 8, 4 * D], BF16, name="Wg")
    Wb = p.sb([128, 8, D], BF16, name="Wb")
    Wo = p.sb([128, 8, D], BF16, name="Wo")
    load_cast(p, Wg, wg_d.rearrange("(c f) n -> f c n", f=128), stage, 2048)
    load_cast(p, Wb, wb_d.rearrange("(c f) n -> f c n", f=128), stage, 2048)
    load_cast(p, Wo, wo_d.rearrange("(c f) n -> f c n", f=128), stage, 2048)
    hTf = p.sb([128, 8, 128]); hTb = p.sb([128, 8, 128], BF16)
    yTf = p.sb([128, 8, 128]); yTb = p.sb([128, 8, 128], BF16)
    if fused:
        rk = p.sb([128, 2], name="rk"); p.dma('sp', rk, rk_d)
        ycand = [p.sb([128, D], name="ycand%d" % i) for i in range(2)]
        ytile = p.sb([128, D], name="ytile")
    ht = p.sb([128, D]); gsb = [p.sb([128, 512]) for _ in range(2)]
    merged = p.sb([128, D]); tmpm = p.sb([128, 512])
    mTb = p.sb([128, 8, 128], BF16)
    pre = p.sb([128, D]); h1 = p.sb([128, D])
    x32 = p.sb([128, 8, 128]); xb = p.sb([128, 8, 128], BF16)
    lg = p.sb([128, 20]); sm = p.sb([128, 16], name="smallr")
    gm4 = p.sb([128, 4]); ge4 = p.sb([128, 4]); em = p.sb([128, 16]); elm = p.sb([128, 16])
    top8 = p.sb([128, 8]); g0 = p.sb([128, 16]); g1 = p.sb([128, 16])
    for t in range(NTL):
        ts_ = slice(t * 128, (t + 1) * 128)
        p.dma('sp', hTf, hT_d.rearrange("(c f) n -> f c n", f=128)[:, :, ts_])
        p.dma('sp', ht, h_d[ts_, :])
        p.copy('pool', hTb, hTf)
        if not fused:
            p.dma('sp', yTf, yT_d.rearrange("(c f) n -> f c n", f=128)[:, :, ts_])
            p.copy('pool', yTb, yTf)
        else:
            for rc in range(2):
                r0 = 48 + HALF * rc + t * 128
                yc = ycand[rc].rearrange("p (m q c) -> p m q c", m=4, q=2)
                for q in range(2):
                    src = G_abd.rows(q, r0, 128).rearrange("p (m c) -> p m c", m=3)
                    p.dma('sp', yc[:, 0:2, q, :], src[:, 0:2, :])
                    p.dma('sp', yc[:, 3, q, :], src[:, 2, :])
                slot = (HALF // 256) * rc + t // 2
                if slot < NSLOT:
                    p.dma('sp', ycand[rc][:, 512:768], G_c.rows(t % 2, slot * 128, 128))
                else:
                    p.memset('pool', ycand[rc][:, 512:768], 0.0)
            p.ts('dve', ytile, ycand[0], rk[:, 0:1], None, ALU.mult)
            p.stt(ytile, ycand[1], rk[:, 1:2], ytile, ALU.mult, ALU.add)
            for c in range(8):
                pt = PS[4 + c // 4]
                p.tr(pt[:, (c % 4) * 128:(c % 4 + 1) * 128], ytile[:, c * 128:(c + 1) * 128], ident)
                if c % 4 == 3:
                    p.copy('act', yTb[:, c - 3:c + 1, :], pt.rearrange("p (a n) -> p a n", a=4))
        for i in range(4):
            for hf in range(2):
                pg = PS[hf]; py = PS[2 + hf]
                for c in range(8):
                    p.mm(pg, hTb[:, c, :], Wg[:, c, i * D + hf * 512: i * D + hf * 512 + 512], start=(c == 0), stop=(c == 7))
                p.act(gsb[hf], pg, AF.Sigmoid)
                for c in range(2):
                    p.mm(py, yTb[:, 2 * i + c, :], Wb[:, 2 * i + c, hf * 512:(hf + 1) * 512], start=(c == 0), stop=(c == 1))
                if i == 0:
                    p.tt('dve', merged[:, hf * 512:(hf + 1) * 512], gsb[hf], py, ALU.mult)
                else:
                    p.tt('dve', tmpm, gsb[hf], py, ALU.mult)
                    p.tt('pool', merged[:, hf * 512:(hf + 1) * 512], merged[:, hf * 512:(hf + 1) * 512], tmpm, ALU.add)
        for c in range(8):
            pt = PS[4 + c // 4]
            p.tr(pt[:, (c % 4) * 128:(c % 4 + 1) * 128], merged[:, c * 128:(c + 1) * 128], ident)
            if c % 4 == 3:
                p.copy('act', mTb[:, c - 3:c + 1, :], pt.rearrange("p (a n) -> p a n", a=4))
        for hf in range(2):
            po = PS[6 + hf]
            for c in range(8):
                p.mm(po, mTb[:, c, :], Wo[:, c, hf * 512:(hf + 1) * 512], start=(c == 0), stop=(c == 7))
            p.stt(pre[:, hf * 512:(hf + 1) * 512], ht[:, hf * 512:(hf + 1) * 512], DN_ALPHA, po, ALU.mult, ALU.add)
        emit_ln(p, pre, h1, G1, B1, lntmp, eps)
        p.dma('sp', h1_d[ts_, :], h1)
        for c in range(8):
            pt = PS[4 + c // 4]
            p.tr(pt[:, (c % 4) * 128:(c % 4 + 1) * 128], h1[:, c * 128:(c + 1) * 128], ident)
            if c % 4 == 3:
                p.copy('act', x32[:, c - 3:c + 1, :], pt.rearrange("p (a n) -> p a n", a=4))
        p.copy('pool', xb, x32)
        p.dma('sp', xT_d[:, :, ts_], xb)
        pr = PS[0]
        for c in range(8):
            p.mm(pr[:, 0:20], x32[:, c, :], Wr[:, c, :], start=(c == 0), stop=(c == 7))
        p.tt('dve', lg, pr[:, 0:20], Br, ALU.add)
        p.op('dve', lambda e: e.reduce_max(sm[:, 0:1], lg[:, 0:4], AX.X), r=[lg], w=[sm])
        p.ts('dve', sm[:, 1:2], sm[:, 0:1], -1.0, None, ALU.mult)
        p.act(ge4, lg[:, 0:4], AF.Exp, bias=sm[:, 1:2], accum_out=sm[:, 2:3])
        p.op('dve', lambda e: e.reciprocal(sm[:, 3:4], sm[:, 2:3]), r=[sm], w=[sm])
        p.ts('dve', gm4, lg[:, 0:4], sm[:, 0:1], None, ALU.is_ge)
        p.copy('dve', em.rearrange("p (g e) -> p g e", e=4), gm4.unsqueeze(2).to_broadcast([128, 4, 4]))
        p.ts('dve', em, em, 1e30, -1e30, ALU.mult, ALU.add)
        p.tt('dve', elm, lg[:, 4:20], em, ALU.add)
        p.op('dve', lambda e: e.max(top8, elm), r=[elm], w=[top8])
        p.tt('dve', sm[:, 4:5], top8[:, 1:2], top8[:, 0:1], ALU.subtract)
        p.act(sm[:, 5:6], sm[:, 4:5], AF.Sigmoid)
        p.ts('dve', sm[:, 6:7], sm[:, 5:6], -1.0, 1.0, ALU.mult, ALU.add)
        p.tt('dve', sm[:, 7:8], sm[:, 6:7], sm[:, 3:4], ALU.mult)
        p.tt('dve', sm[:, 8:9], sm[:, 5:6], sm[:, 3:4], ALU.mult)
        p.ts('dve', g0, elm, top8[:, 0:1], sm[:, 7:8], ALU.is_equal, ALU.mult)
        p.ts('dve', g1, elm, top8[:, 1:2], sm[:, 8:9], ALU.is_equal, ALU.mult)
        p.tt('dve', gate_all[:, t, :], g0, g1, ALU.add)

    p.pop()
    p.push()
    We_g = [p.sb([128, 8, 256], BF16, name="Weg%d" % i) for i in range(2)]
    We_u = [p.sb([128, 8, 256], BF16, name="Weu%d" % i) for i in range(2)]
    We_d = [p.sb([128, 2, D], BF16, name="Wed%d" % i) for i in range(2)]
    xg = p.sb([128, 8, GT * 128], BF16, name="xg")
    yacc = p.sb([128, GT, D], name="yacc")
    sg = [p.sb([128, 512], name="sg%d" % i) for i in range(2)]
    hid = [p.sb([128, 512], BF16, name="hid%d" % i) for i in range(2)]
    h1t = p.sb([128, D], name="h1t"); pre2 = p.sb([128, D], name="pre2"); o2 = p.sb([128, D], name="o2")
    tmpT2 = p.sb([128, 8, 128], name="tmpT2")
    k = 0
    for g0_ in range(0, NTL, GT):
        ntg = min(GT, NTL - g0_)
        p.dma('sp', xg[:, :, 0:ntg * 128], xT_d[:, :, g0_ * 128:(g0_ + ntg) * 128])
        for e_ in range(16):
            wgt, wut, wdt = We_g[k % 2], We_u[k % 2], We_d[k % 2]
            k += 1
            load_cast(p, wgt, mg_d[e_].rearrange("(c f) n -> f c n", f=128), stage, 2048)
            load_cast(p, wut, mu_d[e_].rearrange("(c f) n -> f c n", f=128), stage, 2048)
            load_cast(p, wdt, md_d[e_].rearrange("(c f) n -> f c n", f=128), stage, 2048)
            for tb in range(0, ntg, 4):
                nb = min(4, ntg - tb)
                ncol = nb * 128
                cs = slice(tb * 128, tb * 128 + ncol)
                for j in range(2):
                    pa = PS[j * 2]; pb = PS[j * 2 + 1]
                    for c in range(8):
                        p.mm(pa[:, 0:ncol], wgt[:, c, j * 128:(j + 1) * 128], xg[:, c, cs], start=(c == 0), stop=(c == 7))
                    for c in range(8):
                        p.mm(pb[:, 0:ncol], wut[:, c, j * 128:(j + 1) * 128], xg[:, c, cs], start=(c == 0), stop=(c == 7))
                    p.act(sg[j][:, 0:ncol], pa[:, 0:ncol], AF.Silu)
                    p.tt('dve', hid[j][:, 0:ncol], sg[j][:, 0:ncol], pb[:, 0:ncol], ALU.mult)
                for tt_ in range(nb):
                    tile_i = tb + tt_
                    for hf in range(2):
                        pd = PS[4 + (tt_ * 2 + hf) % 4]
                        for j in range(2):
                            p.mm(pd, hid[j][:, tt_ * 128:(tt_ + 1) * 128], wdt[:, j, hf * 512:(hf + 1) * 512], start=(j == 0), stop=(j == 1))
                        ya = yacc[:, tile_i, hf * 512:(hf + 1) * 512]
                        gcol = gate_all[:, g0_ + tile_i, e_:e_ + 1]
                        if e_ == 0:
                            p.ts('dve', ya, pd, gcol, None, ALU.mult)
                        else:
                            p.stt(ya, pd, gcol, ya, ALU.mult, ALU.add)
        for tt_ in range(ntg):
            ts_ = slice((g0_ + tt_) * 128, (g0_ + tt_ + 1) * 128)
            p.dma('sp', h1t, h1_d[ts_, :])
            p.stt(pre2, h1t, DN_ALPHA, yacc[:, tt_, :], ALU.mult, ALU.add)
            emit_ln(p, pre2, o2, G2, B2, lntmp, eps)
            p.dma('sp', out_d[ts_, :], o2)
            if hT_out is not None:
                emit_transposed(p, PS, o2, hT_out, ts_, ident, tmpT2)
    p.pop()
    p.pop()


def build_post(NTL, GT=8):
    return standalone(emit_post, NTL, GT)


def load_w_bf16(p, src_d, ncols, stage, name):
    W = p.sb([128, 8, ncols], BF16, name=name)
    load_cast(p, W, src_d.rearrange("(c f) n -> f c n", f=128), stage, 2048)
    return W


def load_hblock(p, hT_d, pos0, npos, hTf, hTb):
    p.dma('sp', hTf[:, :, 0:npos], hT_d.rearrange("(c f) n -> f c n", f=128)[:, :, pos0:pos0 + npos])
    p.copy('pool', hTb[:, :, 0:npos], hTf[:, :, 0:npos])


def proj_fm(p, ps_out, W, c0, ncols, hTb, n0, npos):
    for c in range(8):
        p.mm(ps_out, W[:, c, c0:c0 + ncols], hTb[:, c, n0:n0 + npos], start=(c == 0), stop=(c == 7))


def proj_tm(p, ps_out, W, c0, ncols, hTb, n0, npos):
    for c in range(8):
        p.mm(ps_out, hTb[:, c, n0:n0 + npos], W[:, c, c0:c0 + ncols], start=(c == 0), stop=(c == 7))


def gla_gen(cx, NCH, stop_at=99):
    nc, p, PS = cx.nc, cx.p, cx.PS
    P_ = NCH * 64
    BLK = 512
    di = cx.inp
    hT_d = di("hT", [D, P_])
    wqk_d = di("wqk", [D, 128]); wvo_d = di("wvo", [D, 256]); wa_d = di("wa", [D, 16])
    aup_d = di("aup", [16, 64]); ab_d = di("ab", [64, 1]); ng_d = di("ng", [128])
    cm_d = di("cmask", [P_]); mu_d = di("maskU2", [64, 128]); id_d = di("ident", [128, 128])
    y_d = cx.out("y", [P_, 128])
    p.push()
    stage = [p.sb([128, 2048], name="stg%d" % i) for i in range(2)]
    Wqk = load_w_bf16(p, wqk_d, 128, stage, "Wqk")
    Wvo = load_w_bf16(p, wvo_d, 256, stage, "Wvo")
    Wa = load_w_bf16(p, wa_d, 16, stage, "Wa")
    aup = p.sb([16, 64]); p.dma('sp', aup, aup_d)
    ab = [p.sb([32, 1]) for _ in range(2)]
    for h in range(2):
        p.dma('sp', ab[h], ab_d[h * 32:(h + 1) * 32, :])
    ng = p.sb([64, 128]); p.dma('sp', ng, ng_d.partition_broadcast(64))
    maskU = p.sb([64, 128]); p.dma('sp', maskU, mu_d)
    ident = p.sb([128, 128]); p.dma('sp', ident, id_d)
    eps6 = p.sb([64, 1]); p.memset('dve', eps6, 1e-6)
    S = [p.sb([32, 64], name="S%d" % h) for h in range(2)]
    for h in range(2):
        p.memset('dve', S[h], 0.0)
    hTf = p.sb([128, 8, BLK]); hTb = p.sb([128, 8, BLK], BF16)
    cm = p.sb([32, BLK]); xa = p.sb([16, BLK])
    mk = lambda nm: [p.sb([32, BLK], name=nm + str(h)) for h in range(2)]
    qT, kT, la, bT, eb, enb, ekb, qg, kg, ku = (mk(n) for n in ("qT", "kT", "la", "bT", "eb", "enb", "ekb", "qg", "kg", "ku"))
    dec = [p.sb([32, BLK // 64], name="dec%d" % h) for h in range(2)]
    vo = p.sb([64, 256]); sgo = p.sb([64, 128]); kut = p.sb([64, 64]); attm = p.sb([64, 128])
    o_sb = p.sb([64, 128]); sq = p.sb([64, 128]); ms = p.sb([64, 2]); yt = p.sb([64, 128])
    yield
    for b0 in range(0, P_, BLK):
        nb = min(BLK, P_ - b0)
        nch = nb // 64
        load_hblock(p, hT_d, b0, nb, hTf, hTb)
        p.dma('sp', cm[:, 0:nb], cm_d[b0:b0 + nb].partition_broadcast(32))
        proj_fm(p, PS[2][0:16, 0:nb], Wa, 0, 16, hTb, 0, nb)
        p.copy('act', xa[:, 0:nb], PS[2][0:16, 0:nb])
        for h in range(2):
            sl = (slice(None), slice(0, nb))
            proj_fm(p, PS[0][0:32, 0:nb], Wqk, h * 32, 32, hTb, 0, nb)
            p.op('act', lambda e: e.mul(qT[h][sl], PS[0][0:32, 0:nb], 32 ** -0.5), r=[PS[0]], w=[qT[h]])
            proj_fm(p, PS[1][0:32, 0:nb], Wqk, 64 + h * 32, 32, hTb, 0, nb)
            p.copy('act', kT[h][sl], PS[1][0:32, 0:nb])
            p.mm(PS[3][0:32, 0:nb], aup[:, h * 32:(h + 1) * 32], xa[:, 0:nb])
            p.act(la[h][sl], PS[3][0:32, 0:nb], AF.Sigmoid, bias=ab[h])
            p.act(la[h][sl], la[h][sl], AF.Ln)
            p.ts('pool', la[h][sl], la[h][sl], 1.0 / 16.0, None, ALU.mult)
            p.op('dve', lambda e: e.tensor_tensor_scan(bT[h][sl], cm[:, 0:nb], la[h][sl], 0.0, ALU.mult, ALU.add),
                 r=[cm, la[h]], w=[bT[h]])
            p.act(eb[h][sl], bT[h][sl], AF.Exp)
            p.act(enb[h][sl], bT[h][sl], AF.Exp, scale=-1.0)
            b3 = bT[h][sl].rearrange("p (c l) -> p c l", l=64)
            p.tt('dve', ekb[h][sl].rearrange("p (c l) -> p c l", l=64), b3[:, :, 63:64].to_broadcast([32, nch, 64]), b3, ALU.subtract)
            p.act(ekb[h][sl], ekb[h][sl], AF.Exp)
            p.act(dec[h][:, 0:nch], b3[:, :, 63], AF.Exp)
            p.tt('dve', qg[h][sl], qT[h][sl], eb[h][sl], ALU.mult)
            p.tt('pool', kg[h][sl], kT[h][sl], enb[h][sl], ALU.mult)
            p.tt('pool', ku[h][sl], kT[h][sl], ekb[h][sl], ALU.mult)
        for ci in range(nch):
            cs = slice(ci * 64, ci * 64 + 64)
            proj_tm(p, PS[4][0:64, 0:256], Wvo, 0, 256, hTb, ci * 64, 64)
            p.copy('act', vo[:, 0:128], PS[4][0:64, 0:128])
            p.act(sgo, PS[4][0:64, 128:256], AF.Silu)
            for h in range(2):
                p.tr(PS[5][0:64, h * 32:h * 32 + 32], ku[h][:, cs], ident[0:32, 0:32])
            p.copy('act', kut, PS[5][0:64, 0:64])
            for h in range(2):
                p.mm(PS[6][0:64, h * 64:h * 64 + 64], kg[h][:, cs], qg[h][:, cs])
            p.tt('dve', attm, PS[6][0:64, 0:128], maskU, ALU.mult)
            for h in range(2):
                p.mm(PS[7][0:64, h * 64:h * 64 + 64], attm[:, h * 64:h * 64 + 64], vo[:, h * 64:h * 64 + 64], start=True, stop=False)
                p.mm(PS[7][0:64, h * 64:h * 64 + 64], qg[h][:, cs], S[h], start=False, stop=True)
            for h in range(2):
                p.mm(PS[5][0:32, 128 + h * 64:128 + h * 64 + 64], kut[:, h * 32:h * 32 + 32], vo[:, h * 64:h * 64 + 64])
                p.stt(S[h], S[h], dec[h][:, ci:ci + 1], PS[5][0:32, 128 + h * 64:128 + h * 64 + 64], ALU.mult, ALU.add)
            p.copy('act', o_sb, PS[7][0:64, 0:128])
            p.tt('dve', sq, o_sb, o_sb, ALU.mult)
            p.op('dve', lambda e: e.reduce_sum(ms, sq.rearrange("p (h d) -> p h d", d=64), AX.X), r=[sq], w=[ms])
            p.act(ms, ms, AF.Sqrt, bias=eps6, scale=1.0 / 64.0)
            p.op('dve', lambda e: e.reciprocal(ms, ms), r=[ms], w=[ms])
            p.tt('dve', yt.rearrange("p (h d) -> p h d", d=64), o_sb.rearrange("p (h d) -> p h d", d=64),
                 ms.unsqueeze(2).to_broadcast([64, 2, 64]), ALU.mult)
            p.tt('pool', yt, yt, ng, ALU.mult)
            p.tt('pool', yt, yt, sgo, ALU.mult)
            p.dma('sp', y_d[b0 + ci * 64:b0 + ci * 64 + 64, :], yt)
            yield
    p.pop()


def emit_gla(cx, NCH, stop_at=99):
    for _ in gla_gen(cx, NCH, stop_at):
        pass


def build_gla(NCH, stop_at=99):
    return standalone(emit_gla, NCH, stop_at)


def emit_mlstm(cx, NCH):
    nc, p, PS = cx.nc, cx.p, cx.PS
    P_ = NCH * 64
    BLK = 512
    di = cx.inp
    hT_d = di("hT", [D, P_])
    wq_d = di("wq", [D, 128]); wk_d = di("wk", [D, 128]); wvo_d = di("wvo", [D, 256])
    wi_d = di("wi", [D, 128]); wf_d = di("wf", [D, 128])
    cwq_d = di("cwq", [128, 4]); cwk_d = di("cwk", [128, 4]); cbq_d = di("cbq", [128, 1]); cbk_d = di("cbk", [128, 1])
    ib_d = di("ib", [128, 1]); fb_d = di("fb", [128, 1]); ng_d = di("ng", [128])
    cm_d = di("cmask", [P_]); pm_d = di("pm01", [P_]); pn_d = di("pmneg", [P_])
    ml_d = di("maskL", [64, 64]); id_d = di("ident", [128, 128])
    y_d = cx.out("y", [P_, 128])
    p.push()
    stage = [p.sb([128, 2048], name="stg%d" % i) for i in range(2)]
    Wq = load_w_bf16(p, wq_d, 128, stage, "Wq"); Wk = load_w_bf16(p, wk_d, 128, stage, "Wk")
    Wvo = load_w_bf16(p, wvo_d, 256, stage, "Wvo")
    Wi = load_w_bf16(p, wi_d, 128, stage, "Wi"); Wf = load_w_bf16(p, wf_d, 128, stage, "Wf")
    def ld(src, shape, nm):
        t = p.sb(shape, name=nm); p.dma('sp', t, src); return t
    cw = {}
    for h in range(2):
        hs = slice(h * 64, h * 64 + 64)
        cw['q', h] = (ld(cwq_d[hs, :], [64, 4], "cwq%d" % h), ld(cbq_d[hs, :], [64, 1], "cbq%d" % h))
        cw['k', h] = (ld(cwk_d[hs, :], [64, 4], "cwk%d" % h), ld(cbk_d[hs, :], [64, 1], "cbk%d" % h))
    ib = [ld(ib_d[h * 64:h * 64 + 64, :], [64, 1], "ib%d" % h) for h in range(2)]
    fb = [ld(fb_d[h * 64:h * 64 + 64, :], [64, 1], "fb%d" % h) for h in range(2)]
    ng = p.sb([64, 128]); p.dma('sp', ng, ng_d.partition_broadcast(64))
    maskL = ld(ml_d, [64, 64], "maskL"); ident = ld(id_d, [128, 128], "ident")
    eps5 = p.sb([64, 1]); p.memset('dve', eps5, 1e-5)
    Cst = [p.sb([64, 65], name="C%d" % h) for h in range(2)]
    mst = [p.sb([64, 1], name="m%d" % h) for h in range(2)]
    for h in range(2):
        p.memset('dve', Cst[h], 0.0); p.memset('dve', mst[h], 0.0)
    hTf = p.sb([128, 8, BLK]); hTb = p.sb([128, 8, BLK], BF16)
    cm = p.sb([64, BLK]); pm = p.sb([64, BLK]); pn = p.sb([64, BLK])
    mk = lambda nm, w=BLK: [p.sb([64, w], name=nm + str(h)) for h in range(2)]
    qpre = mk("qpre", BLK + 3); kpre = mk("kpre", BLK + 3)
    for h in range(2):
        p.memset('dve', qpre[h], 0.0); p.memset('dve', kpre[h], 0.0)
    acc = mk("acc"); qT = mk("qT"); kT = mk("kT"); liR = mk("liR"); lfR = mk("lfR"); bR = mk("bR"); gR = mk("gR")
    vo1 = p.sb([64, 2, 65]); sgo = p.sb([64, 128]); hh = p.sb([64, 128]); yt = p.sb([64, 128])
    for h in range(2):
        p.memset('dve', vo1[:, h, 64:65], 1.0)
    bcol = p.sb([64, 1]); dl = p.sb([64, 64]); sm = p.sb([64, 12], name="msm"); sw = p.sb([64, 64]); swT = p.sb([64, 64])
    t2 = p.sb([64, 65]); nd = p.sb([64, 65]); wl = p.sb([64, 64]); kw = p.sb([64, 64]); kwt = p.sb([64, 64]); cl = p.sb([64, 65])
    bst = p.sb([64, 6]); bmv = p.sb([64, 2])
    for b0 in range(0, P_, BLK):
        nb = min(BLK, P_ - b0)
        nch = nb // 64
        sl = (slice(None), slice(0, nb))
        load_hblock(p, hT_d, b0, nb, hTf, hTb)
        for tdst, src in ((cm, cm_d), (pm, pm_d), (pn, pn_d)):
            p.dma('sp', tdst[:, 0:nb], src[b0:b0 + nb].partition_broadcast(64))
        for h in range(2):
            for (W, pre, outT, key, scl) in ((Wq, qpre[h], qT[h], 'q', 1.0), (Wk, kpre[h], kT[h], 'k', 0.125)):
                proj_fm(p, PS[0][0:64, 0:nb], W, h * 64, 64, hTb, 0, nb)
                p.copy('act', pre[:, 3:3 + nb], PS[0][0:64, 0:nb])
                cwt, cbt = cw[key, h]
                p.ts('dve', acc[h][sl], pre[:, 3:3 + nb], cwt[:, 3:4], None, ALU.mult)
                for j in range(3):
                    p.stt(acc[h][sl], pre[:, j:j + nb], cwt[:, j:j + 1], acc[h][sl], ALU.mult, ALU.add)
                p.act(outT[sl], acc[h][sl], AF.Silu, bias=cbt)
                if scl != 1.0:
                    p.ts('pool', outT[sl], outT[sl], scl, None, ALU.mult)
                p.copy('pool', pre[:, 0:3], pre[:, nb:nb + 3])
            proj_fm(p, PS[1][0:64, 0:nb], Wi, h * 64, 64, hTb, 0, nb)
            p.stt(liR[h][sl], PS[1][0:64, 0:nb], ib[h], pn[:, 0:nb], ALU.add, ALU.add)
            proj_fm(p, PS[2][0:64, 0:nb], Wf, h * 64, 64, hTb, 0, nb)
            p.act(lfR[h][sl], PS[2][0:64, 0:nb], AF.Sigmoid, bias=fb[h])
            p.act(lfR[h][sl], lfR[h][sl], AF.Ln)
            p.tt('pool', lfR[h][sl], lfR[h][sl], pm[:, 0:nb], ALU.mult)
            p.op('dve', lambda e: e.tensor_tensor_scan(bR[h][sl], cm[:, 0:nb], lfR[h][sl], 0.0, ALU.mult, ALU.add),
                 r=[cm, lfR[h]], w=[bR[h]])
            p.tt('dve', gR[h][sl], liR[h][sl], bR[h][sl], ALU.subtract)
        for ci in range(nch):
            cs = slice(ci * 64, ci * 64 + 64)
            proj_tm(p, PS[3][0:64, 0:256], Wvo, 0, 256, hTb, ci * 64, 64)
            p.copy('act', vo1[:, :, 0:64], PS[3][0:64, 0:128].rearrange("p (h d) -> p h d", d=64))
            p.act(sgo, PS[3][0:64, 128:256], AF.Sigmoid)
            for h in range(2):
                C = Cst[h]; m = mst[h]
                p.tr(PS[4][0:64, 0:64], bR[h][:, cs], ident[0:64, 0:64])
                p.copy('act', bcol, PS[4][0:64, 0:1])
                p.stt(dl, gR[h][:, cs], bcol, maskL, ALU.add, ALU.add)
                p.op('dve', lambda e: e.reduce_max(sm[:, 0:1], dl, AX.X), r=[dl], w=[sm])
                p.tt('dve', sm[:, 1:2], bcol, m, ALU.add)
                p.tt('dve', sm[:, 2:3], sm[:, 0:1], sm[:, 1:2], ALU.max)
                p.ts('dve', sm[:, 3:4], sm[:, 2:3], -1.0, None, ALU.mult)
                p.act(sw, dl, AF.Exp, bias=sm[:, 3:4])
                p.mm(PS[5][0:64, 0:64], qT[h][:, cs], kT[h][:, cs])
                p.tt('dve', sw, sw, PS[5][0:64, 0:64], ALU.mult)
                p.tr(PS[4][0:64, 64:128], sw, ident[0:64, 0:64])
                p.copy('act', swT, PS[4][0:64, 64:128])
                p.mm(PS[6][0:64, 0:65], swT, vo1[:, h, :])
                p.mm(PS[6][0:64, 128:193], qT[h][:, cs], C)
                p.tt('dve', sm[:, 4:5], sm[:, 1:2], sm[:, 2:3], ALU.subtract)
                p.act(sm[:, 5:6], sm[:, 4:5], AF.Exp)
                p.act(t2, PS[6][0:64, 128:193], AF.Copy, scale=sm[:, 5:6])
                p.tt('dve', nd, PS[6][0:64, 0:65], t2, ALU.add)
                p.ts('dve', sm[:, 6:7], nd[:, 64:65], -1.0, None, ALU.mult)
                p.tt('dve', sm[:, 6:7], sm[:, 6:7], nd[:, 64:65], ALU.max)
                p.act(sm[:, 7:8], sm[:, 2:3], AF.Exp, scale=-1.0)
                p.tt('dve', sm[:, 8:9], sm[:, 6:7], sm[:, 7:8], ALU.max)
                p.op('dve', lambda e: e.reciprocal(sm[:, 9:10], sm[:, 8:9]), r=[sm], w=[sm])
                p.ts('dve', hh[:, h * 64:h * 64 + 64], nd[:, 0:64], sm[:, 9:10], None, ALU.mult)
                blast = bR[h][:, ci * 64 + 63:ci * 64 + 64]
                p.op('dve', lambda e: e.reduce_max(sm[:, 10:11], gR[h][:, cs], AX.X), r=[gR[h]], w=[sm])
                p.ts('dve', sm[:, 11:12], sm[:, 10:11], -1.0, None, ALU.mult)
                p.tt('dve', sm[:, 10:11], sm[:, 10:11], blast, ALU.add)
                p.act(wl, gR[h][:, cs], AF.Exp, bias=sm[:, 11:12], scale=1.0)
                p.tt('dve', kw, kT[h][:, cs], wl, ALU.mult)
                p.tr(PS[7][0:64, 0:64], kw, ident[0:64, 0:64])
                p.copy('act', kwt, PS[7][0:64, 0:64])
                p.mm(PS[7][0:64, 128:193], kwt, vo1[:, h, :])
                p.tt('dve', sm[:, 0:1], blast, m, ALU.add)
                p.tt('dve', sm[:, 1:2], sm[:, 0:1], sm[:, 10:11], ALU.max)
                p.tt('dve', sm[:, 2:3], sm[:, 0:1], sm[:, 1:2], ALU.subtract)
                p.act(sm[:, 2:3], sm[:, 2:3], AF.Exp)
                p.tt('dve', sm[:, 3:4], sm[:, 10:11], sm[:, 1:2], ALU.subtract)
                p.act(sm[:, 3:4], sm[:, 3:4], AF.Exp)
                p.act(cl, PS[7][0:64, 128:193], AF.Copy, scale=sm[:, 3:4])
                p.stt(C, C, sm[:, 2:3], cl, ALU.mult, ALU.add)
                p.copy('dve', m, sm[:, 1:2])
            p.tt('dve', hh, hh, sgo, ALU.mult)
            for h in range(2):
                hs = slice(h * 64, h * 64 + 64)
                p.op('dve', lambda e: e.bn_stats(bst, hh[:, hs]), r=[hh], w=[bst])
                p.op('dve', lambda e: e.bn_aggr(bmv, bst), r=[bst], w=[bmv])
                p.act(bmv[:, 1:2], bmv[:, 1:2], AF.Sqrt, bias=eps5)
                p.op('dve', lambda e: e.reciprocal(bmv[:, 1:2], bmv[:, 1:2]), r=[bmv], w=[bmv])
                p.ts('dve', yt[:, hs], hh[:, hs], bmv[:, 0:1], bmv[:, 1:2], ALU.subtract, ALU.mult)
            p.tt('pool', yt, yt, ng, ALU.mult)
            p.dma('sp', y_d[b0 + ci * 64:b0 + ci * 64 + 64, :], yt)
            p.bgsteps(1)
    p.pop()


def build_mlstm(NCH):
    return standalone(emit_mlstm, NCH)


def emit_rwkv(cx, NCH, NSTEP):
    nc, p, PS = cx.nc, cx.p, cx.PS
    P_ = NCH * 64
    BLK = 512
    SUB = 32
    di = cx.inp
    hT_d = di("hT", [D, P_])
    w_d = di("w", [D, 640]); mu_d = di("mu", [128, 6])
    wup_d = di("wup", [64, 128]); aup_d = di("aup", [64, 128]); gup_d = di("gup", [128, 128])
    cols_d = di("cols", [128, 8])
    gng_d = di("gng", [128]); gnb_d = di("gnb", [128]); bo_d = di("blockones", [128, 128]); id_d = di("ident", [128, 128])
    y_d = cx.out("y", [P_, 128])
    scr = lambda n: cx.scratch(n, [P_ + 1, 128])
    k2s, nkas, vs, bons, gs, yraw = (scr(n) for n in ("k2s", "nkas", "vs", "bons", "gs", "yraw"))
    p.push()
    decT = p.sb([128, P_], name="decT"); kkT = p.sb([128, P_], name="kkT"); rTh = p.sb([128, P_ + 1], name="rTh")
    gng = p.sb([128, 128]); p.dma('sp', gng, gng_d.partition_broadcast(128))
    gnb = p.sb([128, 128]); p.dma('sp', gnb, gnb_d.partition_broadcast(128))
    epsg = p.sb([128, 1]); p.memset('dve', epsg, 64e-5)
    p.push()
    stage = [p.sb([128, 2048], name="stg%d" % i) for i in range(2)]
    W = load_w_bf16(p, w_d, 640, stage, "W")
    def ld(src, shape, nm):
        t = p.sb(shape, name=nm); p.dma('sp', t, src); return t
    mu = ld(mu_d, [128, 6], "mu"); wup = ld(wup_d, [64, 128], "wup"); aup = ld(aup_d, [64, 128], "aup")
    gup = ld(gup_d, [128, 128], "gup"); cols = ld(cols_d, [128, 8], "cols")
    bones = ld(bo_d, [128, 128], "bones"); ident = ld(id_d, [128, 128], "ident")
    omka = p.sb([128, 1]); p.ts('dve', omka, cols[:, 3:4], -1.0, 1.0, ALU.mult, ALU.add)
    p.memset('dve', rTh[:, 0:1], 0.0)
    hTf = p.sb([128, 8, BLK]); hTb = p.sb([128, 8, BLK], BF16)
    nrows = [128, 128, 128, 64, 64, 128]
    pre = [p.sb([nrows[i], BLK + 1], name="pre%d" % i) for i in range(6)]
    lp = [p.sb([nrows[i], BLK], name="lp%d" % i) for i in range(6)]
    for i in range(6):
        p.memset('dve', pre[i][:, 0:1], 0.0)
    dtmp = p.sb([128, BLK]); a_t = p.sb([128, BLK]); t1 = p.sb([128, BLK]); k2 = p.sb([128, BLK]); nka = p.sb([128, BLK])
    g_t = p.sb([128, BLK]); bon = p.sb([128, BLK]); tok = p.sb([128, 128], name="tokst")
    for b0 in range(0, P_, BLK):
        nb = min(BLK, P_ - b0)
        sl = (slice(None), slice(0, nb))
        load_hblock(p, hT_d, b0, nb, hTf, hTb)
        c0 = 0
        for i in range(6):
            nr = nrows[i]
            proj_fm(p, PS[i % 2][0:nr, 0:nb], W, c0, nr, hTb, 0, nb)
            c0 += nr
            p.copy('act', pre[i][:, 1:1 + nb], PS[i % 2][0:nr, 0:nb])
            p.tt('dve', dtmp[0:nr, 0:nb], pre[i][:, 0:nb], pre[i][:, 1:1 + nb], ALU.subtract)
            p.stt(lp[i][sl], dtmp[0:nr, 0:nb], mu[0:nr, i:i + 1], pre[i][:, 1:1 + nb], ALU.mult, ALU.add)
            p.copy('pool', pre[i][:, 0:1], pre[i][:, nb:nb + 1])
        r_, k_, v_, xw, xa, xg = lp
        bs = slice(b0, b0 + nb)
        p.copy('pool', rTh[:, 1 + b0:1 + b0 + nb], r_[sl])
        p.act(xw[sl], xw[sl], AF.Tanh)
        p.mm(PS[2][:, 0:nb], wup, xw[sl])
        p.act(t1[sl], PS[2][:, 0:nb], AF.Sigmoid, bias=cols[:, 0:1])
        p.act(decT[:, bs], t1[sl], AF.Exp, scale=-float(np.exp(-0.5)))
        p.mm(PS[3][:, 0:nb], aup, xa[sl])
        p.act(a_t[sl], PS[3][:, 0:nb], AF.Sigmoid, bias=cols[:, 1:2])
        p.act(xg[sl], xg[sl], AF.Sigmoid)
        p.mm(PS[2][:, 0:nb], gup, xg[sl])
        p.copy('act', g_t[sl], PS[2][:, 0:nb])
        p.ts('dve', t1[sl], k_[sl], cols[:, 2:3], None, ALU.mult)
        p.tt('pool', dtmp[sl], t1[sl], t1[sl], ALU.mult)
        p.mm(PS[3][:, 0:nb], bones, dtmp[sl])
        p.act(dtmp[sl], PS[3][:, 0:nb], AF.Sqrt)
        p.ts('dve', dtmp[sl], dtmp[sl], 1e-12, None, ALU.max)
        p.op('dve', lambda e: e.reciprocal(dtmp[sl], dtmp[sl]), r=[dtmp], w=[dtmp])
        p.tt('dve', kkT[:, bs], t1[sl], dtmp[sl], ALU.mult)
        p.ts('dve', t1[sl], a_t[sl], cols[:, 3:4], omka, ALU.mult, ALU.add)
        p.tt('dve', k2[sl], k_[sl], t1[sl], ALU.mult)
        p.stt(nka[sl], kkT[:, bs], -1.0, a_t[sl], ALU.mult, ALU.mult)
        p.stt(t1[sl], r_[sl], cols[:, 4:5], k2[sl], ALU.mult, ALU.mult)
        p.mm(PS[2][:, 0:nb], bones, t1[sl])
        p.tt('dve', bon[sl], PS[2][:, 0:nb], v_[sl], ALU.mult)
        for (src, dst) in ((k2, k2s), (nka, nkas), (v_, vs), (bon, bons), (g_t, gs)):
            for j in range(nb // 128):
                p.tr(PS[4 + j % 2][:, 0:128], src[:, j * 128:(j + 1) * 128], ident)
                p.copy('act', tok, PS[4 + j % 2][:, 0:128])
                p.dma('sp', dst[b0 + j * 128:b0 + (j + 1) * 128, :], tok)
    p.pop()
    ST = p.sb([128, 64], name="ST"); p.memset('dve', ST, 0.0)
    sets = []
    for i in range(2):
        d = dict(KVl=p.sb([2, SUB, 128], name="KVl%d" % i), KAl=p.sb([4, SUB, 128], name="KAl%d" % i),
                 Vr=p.sb([2, SUB, 64], name="Vr%d" % i), Ycp=p.sb([4, SUB, 64], name="Ycp%d" % i),
                 L1=p.sb([128, SUB, 4], name="L1%d" % i))
        p.memset('dve', d["KVl"], 0.0); p.memset('dve', d["KAl"], 0.0); p.memset('dve', d["L1"], 0.0)
        sets.append(d)
    nblk = -(-NSTEP // SUB)

    def load_blk(k):
        s0 = k * SUB
        ns = min(SUB, NSTEP - s0)
        d = sets[k % 2]
        for h in range(2):
            hc = slice(h * 64, h * 64 + 64)
            p.dma('sp', d["KVl"][h:h + 1, 0:ns, hc], k2s[s0:s0 + ns, hc].unsqueeze(0))
            p.dma('sp', d["KAl"][2 * h:2 * h + 1, 0:ns, hc], nkas[s0:s0 + ns, hc].unsqueeze(0))
            p.dma('sp', d["Vr"][h:h + 1, 0:ns, :], vs[s0:s0 + ns, hc].unsqueeze(0))
            p.copy('pool', d["L1"][hc, 0:ns, 2 * h], kkT[hc, s0:s0 + ns])
            p.copy('pool', d["L1"][hc, 0:ns, 2 * h + 1], rTh[hc, s0:s0 + ns])

    load_blk(0)
    for k in range(nblk):
        s0 = k * SUB
        ns = min(SUB, NSTEP - s0)
        if k + 1 < nblk:
            load_blk(k + 1)
        d = sets[k % 2]
        KVl, KAl, Vr, Ycp, L1 = d["KVl"], d["KAl"], d["Vr"], d["Ycp"], d["L1"]
        for s in range(ns):
            t = s0 + s
            pa = PS[t % 2]; pb = PS[2 + t % 2]
            p.mm(pa[0:4, 0:64], L1[:, s, :], ST)
            p.copy('act', Ycp[:, s, :], pa[0:4, 0:64])
            p.mm(pb[:, 0:64], KVl[:, s, :], Vr[:, s, :], start=True, stop=False)
            p.mm(pb[:, 0:64], KAl[:, s, :], Ycp[:, s, :], start=False, stop=True)
            p.stt(ST, ST, decT[:, t:t + 1], pb[:, 0:64], ALU.mult, ALU.add)
        for h in range(2):
            p.dma('sp', yraw[s0:s0 + ns, h * 64:h * 64 + 64].unsqueeze(0), Ycp[2 * h + 1:2 * h + 2, 0:ns, :])
    yt = p.sb([128, 128]); bt = p.sb([128, 128]); gt = p.sb([128, 128]); ot = p.sb([128, 128])
    bst = p.sb([128, 6]); bmv = p.sb([128, 2])
    for t0 in range(0, P_, 128):
        if t0 + 1 >= NSTEP:
            break
        nt = min(128, NSTEP - 1 - t0)
        p.dma('sp', yt[0:nt, :], yraw[t0 + 1:t0 + 1 + nt, :])
        p.dma('sp', bt[0:nt, :], bons[t0:t0 + nt, :])
        p.dma('sp', gt[0:nt, :], gs[t0:t0 + nt, :])
        for h in range(2):
            hs = slice(h * 64, h * 64 + 64)
            p.op('dve', lambda e: e.bn_stats(bst[0:nt, :], yt[0:nt, hs]), r=[yt], w=[bst])
            p.op('dve', lambda e: e.bn_aggr(bmv[0:nt, :], bst[0:nt, :]), r=[bst], w=[bmv])
            p.act(bmv[0:nt, 1:2], bmv[0:nt, 1:2], AF.Sqrt, bias=epsg[0:nt, :])
            p.op('dve', lambda e: e.reciprocal(bmv[0:nt, 1:2], bmv[0:nt, 1:2]), r=[bmv], w=[bmv])
            p.ts('dve', ot[0:nt, hs], yt[0:nt, hs], bmv[0:nt, 0:1], bmv[0:nt, 1:2], ALU.subtract, ALU.mult)
        p.tt('pool', ot[0:nt, :], ot[0:nt, :], gng[0:nt, :], ALU.mult)
        p.tt('pool', ot[0:nt, :], ot[0:nt, :], gnb[0:nt, :], ALU.add)
        p.tt('dve', ot[0:nt, :], ot[0:nt, :], bt[0:nt, :], ALU.add)
        p.tt('dve', ot[0:nt, :], ot[0:nt, :], gt[0:nt, :], ALU.mult)
        p.dma('sp', y_d[t0:t0 + nt, :], ot[0:nt, :])
    p.pop()


def build_rwkv(NCH, NSTEP):
    return standalone(emit_rwkv, NCH, NSTEP)


def emit_dsa(cx, NKT, NSLOT, NROUND):
    nc, p, PS = cx.nc, cx.p, cx.PS
    TK = NKT * 128
    NBq = NSLOT
    di = cx.inp
    hT_d = di("hT", [D, TK])
    fused = "rk" in cx.over
    if not fused:
        hTq_d = di("hTq", [D, NSLOT * 128])
    wq_d = di("wq", [D, 256]); wckv_d = di("wckv", [D, 128]); widx_d = di("widx", [D, 296])
    kvg_d = di("kvg", [128]); wuk_d = di("wuk", [128, 64]); wuv_d = di("wuv", [128, 64])
    b3_d = di("B3raw", [3, 4, 128, 128]); mA_d = di("maskA", [128, 128]); mB_d = di("maskB", [128, 128]); c31_d = di("c31", [128, 4])
    id_d = di("ident", [128, 128])
    y_d = cx.out("y", [NBq * 128, 256])
    p.push()
    stage = [p.sb([128, 512], name="stg%d" % i) for i in range(2)]
    Wq = load_w_bf16(p, wq_d, 256, stage, "Wq"); Wc = load_w_bf16(p, wckv_d, 128, stage, "Wc")
    Widx = p.sb([128, 8, 296], name="Widx"); p.dma('sp', Widx, widx_d.rearrange("(c f) n -> f c n", f=128))
    def ld(src, shape, nm):
        t = p.sb(shape, name=nm); p.dma('sp', t, src); return t
    kvg = p.sb([128, 128]); p.dma('sp', kvg, kvg_d.partition_broadcast(128))
    wuk = ld(wuk_d, [128, 64], "wuk"); wuv = ld(wuv_d, [128, 64], "wuv")
    c31 = ld(c31_d, [128, 4], "c31"); maskA = ld(mA_d, [128, 128], "maskA"); maskB = ld(mB_d, [128, 128], "maskB"); ident = ld(id_d, [128, 128], "ident")
    Badj = [p.sb([128, 4, 128], name="Badj%d" % k) for k in range(3)]
    for k in range(3):
        p.dma('sp', Badj[k], b3_d[k].rearrange("h i s -> i h s"))
        for h in range(4):
            p.ts('dve', Badj[k][:, h, :], Badj[k][:, h, :], c31[:, h:h + 1], None, ALU.subtract)
    eps6 = p.sb([128, 1]); p.memset('dve', eps6, 1e-6)
    kiT = p.sb([32, TK], name="kiT"); kT = p.sb([64, TK], BF16, name="kT"); v_all = p.sb([128, NKT, 64], BF16, name="v_all")
    score = p.sb([128, TK], name="score"); wk = p.sb([128, TK], name="wk")
    hTf = p.sb([128, 8, 128]); hTb = p.sb([128, 8, 128], BF16)
    ct = p.sb([128, 128]); sq = p.sb([128, 128]); cT = p.sb([128, 128]); sm = p.sb([128, 8], name="dsm")
    for kt in range(NKT):
        ks = slice(kt * 128, kt * 128 + 128)
        load_hblock(p, hT_d, kt * 128, 128, hTf, hTb)
        for c in range(8):
            p.mm(PS[0][0:32, 0:128], Widx[:, c, 256:288], hTf[:, c, :], start=(c == 0), stop=(c == 7))
        p.copy('act', kiT[:, ks], PS[0][0:32, 0:128])
        proj_tm(p, PS[1][:, 0:128], Wc, 0, 128, hTb, 0, 128)
        p.copy('act', ct, PS[1][:, 0:128])
        p.tt('dve', sq, ct, ct, ALU.mult)
        p.op('dve', lambda e: e.reduce_sum(sm[:, 0:1], sq, AX.X), r=[sq], w=[sm])
        p.act(sm[:, 0:1], sm[:, 0:1], AF.Sqrt, bias=eps6, scale=1.0 / 128.0)
        p.op('dve', lambda e: e.reciprocal(sm[:, 0:1], sm[:, 0:1]), r=[sm], w=[sm])
        p.stt(ct, ct, sm[:, 0:1], kvg, ALU.mult, ALU.mult)
        p.tr(PS[2][:, 0:128], ct, ident)
        p.copy('act', cT, PS[2][:, 0:128])
        p.mm(PS[3][0:64, 0:128], wuk, cT)
        p.copy('act', kT[:, ks], PS[3][0:64, 0:128])
        p.mm(PS[3][:, 128:192], cT, wuv)
        p.copy('act', v_all[:, kt, :], PS[3][:, 128:192])
    qT = p.sb([64, 4, 128], BF16, name="qT"); qiT = p.sb([32, 8, 128], name="qiT"); wi = p.sb([128, 8], name="wi")
    if fused:
        hTf2 = p.sb([128, 8, 128], name="hTf2"); rkq = p.sb([128, 2], name="rkq"); p.dma('sp', rkq, cx.over["rk"])
    rel = [p.sb([128, 512], name="rel%d" % i) for i in range(2)]
    m8 = p.sb([128, 8], name="m8"); PT = [p.sb([128, 128], BF16, name="PT%d" % i) for i in range(2)]
    yt = p.sb([128, 256], name="yt")
    for bi in range(NSLOT):
        S = min((2 * bi + 2) * 128, TK)
        jA = 2 * bi
        if not fused:
            load_hblock(p, hTq_d, bi * 128, 128, hTf, hTb)
        else:
            hsrc = hT_d.rearrange("(c f) n -> f c n", f=128)
            p.dma('sp', hTf, hsrc[:, :, jA * 128:jA * 128 + 128])
            if (jA + 2) * 128 <= TK:
                p.dma('sp', hTf2, hsrc[:, :, (jA + 1) * 128:(jA + 2) * 128])
            else:
                p.memset('pool', hTf2, 0.0)
            p.ts('dve', hTf, hTf, rkq[:, 0:1], None, ALU.mult)
            p.stt(hTf, hTf2, rkq[:, 1:2], hTf, ALU.mult, ALU.add)
            p.copy('pool', hTb, hTf)
        for h in range(4):
            proj_fm(p, PS[0][0:64, 0:128], Wq, h * 64, 64, hTb, 0, 128)
            p.op('act', lambda e: e.mul(qT[:, h, :], PS[0][0:64, 0:128], 0.125), r=[PS[0]], w=[qT])
        for hi in range(8):
            for c in range(8):
                p.mm(PS[1][0:32, 0:128], Widx[:, c, hi * 32:(hi + 1) * 32], hTf[:, c, :], start=(c == 0), stop=(c == 7))
            p.copy('act', qiT[:, hi, :], PS[1][0:32, 0:128])
        for c in range(8):
            p.mm(PS[2][:, 0:8], hTf[:, c, :], Widx[:, c, 288:296], start=(c == 0), stop=(c == 7))
        p.op('act', lambda e: e.mul(wi, PS[2][:, 0:8], 1.0 / 16.0), r=[PS[2]], w=[wi])
        for k0 in range(0, S, 512):
            kn = min(512, S - k0)
            for hi in range(8):
                pb = PS[3 + hi % 2]
                p.mm(pb[:, 0:kn], qiT[:, hi, :], kiT[:, k0:k0 + kn])
                r_ = rel[hi % 2]
                p.act(r_[:, 0:kn], pb[:, 0:kn], AF.Relu)
                if hi == 0:
                    p.ts('dve', score[:, k0:k0 + kn], r_[:, 0:kn], wi[:, 0:1], None, ALU.mult)
                else:
                    p.stt(score[:, k0:k0 + kn], r_[:, 0:kn], wi[:, hi:hi + 1], score[:, k0:k0 + kn], ALU.mult, ALU.add)
        p.memset('dve', score[:, 0:N_META], 1e30)
        p.tt('dve', score[:, jA * 128:jA * 128 + 128], score[:, jA * 128:jA * 128 + 128], maskA, ALU.min)
        if (jA + 2) * 128 <= S:
            p.tt('dve', score[:, (jA + 1) * 128:(jA + 2) * 128], score[:, (jA + 1) * 128:(jA + 2) * 128], maskB, ALU.min)
        if S > NROUND * 8:
            p.copy('pool', wk[:, 0:S], score[:, 0:S])
            p.memset('pool', wk[:, 0:N_META], -3e38)
            NR = NROUND - N_META // 8
            for r in range(NR):
                p.op('dve', lambda e: e.max(m8, wk[:, 0:S]), r=[wk], w=[m8])
                if r < NR - 1:
                    p.op('dve', lambda e: e.match_replace(wk[:, 0:S], m8, wk[:, 0:S], -3e38), r=[m8, wk], w=[wk])
            p.ts('dve', sm[:, 1:2], m8[:, 7:8], -1e29, None, ALU.max)
        else:
            p.memset('dve', sm[:, 1:2], -1e29)
        p.ts('dve', wk[:, 0:S], score[:, 0:S], sm[:, 1:2], 1.0, ALU.is_ge, ALU.subtract)
        lg = score
        for h in range(4):
            for k0 in range(0, S, 512):
                kn = min(512, S - k0)
                pb = PS[5 + (k0 // 512) % 2]
                p.mm(pb[:, 0:kn], qT[:, h, :], kT[:, k0:k0 + kn])
                p.stt(lg[:, k0:k0 + kn], wk[:, k0:k0 + kn], 1e30, pb[:, 0:kn], ALU.mult, ALU.add)
            for k in range(3):
                jb = jA - 1 + k
                if jb >= 0 and (jb + 1) * 128 <= S:
                    p.tt('dve', lg[:, jb * 128:(jb + 1) * 128], lg[:, jb * 128:(jb + 1) * 128], Badj[k][:, h, :], ALU.add)
            p.op('dve', lambda e: e.reduce_max(sm[:, 2:3], lg[:, 0:S], AX.X), r=[lg], w=[sm])
            p.ts('dve', sm[:, 3:4], sm[:, 2:3], -1.0, None, ALU.mult)
            p.act(lg[:, 0:S], lg[:, 0:S], AF.Exp, bias=sm[:, 3:4], accum_out=sm[:, 4:5])
            nkb = S // 128
            for kb in range(nkb):
                pt = PS[1 + kb % 2]
                p.tr(pt[:, 0:128], lg[:, kb * 128:(kb + 1) * 128], ident)
                p.copy('act' if kb % 2 == 0 else 'dve', PT[kb % 2], pt[:, 0:128])
                p.mm(PS[7][:, 0:64], PT[kb % 2], v_all[:, kb, :], start=(kb == 0), stop=(kb == nkb - 1))
            p.op('dve', lambda e: e.reciprocal(sm[:, 5:6], sm[:, 4:5]), r=[sm], w=[sm])
            p.ts('dve', yt[:, h * 64:(h + 1) * 64], PS[7][:, 0:64], sm[:, 5:6], None, ALU.mult)
        p.dma('sp', y_d[bi * 128:(bi + 1) * 128, :], yt)
    p.pop()


def build_dsa(NKT, NSLOT, NROUND):
    return standalone(emit_dsa, NKT, NSLOT, NROUND)


OFFS = [0, 1024, 1808, 2488, 3520, 7616]


def _c(a):
    return np.ascontiguousarray(a, dtype=np.float32)


def _t5_bucket(n):
    n = np.maximum(n, 0)
    me = 16
    large = me + (np.log(np.maximum(n, 1).astype(np.float32) / me) / np.log(128 / me) * (32 - me)).astype(np.int32)
    return np.where(n < me, n, np.minimum(large, 31))


def _run(nc, in_maps):
    res = run_bass_kernel_spmd(nc, in_maps, core_ids=list(range(NCORES)))
    return res.results


def _gla_inputs(inp, l, hp, hT, NCH):
    w_in = inp['w_in'][l]; c0 = OFFS[1]
    heads = [2 * hp, 2 * hp + 1]
    qcols = np.concatenate([np.arange(c0 + hh * 32, c0 + hh * 32 + 32) for hh in heads])
    kcols = qcols + 128
    vcols = np.concatenate([np.arange(c0 + 256 + hh * 64, c0 + 256 + hh * 64 + 64) for hh in heads])
    acols = np.arange(c0 + 512, c0 + 528)
    ocols = vcols + 256 + 16
    return dict(hT=hT, wqk=_c(w_in[:, np.concatenate([qcols, kcols])]), wvo=_c(w_in[:, np.concatenate([vcols, ocols])]),
                wa=_c(w_in[:, acols]), aup=_c(inp['gla_a_up'][l][:, hp * 64:(hp + 1) * 64]),
                ab=_c(inp['gla_a_b'][l][hp * 64:(hp + 1) * 64, None]), ng=_c(np.tile(inp['gla_norm_g'][l], 2)),
                cmask=(np.arange(NCH * 64) % 64 != 0).astype(np.float32),
                maskU2=_c(np.tile(np.triu(np.ones((64, 64), np.float32)), (1, 2))), ident=np.eye(128, dtype=np.float32))


def _mlstm_inputs(inp, l, hp, hT, NCH):
    w_in = inp['w_in'][l]; c0 = OFFS[3]
    heads = [2 * hp, 2 * hp + 1]
    hc = np.concatenate([np.arange(hh * 64, hh * 64 + 64) for hh in heads])
    P = NCH * 64; pos = np.arange(P); real = (pos >= 48)
    return dict(hT=hT, wq=_c(w_in[:, c0 + hc]), wk=_c(w_in[:, c0 + 256 + hc]),
                wvo=_c(w_in[:, np.concatenate([c0 + 512 + hc, c0 + 776 + hc])]),
                wi=_c(np.repeat(w_in[:, [c0 + 768 + hh for hh in heads]], 64, axis=1)),
                wf=_c(np.repeat(w_in[:, [c0 + 772 + hh for hh in heads]], 64, axis=1)),
                cwq=_c(inp['mlstm_conv_w'][l][:, hc].T), cwk=_c(inp['mlstm_conv_w'][l][:, 256 + hc].T),
                cbq=_c(inp['mlstm_conv_b'][l][hc, None]), cbk=_c(inp['mlstm_conv_b'][l][256 + hc, None]),
                ib=_c(np.repeat(inp['mlstm_i_b'][l][heads], 64)[:, None]), fb=_c(np.repeat(inp['mlstm_f_b'][l][heads], 64)[:, None]),
                ng=_c(inp['mlstm_norm_g'][l][hc]), cmask=(pos % 64 != 0).astype(np.float32),
                pm01=real.astype(np.float32), pmneg=np.where(real, 0.0, -1e30).astype(np.float32),
                maskL=np.where(np.tril(np.ones((64, 64))) > 0, 0.0, -1e30).astype(np.float32), ident=np.eye(128, dtype=np.float32))


def _rwkv_inputs(inp, l, hp, hT):
    w_in = inp['w_in'][l]
    hc = np.arange(hp * 128, hp * 128 + 128)
    colsel = np.concatenate([hc, 256 + hc, 512 + hc, np.arange(768, 832), np.arange(832, 896), np.arange(896, 1024)])
    mu = inp['rwkv_mu'][l]
    mu6 = np.zeros((128, 6), np.float32)
    mu6[:, 0] = mu[hc]; mu6[:, 1] = mu[256 + hc]; mu6[:, 2] = mu[512 + hc]
    mu6[:64, 3] = mu[768:832]; mu6[:64, 4] = mu[832:896]; mu6[:, 5] = mu[896:1024]
    cols = np.zeros((128, 8), np.float32)
    cols[:, 0] = inp['rwkv_w0'][l][hc]; cols[:, 1] = inp['rwkv_a0'][l][hc]; cols[:, 2] = inp['rwkv_k_k'][l][hc]
    cols[:, 3] = inp['rwkv_k_a'][l][hc]; cols[:, 4] = inp['rwkv_r_k'][l].reshape(-1)[hc]
    bo = np.zeros((128, 128), np.float32); bo[:64, :64] = 1; bo[64:, 64:] = 1
    return dict(hT=hT, w=_c(w_in[:, colsel]), mu=mu6, wup=_c(inp['rwkv_w_up'][l][:, hc]), aup=_c(inp['rwkv_a_up'][l][:, hc]),
                gup=_c(inp['rwkv_g_up'][l][:, hc]), cols=cols, gng=_c(inp['rwkv_gn_g'][l][hc]), gnb=_c(inp['rwkv_gn_b'][l][hc]),
                blockones=bo, ident=np.eye(128, dtype=np.float32))


def _dsa_inputs(inp, l, half, hT_tok, NSLOT):
    w_in = inp['w_in'][l]; c0 = OFFS[2]; rb = inp['rel_bias']
    i = np.arange(128)[:, None]; s = np.arange(128)[None, :]
    bd = _t5_bucket(i - s); bp = _t5_bucket(128 + i - s)
    Draw = np.stack([np.where(s <= i, rb[bd, hh], rb[31, hh]) for hh in range(4)]).astype(np.float32)
    Praw = np.stack([rb[bp, hh] for hh in range(4)]).astype(np.float32)
    c31t = np.stack([np.full((128, 128), rb[31, hh]) for hh in range(4)]).astype(np.float32)
    B3 = np.stack([Praw, Draw, c31t]) if half == 0 else np.stack([c31t, Praw, Draw])
    mm = np.where(s <= i, 3e38, -1e30).astype(np.float32)
    maskA = mm if half == 0 else np.full((128, 128), 3e38, np.float32)
    maskB = np.full((128, 128), -1e30, np.float32) if half == 0 else mm
    hTq = np.zeros((1024, NSLOT * 128), np.float32)
    for ii in range(NSLOT):
        j = 2 * ii + half
        if j * 128 < hT_tok.shape[1]:
            hTq[:, ii * 128:(ii + 1) * 128] = hT_tok[:, j * 128:(j + 1) * 128]
    return dict(hT=hT_tok, hTq=hTq, B3raw=_c(B3), maskA=maskA, maskB=maskB,
                wq=_c(w_in[:, c0:c0 + 256]), wckv=_c(w_in[:, c0 + 256:c0 + 384]), widx=_c(w_in[:, c0 + 384:c0 + 680]),
                kvg=_c(inp['dsa_kv_norm_g'][l]), wuk=_c(inp['dsa_w_uk'][l]), wuv=_c(inp['dsa_w_uv'][l]),
                c31=_c(np.tile(rb[31][None, :], (128, 1))), ident=np.eye(128, dtype=np.float32))


def build_fused(NTL, NCH, NSTEP, NKT, NSLOT, NROUND, HALF):
    nc, p, PS = new_prog()
    NTOK = NTL * 128; P_ = NCH * 64; TK = NKT * 128
    PW = max(48 + HALF + NTOK, 48 + TK, P_)
    PWy = PW
    groups = [[2 * g, 2 * g + 1] for g in range(NCORES // 2)]
    ext = lambda n, s_: nc.dram_tensor(n, list(s_), F32, kind="ExternalInput").ap()
    itn = lambda n, s_: nc.dram_tensor(n, list(s_), F32, kind="Internal").ap()
    ident_d = ext("ident", [128, 128]); rk_d = ext("rk", [128, 2]); x_d = ext("x", [NTOK, D])
    out_d = nc.dram_tensor("out", [NTOK, D], F32, kind="ExternalOutput").ap()
    h_own = itn("h_own", [NTOK, D]); hT_own = itn("hT_own", [D, NTOK])
    hT_pos = itn("hT_pos", [D, PW]); Y_abd = itn("Y_abd", [PWy, 384]); Y_c = itn("Y_c", [NSLOT * 128, 256])

    class Gathered:
        def __init__(self, name, src, bounds):
            self.src, self.bounds = src, bounds
            self.bufs = [itn("%s_%d" % (name, k), [2 * (b1 - b0), src.shape[1]]) for k, (b0, b1) in enumerate(bounds)]

        def gather(self):
            for (b0, b1), g in zip(self.bounds, self.bufs):
                p.coll("AllGather", self.src[b0:b1, :], g, groups)

        def rows(self, q, r0, n):
            for (b0, b1), g in zip(self.bounds, self.bufs):
                if b0 <= r0 and r0 + n <= b1:
                    return g[q * (b1 - b0) + r0 - b0:q * (b1 - b0) + r0 - b0 + n, :]
            raise ValueError("row range straddles gather chunks")

    def bounds_of(total, step, first=None):
        bs = []
        b0 = 0
        nxt = first if first is not None else step
        while b0 < total:
            b1 = min(total, nxt)
            bs.append((b0, b1)); b0 = b1; nxt = b1 + step
        return bs
    G_h = Gathered("G_h", hT_own, bounds_of(D, 64))
    G_abd = Gathered("G_abd", Y_abd, bounds_of(PWy, 1024, 48 + 1024))
    G_c = Gathered("G_c", Y_c, bounds_of(NSLOT * 128, 1024))
    p.push()
    z = p.sb([128, 512], name="zeros"); p.memset('dve', z, 0.0)
    for c in range(8):
        p.dma('sp', hT_pos[c * 128:(c + 1) * 128, 0:48], z[:, 0:48])
    for r0 in range(0, PWy, 128):
        n = min(128, PWy - r0)
        p.dma('sp', Y_abd[r0:r0 + n, :], z[0:n, 0:384])
    p.pop()
    emit_ln_in(Ctx(nc, p, PS, "ln_", dict(x=x_d, y=h_own, hT_out=hT_own, ident=ident_d)), NTL)
    for l in range(DEPTH):
        pre = "l%d_" % l
        G_h.gather()
        for q in range(2):
            for (b0, b1) in G_h.bounds:
                p.dma('sp', hT_pos[b0:b1, 48 + HALF * q:48 + HALF * q + NTOK], G_h.rows(q, b0, b1 - b0))
        emit_rwkv(Ctx(nc, p, PS, pre + "rwkv_", dict(hT=hT_pos[:, 0:P_], y=Y_abd[0:P_, 0:128], ident=ident_d)), NCH, NSTEP)
        gg = gla_gen(Ctx(nc, p, PS, pre + "gla_", dict(hT=hT_pos[:, 0:P_], y=Y_abd[0:P_, 128:256], ident=ident_d)), NCH)
        next(gg)
        p.bg = gg
        emit_mlstm(Ctx(nc, p, PS, pre + "mlstm_", dict(hT=hT_pos[:, 0:P_], y=Y_abd[0:P_, 256:384], ident=ident_d)), NCH)
        p.drain_bg()
        emit_dsa(Ctx(nc, p, PS, pre + "dsa_", dict(hT=hT_pos[:, 48:48 + TK], y=Y_c, ident=ident_d, rk=rk_d)), NKT, NSLOT, NROUND)
        G_abd.gather()
        G_c.gather()
        last = (l == DEPTH - 1)
        over = dict(h=h_own, hT=hT_own, G_abd=G_abd, G_c=G_c, rk=rk_d, PWy=PWy, HALF=HALF, NSLOT=NSLOT, ident=ident_d,
                    out=(out_d if last else h_own))
        if not last:
            over["hT_out"] = hT_own
        emit_post(Ctx(nc, p, PS, pre + "post_", over), NTL, 17)
    p.finish()
    return nc


def kernel(**inputs):
    inp = {k: np.asarray(v) for k, v in inputs.items()}
    x = inp['x'].astype(np.float32)
    B, S, _ = x.shape
    T = S + N_META
    HALF = S // 2
    NTL = -(-(T - HALF) // 128)
    NTOK = NTL * 128
    NCH = -(-(T + 48 + 1) // 64); NCH += NCH % 2
    NSTEP = 48 + T + 1
    NKT = -(-T // 128)
    NSLOT = (NKT + 1) // 2
    NROUND = min(256, S // 4) // 8
    assert B * 2 == NCORES and (HALF // 128) % 2 == 0
    nc = build_fused(NTL, NCH, NSTEP, NKT, NSLOT, NROUND, HALF)
    hcat = np.concatenate([np.broadcast_to(inp['meta'][None].astype(np.float32), (B, N_META, D)), x], 1)
    maps = []
    for c in range(NCORES):
        b, r = c // 2, c % 2
        xo = np.zeros((NTOK, D), np.float32)
        seg = hcat[b, r * HALF:min(T, r * HALF + NTOK)] if r == 1 else hcat[b, 0:HALF]
        xo[:len(seg)] = seg
        m = dict(x=xo, ident=np.eye(128, dtype=np.float32), rk=np.tile(np.array([[1.0 - r, float(r)]], np.float32), (128, 1)))
        m["ln_g"] = _c(inp['ln_in_g']); m["ln_b"] = _c(inp['ln_in_b'])
        dummy = np.zeros((D, 1), np.float32)
        for l in range(DEPTH):
            pre = "l%d_" % l
            for nm, d in (("rwkv_", _rwkv_inputs(inp, l, r, dummy)), ("gla_", _gla_inputs(inp, l, r, dummy, NCH)),
                          ("mlstm_", _mlstm_inputs(inp, l, r, dummy, NCH)), ("dsa_", _dsa_inputs(inp, l, r, dummy, 0))):
                for k, v in d.items():
                    if k not in ("hT", "hTq", "ident"):
                        m[pre + nm + k] = v
            wr = _c(np.concatenate([inp['moe_w_grp'][l], inp['moe_w_rt'][l]], 1))
            br = _c(np.concatenate([inp['moe_b_grp'][l], inp['moe_b_rt'][l]], 0))
            post = dict(wg=_c(inp['w_in'][l][:, OFFS[4]:OFFS[5]]), wb=_c(inp['w_branch'][l].reshape(D, D)), wo=_c(inp['w_out'][l]),
                        ln1g=_c(inp['ln1_g'][l]), ln1b=_c(inp['ln1_b'][l]), ln2g=_c(inp['ln2_g'][l]), ln2b=_c(inp['ln2_b'][l]),
                        wr=wr, br=br, mg=_c(inp['moe_w_gate'][l]), mu=_c(inp['moe_w_up'][l]), md=_c(inp['moe_w_down'][l]))
            for k, v in post.items():
                m[pre + "post_" + k] = v
        maps.append(m)
    res = _run(nc, maps)
    out = np.zeros((B, T, D), np.float32)
    for c in range(NCORES):
        b, r = c // 2, c % 2
        n = HALF if r == 0 else T - HALF
        out[b, r * HALF:r * HALF + n] = res[c]["out"][:n]
    return np.ascontiguousarray(out[:, N_META:])


def kernel_unfused(**inputs):
    inp = {k: np.asarray(v) for k, v in inputs.items()}
    x = inp['x'].astype(np.float32)
    B, S, _ = x.shape
    T = S + N_META
    NTOKC = (B * T) // NCORES
    NTL = -(-NTOKC // 128)
    NCH = -(-(T + 48) // 64); NCH += NCH % 2
    if NCH * 64 < 48 + T + 1:
        NCH += 2
    P = NCH * 64
    NSTEP = 48 + T + 1
    NKT = -(-T // 128)
    NSLOT = (NKT + 1) // 2
    NROUND = min(256, S // 4) // 8
    ident = np.eye(128, dtype=np.float32)

    def tok_split(a):
        out = []
        for c in range(NCORES):
            sh = np.zeros((NTL * 128, a.shape[1]), np.float32)
            sh[:NTOKC] = a[c * NTOKC:(c + 1) * NTOKC]
            out.append(sh)
        return out

    def tok_merge(res, key):
        return np.concatenate([r[key][:NTOKC] for r in res], 0)

    hcat = np.concatenate([np.broadcast_to(inp['meta'][None], (B, N_META, D)), x], 1).reshape(B * T, D)
    res = _run(build_ln_in(NTL), [dict(x=s_, g=_c(inp['ln_in_g']), b=_c(inp['ln_in_b'])) for s_ in tok_split(hcat)])
    h = tok_merge(res, "y")

    for l in range(DEPTH):
        hb = h.reshape(B, T, D)
        hTpos = []
        hTtok = []
        for b in range(B):
            a = np.zeros((D, P), np.float32); a[:, 48:48 + T] = hb[b].T; hTpos.append(a)
            a2 = np.zeros((D, NKT * 128), np.float32); a2[:, :T] = hb[b].T; hTtok.append(a2)
        y = np.zeros((B, T, D), np.float32)
        res = _run(build_rwkv(NCH, NSTEP), [_rwkv_inputs(inp, l, c % 2, hTpos[c // 2]) for c in range(NCORES)])
        for c in range(NCORES):
            y[c // 2, :, (c % 2) * 128:(c % 2) * 128 + 128] = res[c]["y"][48:48 + T]
        res = _run(build_gla(NCH), [_gla_inputs(inp, l, c % 2, hTpos[c // 2], NCH) for c in range(NCORES)])
        for c in range(NCORES):
            y[c // 2, :, 256 + (c % 2) * 128:256 + (c % 2) * 128 + 128] = res[c]["y"][48:48 + T]
        res = _run(build_mlstm(NCH), [_mlstm_inputs(inp, l, c % 2, hTpos[c // 2], NCH) for c in range(NCORES)])
        for c in range(NCORES):
            y[c // 2, :, 768 + (c % 2) * 128:768 + (c % 2) * 128 + 128] = res[c]["y"][48:48 + T]
        res = _run(build_dsa(NKT, NSLOT, NROUND), [_dsa_inputs(inp, l, c % 2, hTtok[c // 2], NSLOT) for c in range(NCORES)])
        for c in range(NCORES):
            half = c % 2
            for ii in range(NSLOT):
                j = 2 * ii + half
                if j * 128 >= T:
                    continue
                n = min(128, T - j * 128)
                y[c // 2, j * 128:j * 128 + n, 512:768] = res[c]["y"][ii * 128:ii * 128 + n]
        yf = y.reshape(B * T, D)
        hs = tok_split(h); ys = tok_split(yf)
        wr = _c(np.concatenate([inp['moe_w_grp'][l], inp['moe_w_rt'][l]], 1))
        br = _c(np.concatenate([inp['moe_b_grp'][l], inp['moe_b_rt'][l]], 0))
        common = dict(wg=_c(inp['w_in'][l][:, OFFS[4]:OFFS[5]]), wb=_c(inp['w_branch'][l].reshape(D, D)), wo=_c(inp['w_out'][l]),
                      ln1g=_c(inp['ln1_g'][l]), ln1b=_c(inp['ln1_b'][l]), ln2g=_c(inp['ln2_g'][l]), ln2b=_c(inp['ln2_b'][l]),
                      wr=wr, br=br, mg=_c(inp['moe_w_gate'][l]), mu=_c(inp['moe_w_up'][l]), md=_c(inp['moe_w_down'][l]), ident=ident)
        maps = [dict(h=hs[c], hT=_c(hs[c].T), yT=_c(ys[c].T), **common) for c in range(NCORES)]
        res = _run(build_post(NTL, 8), maps)
        h = tok_merge(res, "out")
    return np.ascontiguousarray(h.reshape(B, T, D)[:, N_META:]).astype(np.float32)
```
